# Optimizing a Trainium2 kernel written in Bass

```python
import jax, jax.numpy as jnp
from jax import lax
import numpy as np

D_MODEL = 2048
BATCH = 8
SEQ = 2048
DEPTH = 2

M_HEADS = 4
M_QK = 128
M_V = 256
M_QK_W = M_HEADS * M_QK
M_V_W = M_HEADS * M_V
M_CHUNK = 128
MLA_HEADS = 8
NOPE_DIM = 128
ROPE_DIM = 64
QK_DIM = NOPE_DIM + ROPE_DIM
MLA_V = 128
MLA_W = MLA_HEADS * MLA_V
Q_RANK = 512
KV_RANK = 256
ROPE_THETA = 10000.0
Q_BLOCK = 128
POOL_WINDOWS = (2, 4, 8, 16)
POOL_GROUPS = 4
POOL_GROUP_W = 256
POOL_W = POOL_GROUPS * POOL_GROUP_W
N_BRANCH = 3
BRANCH_W = 1024
FFN_DIM = ((8 * D_MODEL // 3 + 255) // 256) * 256
NORM_EPS = 1e-6
IN_SIZES = (M_QK_W, M_QK_W, M_V_W, M_V_W, M_HEADS, M_HEADS, Q_RANK, KV_RANK, ROPE_DIM, POOL_W, N_BRANCH * D_MODEL)
IN_W = sum(IN_SIZES)

kernel_name = "hybrid_mlstm_mla_pool_adaln"


def rmsnorm(x, g):
    xf = x.astype(jnp.float32)
    y = xf * lax.rsqrt(jnp.mean(xf * xf, axis=-1, keepdims=True) + NORM_EPS)
    return (y * g.astype(jnp.float32)).astype(x.dtype)


def modulate(h, shift, scale):
    return h * (1 + scale[:, None, :]) + shift[:, None, :]


def split_cols(z, sizes):
    out = []
    off = 0
    for s in sizes:
        out.append(z[..., off:off + s])
        off += s
    return out


def rope(x):
    S, R = x.shape[1], x.shape[-1]
    pos = jnp.arange(S, dtype=jnp.float32)
    freqs = ROPE_THETA ** (-jnp.arange(0, R, 2, dtype=jnp.float32) / R)
    ang = pos[:, None] * freqs[None, :]
    cos = jnp.cos(ang)[None, :, None, :].astype(x.dtype)
    sin = jnp.sin(ang)[None, :, None, :].astype(x.dtype)
    x1, x2 = x[..., : R // 2], x[..., R // 2:]
    return jnp.concatenate([x1 * cos - x2 * sin, x1 * sin + x2 * cos], axis=-1)


def causal_attention(q, k, v):
    S, Dh = q.shape[1], q.shape[-1]
    scale = Dh ** -0.5
    outs = []
    for blk in range(S // Q_BLOCK):
        s0, s1 = blk * Q_BLOCK, (blk + 1) * Q_BLOCK
        logits = jnp.einsum('bqhd,bkhd->bhqk', q[:, s0:s1], k[:, :s1],
                            preferred_element_type=jnp.float32) * scale
        mask = (s0 + jnp.arange(Q_BLOCK))[:, None] >= jnp.arange(s1)[None, :]
        logits = jnp.where(mask, logits, -jnp.inf)
        p = jax.nn.softmax(logits, axis=-1).astype(v.dtype)
        outs.append(jnp.einsum('bhqk,bkhv->bqhv', p, v[:, :s1]))
    return jnp.concatenate(outs, axis=1)


def mlstm_chunkwise(q, k, v, i_pre, f_pre):
    f32 = jnp.float32
    B, S, H, DK = q.shape
    DV = v.shape[-1]
    L = M_CHUNK
    NC = S // L
    q = q.astype(f32)
    k = k.astype(f32) * (DK ** -0.5)
    v = v.astype(f32)
    ig = i_pre.astype(f32)
    logf = jax.nn.log_sigmoid(f_pre.astype(f32))

    def to_chunks(a):
        return jnp.moveaxis(a.reshape((B, NC, L) + a.shape[2:]), (1, 3), (0, 2))

    qc, kc, vc = to_chunks(q), to_chunks(k), to_chunks(v)
    ic = to_chunks(ig)
    bc = jnp.cumsum(to_chunks(logf), axis=-1)
    tril = jnp.tril(jnp.ones((L, L), dtype=bool))

    def step(carry, inp):
        C, n, m = carry
        qb, kb, vb, ib, bb = inp
        a = bb + m[..., None]
        Dm = bb[..., :, None] - bb[..., None, :] + ib[..., None, :]
        Dm = jnp.where(tril, Dm, -jnp.inf)
        m_t = jnp.maximum(a, jnp.max(Dm, axis=-1))
        w_inter = jnp.exp(a - m_t)
        s = jnp.einsum('bhtd,bhsd->bhts', qb, kb) * jnp.exp(Dm - m_t[..., None])
        num = (w_inter[..., None] * jnp.einsum('bhvd,bhtd->bhtv', C, qb)
               + jnp.einsum('bhts,bhsv->bhtv', s, vb))
        den = w_inter * jnp.einsum('bhd,bhtd->bht', n, qb) + jnp.sum(s, axis=-1)
        h = num / jnp.maximum(jnp.abs(den), jnp.exp(-m_t))[..., None]
        bL = bb[..., -1]
        g = bL[..., None] - bb + ib
        m_new = jnp.maximum(bL + m, jnp.max(g, axis=-1))
        decay = jnp.exp(bL + m - m_new)
        ws = jnp.exp(g - m_new[..., None])
        C_new = decay[..., None, None] * C + jnp.einsum('bhs,bhsv,bhsd->bhvd', ws, vb, kb)
        n_new = decay[..., None] * n + jnp.einsum('bhs,bhsd->bhd', ws, kb)
        return (C_new, n_new, m_new), h

    init = (jnp.zeros((B, H, DV, DK), f32), jnp.zeros((B, H, DK), f32), jnp.zeros((B, H), f32))
    _, hs = lax.scan(step, init, (qc, kc, vc, ic, bc))
    return jnp.moveaxis(hs, (0, 2), (1, 3)).reshape(B, S, H, DV)


def pool_mixer(u, w_pool, b_pool, s_pool):
    B, S, _ = u.shape
    uf = u.astype(jnp.float32).reshape(B, S, POOL_GROUPS, POOL_GROUP_W)
    cs = jnp.cumsum(uf, axis=1)
    t = jnp.arange(S)
    outs = []
    for g, w in enumerate(POOL_WINDOWS):
        csg = cs[:, :, g]
        lag = jnp.pad(csg, ((0, 0), (w, 0), (0, 0)))[:, :S]
        cnt = jnp.minimum(t + 1, w).astype(jnp.float32)[None, :, None]
        outs.append((csg - lag) / cnt - uf[:, :, g])
    pooled = jnp.stack(outs, axis=2).astype(u.dtype)
    y = jnp.einsum('bsgc,gce->bsge', pooled, w_pool) + b_pool
    return y.reshape(B, S, POOL_W) * s_pool


def setup_inputs(seed: int = 0) -> dict:
    key = jax.random.key(seed)
    ks = jax.random.split(key, 24)
    f32 = jnp.float32

    def dense(k, shape, fan_in):
        return jax.random.normal(k, shape, f32) * (fan_in ** -0.5)

    def gain(k, shape):
        return 1.0 + 0.1 * jax.random.normal(k, shape, f32)

    kb = jax.random.split(ks[6], 2)
    b_mgate = jnp.stack([0.1 * jax.random.normal(kb[0], (DEPTH, M_HEADS), f32),
                         3.0 + 0.5 * jax.random.normal(kb[1], (DEPTH, M_HEADS), f32)], axis=1)
    return {
        "x": jax.random.normal(ks[0], (BATCH, SEQ, D_MODEL), f32),
        "c": jax.random.normal(ks[1], (BATCH, D_MODEL), f32),
        "w_ada": dense(ks[2], (DEPTH, D_MODEL, 6 * D_MODEL), D_MODEL),
        "b_ada": 0.02 * jax.random.normal(ks[3], (DEPTH, 6 * D_MODEL), f32),
        "g_norm1": gain(ks[4], (DEPTH, D_MODEL)),
        "w_in": dense(ks[5], (DEPTH, D_MODEL, IN_W), D_MODEL),
        "b_mgate": b_mgate,
        "g_mnorm": gain(ks[7], (DEPTH, M_HEADS, M_V)),
        "g_qlat": gain(ks[8], (DEPTH, Q_RANK)),
        "w_uq": dense(ks[9], (DEPTH, Q_RANK, MLA_HEADS * QK_DIM), Q_RANK),
        "g_kvlat": gain(ks[10], (DEPTH, KV_RANK)),
        "w_ukv": dense(ks[11], (DEPTH, KV_RANK, MLA_HEADS * (NOPE_DIM + MLA_V)), KV_RANK),
        "g_qn": gain(ks[12], (DEPTH, QK_DIM)),
        "g_kn": gain(ks[13], (DEPTH, QK_DIM)),
        "w_pool": dense(ks[14], (DEPTH, POOL_GROUPS, POOL_GROUP_W, POOL_GROUP_W), POOL_GROUP_W),
        "b_pool": 0.02 * jax.random.normal(ks[15], (DEPTH, POOL_GROUPS, POOL_GROUP_W), f32),
        "s_pool": gain(ks[16], (DEPTH, POOL_W)),
        "w_branch": dense(ks[17], (DEPTH, N_BRANCH, BRANCH_W, D_MODEL), BRANCH_W),
        "w_out": dense(ks[18], (DEPTH, D_MODEL, D_MODEL), D_MODEL),
        "g_norm2": gain(ks[19], (DEPTH, D_MODEL)),
        "w_ffn_in": dense(ks[20], (DEPTH, D_MODEL, 2 * FFN_DIM), D_MODEL),
        "w_ffn_out": dense(ks[21], (DEPTH, FFN_DIM, D_MODEL), FFN_DIM),
    }


def reference(x, c, w_ada, b_ada, g_norm1, w_in, b_mgate, g_mnorm, g_qlat, w_uq, g_kvlat, w_ukv,
              g_qn, g_kn, w_pool, b_pool, s_pool, w_branch, w_out, g_norm2, w_ffn_in, w_ffn_out):
    B, S, D = x.shape
    c_act = jax.nn.silu(c)
    for l in range(DEPTH):
        mod = c_act @ w_ada[l] + b_ada[l]
        shift1, scale1, gate1, shift2, scale2, gate2 = jnp.split(mod, 6, axis=-1)

        h = modulate(rmsnorm(x, g_norm1[l]), shift1, scale1)
        z = h @ w_in[l]
        q_m, k_m, v_m, o_m, i_m, f_m, q_lat, kv_lat, k_r, u_pool, gates = split_cols(z, IN_SIZES)

        hm = mlstm_chunkwise(q_m.reshape(B, S, M_HEADS, M_QK), k_m.reshape(B, S, M_HEADS, M_QK),
                             v_m.reshape(B, S, M_HEADS, M_V),
                             i_m + b_mgate[l, 0], f_m + b_mgate[l, 1]).astype(x.dtype)
        hm = rmsnorm(hm, g_mnorm[l]) * jax.nn.sigmoid(o_m.reshape(B, S, M_HEADS, M_V))
        br_a = hm.reshape(B, S, M_V_W)

        q = (rmsnorm(q_lat, g_qlat[l]) @ w_uq[l]).reshape(B, S, MLA_HEADS, QK_DIM)
        kv = (rmsnorm(kv_lat, g_kvlat[l]) @ w_ukv[l]).reshape(B, S, MLA_HEADS, NOPE_DIM + MLA_V)
        k_nope, v = kv[..., :NOPE_DIM], kv[..., NOPE_DIM:]
        k = jnp.concatenate([k_nope, jnp.broadcast_to(k_r[:, :, None, :], (B, S, MLA_HEADS, ROPE_DIM))], axis=-1)
        q = rmsnorm(q, g_qn[l])
        k = rmsnorm(k, g_kn[l])
        q = jnp.concatenate([q[..., :NOPE_DIM], rope(q[..., NOPE_DIM:])], axis=-1)
        k = jnp.concatenate([k[..., :NOPE_DIM], rope(k[..., NOPE_DIM:])], axis=-1)
        br_b = causal_attention(q, k, v).reshape(B, S, MLA_W)

        br_c = pool_mixer(u_pool, w_pool[l], b_pool[l], s_pool[l])

        g = jax.nn.sigmoid(gates).reshape(B, S, N_BRANCH, D)
        merged = (g[:, :, 0] * (br_a @ w_branch[l, 0])
                  + g[:, :, 1] * (br_b @ w_branch[l, 1])
                  + g[:, :, 2] * (br_c @ w_branch[l, 2]))
        x = x + gate1[:, None, :] * (merged @ w_out[l])

        h2 = modulate(rmsnorm(x, g_norm2[l]), shift2, scale2)
        gu = h2 @ w_ffn_in[l]
        ff = (jax.nn.silu(gu[..., :FFN_DIM]) * gu[..., FFN_DIM:]) @ w_ffn_out[l]
        x = x + gate2[:, None, :] * ff
    return x
```

```python
import types
import numpy as np
from contextlib import ExitStack
import concourse.bass as bass
import concourse.mybir as mybir
from concourse.bass_utils import run_bass_kernel_spmd

F32 = mybir.dt.float32
BF16 = mybir.dt.bfloat16
AF = mybir.ActivationFunctionType
ALU = mybir.AluOpType
AX = mybir.AxisListType

D = 2048
T = 2048
DEPTH = 2
NTB = T // 128
FFN = 5632
IN_W = 11080
EPS = 1e-6
C_QM, C_KM, C_VM, C_OM, C_IF, C_QL, C_KV, C_KR, C_UP, C_GT = 0, 512, 1024, 2048, 3072, 3080, 3592, 3848, 3912, 4936
NEG = -30000.0


class Buf:
    __slots__ = ("t", "w", "r", "name", "alt")

    def __init__(self, t, name=""):
        self.t = t
        self.w = {}
        self.r = {}
        self.name = name
        self.alt = None

    def __getitem__(self, k):
        return self.t[k]


def _freeze(fn):
    if fn.__closure__ is None:
        return fn
    cells = []
    for c in fn.__closure__:
        try:
            cells.append(types.CellType(c.cell_contents))
        except ValueError:
            cells.append(c)
    return types.FunctionType(fn.__code__, fn.__globals__, fn.__name__, fn.__defaults__, tuple(cells))


class Op:
    __slots__ = ("fn", "waits", "dma")

    def __init__(self, fn, waits, dma=None):
        self.fn = fn
        self.waits = waits
        self.dma = dma


ENGS = ["pe", "act", "dve", "pool", "sp"]


class KB:
    def __init__(self, nc, es, n_dma=24):
        self.nc = nc
        self.ops = {e: [] for e in ENGS}
        self.known = {e: {} for e in ENGS}
        self.sigset = {e: set() for e in ENGS}
        self.lastc = {e: 0 for e in ENGS}
        self.n_dma = n_dma
        self.dma_val = [0] * n_dma
        self.dma_rr2 = {True: 0, False: 0}
        self.csem = {e: es.enter_context(nc.semaphore("c_" + e)) for e in ENGS if e != "sp"}
        self.dsem = [es.enter_context(nc.semaphore("d%d" % i)) for i in range(n_dma)]

    def _collect(self, eng, reads, writes, wpart=()):
        need = {}
        own = ("c", eng)
        for b in reads:
            for k, v in b.w.items():
                if v > need.get(k, 0):
                    need[k] = v
        for b in writes:
            for dct in (b.w, b.r):
                for k, v in dct.items():
                    if v > need.get(k, 0):
                        need[k] = v
        for b in wpart:
            for k, v in b.w.items():
                if k == own:
                    continue
                if v > need.get(k, 0):
                    need[k] = v
        if eng == "pe":
            need.pop(own, None)
        waits = []
        kn = self.known[eng]
        for k, v in need.items():
            if kn.get(k, 0) >= v:
                continue
            kn[k] = v
            waits.append((k, v))
            if k[0] == "c":
                self.sigset[k[1]].add(v)
        return waits

    def op(self, eng, fn, reads=(), writes=(), wpart=()):
        waits = self._collect(eng, reads, writes, wpart)
        self.ops[eng].append(Op(_freeze(fn), waits))
        idx = len(self.ops[eng])
        self.lastc[eng] = idx
        key = ("c", eng)
        for b in reads:
            b.r[key] = idx
        for b in writes:
            b.w = {key: idx}
            b.r = {}
        for b in wpart:
            b.w[key] = idx
        return idx

    def dma(self, q, out_ap, in_ap, reads=(), writes=(), wpart=(), slow=False):
        lo, hi = (0, 6) if q == "pool" else (6, self.n_dma)
        k = lo + self.dma_rr2[q == "pool"] % (hi - lo)
        self.dma_rr2[q == "pool"] += 1
        prev = self.dma_val[k]
        waits = self._collect(q, reads, writes, wpart)
        key = ("d", k)
        if prev > 0 and self.known[q].get(key, 0) < prev:
            waits.append((key, prev))
            self.known[q][key] = prev
        new = prev + 16
        self.dma_val[k] = new
        if slow:
            fn = lambda e: e.dma_start(out=out_ap, in_=in_ap, allow_slow_non_contiguous=True)
        else:
            fn = lambda e: e.dma_start(out=out_ap, in_=in_ap)
        self.ops[q].append(Op(fn, waits, dma=k))
        for b in reads:
            b.r[key] = new
        for b in writes:
            b.w = {key: new}
            b.r = {}
        for b in wpart:
            b.w[key] = new

    def barrier(self):
        outstanding = {}
        for e in ENGS:
            if e != "sp" and self.lastc[e] > 0:
                outstanding[("c", e)] = self.lastc[e]
        for k in range(self.n_dma):
            if self.dma_val[k] > 0:
                outstanding[("d", k)] = self.dma_val[k]
        for e in ENGS:
            waits = []
            kn = self.known[e]
            for k, v in outstanding.items():
                if kn.get(k, 0) >= v:
                    continue
                kn[k] = v
                waits.append((k, v))
                if k[0] == "c":
                    self.sigset[k[1]].add(v)
            if waits:
                self.ops[e].append(Op(lambda eng: eng.nop(), waits))

    def emit(self):
        nc = self.nc
        rank = {}
        for e in ENGS:
            rank[e] = {idx: i + 1 for i, idx in enumerate(sorted(self.sigset[e]))}

        def run(e, engobj):
            rk = rank[e]
            for i, op in enumerate(self.ops[e], start=1):
                for (k, v) in op.waits:
                    if k[0] == "c":
                        engobj.wait_ge(self.csem[k[1]], rank[k[1]][v])
                    else:
                        engobj.wait_ge(self.dsem[k[1]], v)
                ins = op.fn(engobj)
                if op.dma is not None:
                    ins.then_inc(self.dsem[op.dma], 16)
                elif i in rk:
                    ins.then_inc(self.csem[e], 1)

        with nc.Block() as block:
            @block.tensor
            def _(pe):
                run("pe", pe)

            @block.scalar
            def _(act):
                run("act", act)

            @block.vector
            def _(dve):
                run("dve", dve)

            @block.gpsimd
            def _(pool):
                run("pool", pool)

            @block.sync
            def _(sp):
                run("sp", sp)


class Prog:
    def __init__(self, debug=None, stop_after=None):
        self.debug = debug or []
        self.stop_after = stop_after
        self.nc = bass.Bass("TRN2", target_bir_lowering=False)
        self.es = ExitStack()

    def din(self, name, shape, dt=F32):
        return self.nc.dram_tensor(name, list(shape), dt, kind="ExternalInput").ap()

    def dscr(self, name, shape, dt=BF16):
        kind = "ExternalOutput" if name in self.debug else "Internal"
        return self.nc.dram_tensor(name, list(shape), dt, kind=kind).ap()

    def sb(self, ps, name, shape, dt):
        self.uid = getattr(self, "uid", 0) + 1
        name = "%s_%d" % (name, self.uid)
        return Buf(ps.enter_context(self.nc.sbuf_tensor(name, list(shape), dt)), name)

    def pm(self, ps, name, shape, dt=F32):
        self.uid = getattr(self, "uid", 0) + 1
        name = "%s_%d" % (name, self.uid)
        return Buf(ps.enter_context(self.nc.psum_tensor(name, list(shape), dt)), name)

    def build(self):
        nc = self.nc
        es = self.es
        kb = self.kb = KB(nc, es)
        self.x = self.din("x", [T, D])
        self.cT = self.din("cT", [128, 16])
        self.w_ada = self.din("w_ada", [DEPTH, D, 6 * D])
        self.b_ada = self.din("b_ada", [DEPTH, 6 * D])
        self.g1T = self.din("g1T", [DEPTH, 128, 16])
        self.g2T = self.din("g2T", [DEPTH, 128, 16])
        self.w_in = self.din("w_in", [DEPTH, D, IN_W])
        self.b_mgate = self.din("b_mgate", [DEPTH, 8])
        self.g_mnorm = self.din("g_mnorm", [DEPTH, 1024])
        self.gqlT = self.din("gqlT", [DEPTH, 128, 4])
        self.w_uq = self.din("w_uq", [DEPTH, 512, 1536])
        self.gkvT = self.din("gkvT", [DEPTH, 128, 2])
        self.w_ukv = self.din("w_ukv", [DEPTH, 256, 2048])
        self.gqnT = self.din("gqnT", [DEPTH, 128, 2])
        self.gknT = self.din("gknT", [DEPTH, 128, 2])
        self.w_pool = self.din("w_pool", [DEPTH, 4, 256, 256])
        self.bpT = self.din("bpT", [DEPTH, 128, 8])
        self.spT = self.din("spT", [DEPTH, 128, 8])
        self.w_branch = self.din("w_branch", [DEPTH, 3, 1024, D])
        self.w_out = self.din("w_out", [DEPTH, D, D])
        self.w_ffn_in = self.din("w_ffn_in", [DEPTH, D, 2 * FFN])
        self.w_ffn_out = self.din("w_ffn_out", [DEPTH, FFN, D])
        self.cst = self.din("cst", [128, 128 * 6])
        self.ropec = self.din("ropec", [64, 2 * T + 64])
        self.poolinv = self.din("poolinv", [4, T])
        self.out = self.nc.dram_tensor("out", [T, D], F32, kind="ExternalOutput").ap()
        self.mod_d = self.dscr("mod_d", [DEPTH, 6 * D], F32)
        self.qmT = self.dscr("qmT", [512, T])
        self.kmT = self.dscr("kmT", [512, T])
        self.k_tm = self.dscr("k_tm", [T, 512])
        self.v_tm = self.dscr("v_tm", [T, 1024])
        self.so_tm = self.dscr("so_tm", [T, 1024])
        self.if_d = self.dscr("if_d", [T, 8], F32)
        self.qlatT = self.dscr("qlatT", [512, T])
        self.kvlatT = self.dscr("kvlatT", [256, T])
        self.krT = self.dscr("krT", [64, T])
        self.upT = self.dscr("upT", [1024, T])
        self.gatesT = self.dscr("gatesT", [6144, T])
        self.braT = self.dscr("braT", [1024, T])
        self.brbT = self.dscr("brbT", [1024, T])
        self.brcT = self.dscr("brcT", [1024, T])
        self.mergedT = self.dscr("mergedT", [D, T])
        self.xmid = self.dscr("xmid", [T, D], F32)
        self.x1 = self.dscr("x1", [T, D], F32)

        self.cf = self.sb(es, "cf", [128, 128 * 6], F32)
        self.cb = self.sb(es, "cb", [128, 128 * 6], BF16)
        kb.dma("sp", self.cf[:, :], self.cst[:, :], writes=[self.cf])
        kb.dma("pool", self.cb[:, :], self.cst[:, :], writes=[self.cb])
        self.stages = [
            ("mod", lambda: self.phase_mod()),
        ]
        xin = self.x
        for l in range(DEPTH):
            xo = self.out if l == DEPTH - 1 else self.x1
            self.stages += [
                ("A%d" % l, lambda l=l, xin=xin: self.phase_win(l, xin)),
                ("C%d" % l, lambda l=l: self.phase_mlstm(l)),
                ("D%d" % l, lambda l=l: self.phase_mla(l)),
                ("P%d" % l, lambda l=l: self.phase_pool(l)),
                ("E%d" % l, lambda l=l, xin=xin: self.phase_merge(l, xin)),
                ("F%d" % l, lambda l=l, xo=xo: self.phase_ffn(l, xo)),
            ]
            xin = xo
        for name, fn in self.stages:
            fn()
            kb.barrier()
            if self.stop_after == name:
                break
        kb.barrier()
        kb.emit()
        return nc

    def cF(self, i):
        return self.cf[:, i * 128:(i + 1) * 128]

    def cB(self, i):
        return self.cb[:, i * 128:(i + 1) * 128]

    def phase_mod(self):
        nc, kb = self.nc, self.kb
        with ExitStack() as ps:
            ct = self.sb(ps, "ct", [128, 16], F32)
            ca = self.sb(ps, "ca", [128, 16], F32)
            wt = [self.sb(ps, "wada%d" % i, [128, 16, 512], F32) for i in range(2)]
            pr = [self.pm(ps, "pmod%d" % i, [1, 512]) for i in range(2)]
            brow = self.sb(ps, "brow", [1, DEPTH * 6 * D], F32)
            orow = [self.sb(ps, "orow%d" % i, [1, 512], F32) for i in range(2)]
            kb.dma("sp", ct[:, :], self.cT[:, :], writes=[ct])
            kb.dma("sp", brow[:, :], self.b_ada.rearrange("l n -> (l n)").rearrange("(o n) -> o n", o=1), writes=[brow])
            kb.op("act", lambda e: e.activation(out=ca[:, :], in_=ct[:, :], func=AF.Silu), reads=[ct], writes=[ca])
            tiles = [(l, cbk) for l in range(DEPTH) for cbk in range(24)]

            def load(i):
                l, cbk = tiles[i]
                src = self.w_ada[l, :, cbk * 512:(cbk + 1) * 512].rearrange("(kc p) n -> p kc n", p=128)
                kb.dma("sp", wt[i % 2][:, :, :], src, writes=[wt[i % 2]])

            load(0)
            for i, (l, cbk) in enumerate(tiles):
                if i + 1 < len(tiles):
                    load(i + 1)
                w = wt[i % 2]
                p = pr[i % 2]
                o = orow[i % 2]
                for kc in range(16):
                    kb.op("pe", lambda e, w=w, p=p, kc=kc: e.matmul(p[0:1, :], lhsT=ca[:, kc:kc + 1], rhs=w[:, kc, :],
                                                                    start=(kc == 0), stop=(kc == 15)),
                          reads=[ca, w], writes=[p] if kc == 0 else [], wpart=[p] if kc else [])
                off = l * 6 * D + cbk * 512
                kb.op("dve", lambda e, p=p, o=o, off=off: e.tensor_tensor(out=o[0:1, :], in0=p[0:1, :], in1=brow[0:1, off:off + 512], op=ALU.add),
                      reads=[p, brow], writes=[o])
                kb.dma("sp", self.mod_d[l:l + 1, cbk * 512:(cbk + 1) * 512], o[0:1, :], reads=[o])

    def load_mod_cols(self, ps, l, which, gT, name):
        nc, kb = self.nc, self.kb
        sh = self.sb(ps, name + "sh", [128, 16], F32)
        sc = self.sb(ps, name + "sc", [128, 16], F32)
        g = self.sb(ps, name + "g", [128, 16], F32)
        gs = self.sb(ps, name + "gs", [128, 16], F32)
        base = 0 if which == 1 else 3 * D
        kb.dma("sp", sh[:, :], self.mod_d[l, base:base + D].rearrange("(j p) -> p j", p=128), writes=[sh], slow=True)
        kb.dma("sp", sc[:, :], self.mod_d[l, base + D:base + 2 * D].rearrange("(j p) -> p j", p=128), writes=[sc], slow=True)
        kb.dma("sp", g[:, :], gT[l], writes=[g])
        kb.op("dve", lambda e: e.tensor_scalar(out=sc[:, :], in0=sc[:, :], scalar1=1.0, scalar2=1.0, op0=ALU.add, op1=ALU.mult),
              reads=[sc], writes=[sc])
        kb.op("dve", lambda e: e.tensor_tensor(out=gs[:, :], in0=sc[:, :], in1=g[:, :], op=ALU.mult), reads=[sc, g], writes=[gs])
        return gs, sh

    def norm_to_hT(self, ps, xsrc, tb0, ntb, hT, gs, sh, tag):
        nc, kb = self.nc, self.kb
        xt = [self.sb(ps, tag + "xt%d" % i, [128, D], F32) for i in range(2)]
        junk = self.sb(ps, tag + "junk", [128, D], BF16)
        xn = [self.sb(ps, tag + "xn%d" % i, [128, D], BF16) for i in range(2)]
        ss = [self.sb(ps, tag + "ss%d" % i, [128, 1], F32) for i in range(2)]
        rs = [self.sb(ps, tag + "rs%d" % i, [128, 1], F32) for i in range(2)]
        pt = [self.pm(ps, tag + "pt%d" % i, [128, 1024], BF16) for i in range(4)]
        ident = self.cB(0)
        for j in range(ntb):
            tb = tb0 + j
            x_, xn_, ss_, rs_ = xt[j % 2], xn[j % 2], ss[j % 2], rs[j % 2]
            kb.dma("sp", x_[:, :], xsrc[tb * 128:(tb + 1) * 128, :], writes=[x_])
            kb.op("act", lambda e, x_=x_, ss_=ss_: e.activation(out=junk[:, :], in_=x_[:, :], func=AF.Square, accum_out=ss_[:, 0:1]),
                  reads=[x_], writes=[junk, ss_])
            kb.op("act", lambda e, ss_=ss_: e.activation(out=ss_[:, :], in_=ss_[:, :], func=AF.Sqrt, bias=EPS, scale=1.0 / D),
                  reads=[ss_], writes=[ss_])
            kb.op("dve", lambda e, ss_=ss_, rs_=rs_: e.reciprocal(out=rs_[:, :], in_=ss_[:, :]), reads=[ss_], writes=[rs_])
            kb.op("act", lambda e, x_=x_, xn_=xn_, rs_=rs_: e.activation(out=xn_[:, :], in_=x_[:, :], func=AF.Identity, scale=rs_[:, 0:1]),
                  reads=[x_, rs_], writes=[xn_])
            for half in range(2):
                p = pt[(j * 2 + half) % 4]
                for q in range(8):
                    dc = half * 8 + q
                    kb.op("pe", lambda e, p=p, q=q, dc=dc, xn_=xn_: e.transpose(p[:, q * 128:(q + 1) * 128], xn_[:, dc * 128:(dc + 1) * 128], ident),
                          reads=[xn_, self.cb], writes=[p] if q == 0 else [], wpart=[p] if q else [])
                for q in range(8):
                    dc = half * 8 + q
                    if half == 0:
                        kb.op("dve", lambda e, p=p, q=q, dc=dc, j=j: e.tensor_scalar(
                            out=hT[:, dc, j * 128:(j + 1) * 128], in0=p[:, q * 128:(q + 1) * 128],
                            scalar1=gs[:, dc:dc + 1], scalar2=sh[:, dc:dc + 1], op0=ALU.mult, op1=ALU.add),
                            reads=[p, gs, sh], wpart=[hT])
                    else:
                        kb.op("act", lambda e, p=p, q=q, dc=dc, j=j: e.activation(
                            out=hT[:, dc, j * 128:(j + 1) * 128], in_=p[:, q * 128:(q + 1) * 128],
                            func=AF.Identity, bias=sh[:, dc:dc + 1], scale=gs[:, dc:dc + 1]),
                            reads=[p, gs, sh], wpart=[hT.alt])

    class WStream:
        def __init__(self, prog, ps, name, kc, ncols, nbuf=3):
            self.prog = prog
            self.bufs = [prog.sb(ps, "%s%d" % (name, i), [128, kc, ncols], BF16) for i in range(nbuf)]
            self.i = 0
            self.kc = kc

        def load(self, src2d, n):
            b = self.bufs[self.i % len(self.bufs)]
            self.i += 1
            self.prog.kb.dma("pool", b[:, :, 0:n], src2d.rearrange("(kc p) n -> p kc n", p=128), writes=[b])
            return b

    def phase_win(self, l, xin):
        nc, kb = self.nc, self.kb
        with ExitStack() as ps:
            hT = self.sb(ps, "hT", [128, 16, T], BF16)
            hT.alt = hT
            with ExitStack() as ps2:
                gs, sh = self.load_mod_cols(ps2, l, 1, self.g1T, "m1")
                self.norm_to_hT(ps2, xin, 0, NTB, hT, gs, sh, "n1")
            kb.barrier()
            ws = self.WStream(self, ps, "wi", 16, 512, 3)
            pacc = [self.pm(ps, "pacc%d" % i, [128, 512]) for i in range(8)]
            ot = [self.sb(ps, "ot%d" % i, [128, T], BF16) for i in range(3)]
            otm = [self.sb(ps, "otm%d" % i, [128, 512], BF16) for i in range(3)]
            ifs = self.sb(ps, "ifs", [128, NTB, 8], F32)
            W = self.w_in[l]
            state = {"pset": 0, "ot": 0, "pb": 0, "otm": 0, "ev": 0}

            def fm_tile(c0, n, dst, func, scale):
                wt = ws.load(W[:, c0:c0 + n], n)
                for f0 in range(0, n, 128):
                    m = min(128, n - f0)
                    pset = state["pset"]
                    state["pset"] ^= 1
                    banks = pacc[pset * 4:(pset + 1) * 4]
                    for k in range(16):
                        for tt in range(4):
                            p = banks[tt]
                            kb.op("pe", lambda e, p=p, k=k, tt=tt, f0=f0, m=m, wt=wt: e.matmul(
                                p[0:m, :], lhsT=wt[:, k, f0:f0 + m], rhs=hT[:, k, tt * 512:(tt + 1) * 512],
                                start=(k == 0), stop=(k == 15)),
                                reads=[wt, hT, hT.alt], writes=[p] if k == 0 else [], wpart=[p] if k else [])
                    o = ot[state["ot"] % 3]
                    state["ot"] += 1
                    for tt in range(4):
                        p = banks[tt]
                        eng = "act" if (func is not None or tt % 2 == 1) else "dve"
                        first = (tt == 0)
                        if eng == "act":
                            kb.op("act", lambda e, p=p, o=o, tt=tt, m=m: e.activation(
                                out=o[0:m, tt * 512:(tt + 1) * 512], in_=p[0:m, :], func=(func or AF.Identity), scale=scale),
                                reads=[p], writes=[o] if first else [], wpart=[] if first else [o])
                        else:
                            kb.op("dve", lambda e, p=p, o=o, tt=tt, m=m: e.tensor_scalar(
                                out=o[0:m, tt * 512:(tt + 1) * 512], in0=p[0:m, :], scalar1=scale, scalar2=None, op0=ALU.mult),
                                reads=[p], writes=[o] if first else [], wpart=[] if first else [o])
                    r0 = c0 - dst[1] + f0
                    kb.dma("sp", dst[0][r0:r0 + m, :], o[0:m, :], reads=[o])

            def tm_tile(c0, n, dst, func, scale):
                wt = ws.load(W[:, c0:c0 + n], n)
                for tb in range(NTB):
                    p = pacc[state["pb"] % 8]
                    state["pb"] += 1
                    for k in range(16):
                        kb.op("pe", lambda e, p=p, k=k, tb=tb, wt=wt: e.matmul(
                            p[:, 0:n], lhsT=hT[:, k, tb * 128:(tb + 1) * 128], rhs=wt[:, k, 0:n],
                            start=(k == 0), stop=(k == 15)),
                            reads=[wt, hT, hT.alt], writes=[p] if k == 0 else [], wpart=[p] if k else [])
                    if dst is None:
                        kb.op("dve", lambda e, p=p, tb=tb: e.tensor_copy(out=ifs[:, tb, :], in_=p[:, 0:8]), reads=[p], wpart=[ifs])
                        continue
                    o = otm[state["otm"] % 3]
                    state["otm"] += 1
                    state["ev"] += 1
                    if func is not None or state["ev"] % 2:
                        kb.op("act", lambda e, p=p, o=o: e.activation(out=o[:, 0:n], in_=p[:, 0:n], func=(func or AF.Identity), scale=scale),
                              reads=[p], writes=[o])
                    else:
                        kb.op("dve", lambda e, p=p, o=o: e.tensor_scalar(out=o[:, 0:n], in0=p[:, 0:n], scalar1=scale, scalar2=None, op0=ALU.mult),
                              reads=[p], writes=[o])
                    cc = c0 - dst[1]
                    kb.dma("sp", dst[0][tb * 128:(tb + 1) * 128, cc:cc + n], o[:, 0:n], reads=[o])

            ksc = float(128 ** -0.5)
            tm_tile(C_IF, 8, None, None, 1.0)
            tm_tile(C_KM, 512, (self.k_tm, C_KM), None, ksc)
            for c0 in (C_VM, C_VM + 512):
                tm_tile(c0, 512, (self.v_tm, C_VM), None, 1.0)
            for c0 in (C_OM, C_OM + 512):
                tm_tile(c0, 512, (self.so_tm, C_OM), AF.Sigmoid, 1.0)
            kb.dma("sp", self.if_d.rearrange("(c p) e -> p c e", p=128), ifs[:, :, :], reads=[ifs])
            fm_tile(C_QM, 512, (self.qmT, C_QM), None, 1.0)
            fm_tile(C_KM, 512, (self.kmT, C_KM), None, ksc)
            fm_tile(C_QL, 512, (self.qlatT, C_QL), None, 1.0)
            fm_tile(C_KV, 256, (self.kvlatT, C_KV), None, 1.0)
            fm_tile(C_KR, 64, (self.krT, C_KR), None, 1.0)
            for c0 in (C_UP, C_UP + 512):
                fm_tile(c0, 512, (self.upT, C_UP), None, 1.0)
            for c0 in range(C_GT, IN_W, 512):
                fm_tile(c0, 512, (self.gatesT, C_GT), AF.Sigmoid, 1.0)

    def phase_mlstm(self, l):
        nc, kb = self.nc, self.kb
        with ExitStack() as ps:
            cf, cbf = self.cf, self.cb
            identF, U, negT, neg, onesF = self.cF(0), self.cF(1), self.cF(2), self.cF(3), self.cF(5)
            identB = self.cB(0)
            qT = self.sb(ps, "mqT", [128, 4, T], BF16)
            kT = self.sb(ps, "mkT", [128, 4, T], BF16)
            ktm = self.sb(ps, "mktm", [128, NTB, 512], BF16)
            vaug = self.sb(ps, "mvaug", [128, NTB, 4, 257], BF16)
            ifs = self.sb(ps, "mifs", [128, NTB, 8], F32)
            bmg = self.sb(ps, "mbmg", [128, 8], F32)
            gmn = self.sb(ps, "mgmn", [128, 1024], F32)
            kb.dma("sp", qT[:, :, :], self.qmT.rearrange("(h d) t -> d h t", d=128), writes=[qT])
            kb.dma("sp", kT[:, :, :], self.kmT.rearrange("(h d) t -> d h t", d=128), writes=[kT])
            kb.dma("sp", ktm[:, :, :], self.k_tm.rearrange("(c p) e -> p c e", p=128), writes=[ktm])
            kb.op("pool", lambda e: e.memset(vaug[:, :, :, :], 1.0), writes=[vaug])
            for c in range(NTB):
                kb.dma("sp", vaug[:, c, :, 0:256], self.v_tm[c * 128:(c + 1) * 128, :].rearrange("p (h v) -> p h v", h=4), reads=[], wpart=[vaug])
            kb.dma("sp", ifs[:, :, :], self.if_d.rearrange("(c p) e -> p c e", p=128), writes=[ifs])
            kb.dma("sp", bmg[:, :], self.b_mgate[l].partition_broadcast(128), writes=[bmg])
            kb.dma("sp", gmn[:, :], self.g_mnorm[l].partition_broadcast(128), writes=[gmn])
            NI = NTB * 4
            mk = lambda n, w=NI: self.sb(ps, n, [128, w], F32)
            ipre, fpre, logf, bcs, bL, g, cm, gmax = [mk("m" + n) for n in ("ipre", "fpre", "logf", "bcs", "bL", "g", "cm", "gmax")]
            mprev = mk("mprev", NI + 4)
            mxL, decay, wsc, mx, nmx, em = [mk("m" + n) for n in ("mxL", "decay", "wsc", "mx", "nmx", "em")]
            pP = self.pm(ps, "mpP", [128, 512])
            pT = self.pm(ps, "mpT", [128, 1024], BF16)
            pX = [self.pm(ps, "mpX%d" % i, [128, 384]) for i in range(2)]
            pN = [self.pm(ps, "mpN%d" % i, [128, 257]) for i in range(2)]
            pC = [self.pm(ps, "mpC%d" % i, [128, 257]) for i in range(2)]
            for c in range(NTB):
                kb.op("dve", lambda e, c=c: e.tensor_tensor(out=ipre[:, c * 4:(c + 1) * 4], in0=ifs[:, c, 0:4], in1=bmg[:, 0:4], op=ALU.add),
                      reads=[ifs, bmg], wpart=[ipre])
                kb.op("dve", lambda e, c=c: e.tensor_tensor(out=fpre[:, c * 4:(c + 1) * 4], in0=ifs[:, c, 4:8], in1=bmg[:, 4:8], op=ALU.add),
                      reads=[ifs, bmg], wpart=[fpre])
            kb.op("act", lambda e: e.activation(out=logf[:, :], in_=fpre[:, :], func=AF.Exp, scale=-1.0), reads=[fpre], writes=[logf])
            kb.op("act", lambda e: e.activation(out=logf[:, :], in_=logf[:, :], func=AF.Ln, bias=1.0, scale=1.0), reads=[logf], writes=[logf])
            kb.op("dve", lambda e: e.tensor_scalar(out=logf[:, :], in0=logf[:, :], scalar1=-1.0, scalar2=None, op0=ALU.mult), reads=[logf], writes=[logf])
            kb.op("pe", lambda e: e.matmul(pP[:, 0:NI], lhsT=U, rhs=logf[:, :], start=True, stop=True), reads=[logf, cf], writes=[pP])
            kb.op("dve", lambda e: e.tensor_copy(out=bcs[:, :], in_=pP[:, 0:NI]), reads=[pP], writes=[bcs])
            kb.op("pe", lambda e: e.matmul(pP[:, 0:NI], lhsT=onesF, rhs=logf[:, :], start=True, stop=True), reads=[logf, cf], writes=[pP])
            kb.op("dve", lambda e: e.tensor_copy(out=bL[:, :], in_=pP[:, 0:NI]), reads=[pP], writes=[bL])
            kb.op("dve", lambda e: e.tensor_tensor(out=g[:, :], in0=ipre[:, :], in1=bcs[:, :], op=ALU.subtract), reads=[ipre, bcs], writes=[g])
            gb = [self.sb(ps, "mgb%d" % i, [128, 128], F32) for i in range(2)]
            tmpm = [self.sb(ps, "mtmpm%d" % i, [128, 128], F32) for i in range(2)]
            for idx in range(NI):
                gb_ = gb[idx % 2]
                tm_ = tmpm[idx % 2]
                pg = pX[idx % 2]
                kb.op("pool", lambda e, gb_=gb_, idx=idx: e.tensor_copy(out=gb_[:, :], in_=g[:, idx:idx + 1].to_broadcast([128, 128])), reads=[g], writes=[gb_])
                kb.op("pe", lambda e, gb_=gb_, pg=pg: e.matmul(pg[:, 0:128], lhsT=gb_[:, :], rhs=identF, start=True, stop=True), reads=[gb_, cf], writes=[pg])
                kb.op("dve", lambda e, pg=pg, tm_=tm_: e.tensor_tensor(out=tm_[:, :], in0=pg[:, 0:128], in1=neg, op=ALU.add), reads=[pg, cf], writes=[tm_])
                kb.op("dve", lambda e, tm_=tm_, idx=idx: e.reduce_max(out=cm[:, idx:idx + 1], in_=tm_[:, :], axis=AX.X), reads=[tm_], wpart=[cm])
                kb.op("dve", lambda e, pg=pg, idx=idx: e.reduce_max(out=gmax[:, idx:idx + 1], in_=pg[:, 0:128], axis=AX.X), reads=[pg], wpart=[gmax])
            kb.op("dve", lambda e: e.memset(mprev[:, 0:4], 0.0), writes=[mprev])
            for c in range(NTB):
                sl = slice(c * 4, (c + 1) * 4)
                sl2 = slice((c + 1) * 4, (c + 2) * 4)
                kb.op("dve", lambda e, sl=sl: e.tensor_tensor(out=mxL[:, sl], in0=mprev[:, sl], in1=gmax[:, sl], op=ALU.max), reads=[mprev, gmax], writes=[mxL])
                kb.op("dve", lambda e, sl=sl, sl2=sl2: e.tensor_tensor(out=mprev[:, sl2], in0=bL[:, sl], in1=mxL[:, sl], op=ALU.add), reads=[bL, mxL], writes=[mprev])
            mp = mprev
            kb.op("dve", lambda e: e.tensor_tensor(out=decay[:, :], in0=mp[:, 0:NI], in1=mxL[:, :], op=ALU.subtract), reads=[mp, mxL], writes=[decay])
            kb.op("act", lambda e: e.activation(out=decay[:, :], in_=decay[:, :], func=AF.Exp), reads=[decay], writes=[decay])
            kb.op("dve", lambda e: e.tensor_tensor(out=wsc[:, :], in0=g[:, :], in1=mxL[:, :], op=ALU.subtract), reads=[g, mxL], writes=[wsc])
            kb.op("act", lambda e: e.activation(out=wsc[:, :], in_=wsc[:, :], func=AF.Exp), reads=[wsc], writes=[wsc])
            kb.op("dve", lambda e: e.tensor_tensor(out=mx[:, :], in0=mp[:, 0:NI], in1=cm[:, :], op=ALU.max), reads=[mp, cm], writes=[mx])
            kb.op("dve", lambda e: e.tensor_scalar(out=nmx[:, :], in0=mx[:, :], scalar1=-1.0, scalar2=None, op0=ALU.mult), reads=[mx], writes=[nmx])
            kb.op("dve", lambda e: e.tensor_tensor(out=em[:, :], in0=bcs[:, :], in1=mx[:, :], op=ALU.add), reads=[bcs, mx], writes=[em])
            kb.op("act", lambda e: e.activation(out=em[:, :], in_=em[:, :], func=AF.Exp, scale=-1.0), reads=[em], writes=[em])
            CT = [self.sb(ps, "mCT%d" % h, [128, 257], F32) for h in range(4)]
            CTb = [self.sb(ps, "mCTb%d" % h, [128, 257], BF16) for h in range(4)]
            for h in range(4):
                kb.op("pool", lambda e, h=h: e.memset(CT[h][:, :], 0.0), writes=[CT[h]])
                kb.op("pool", lambda e, h=h: e.memset(CTb[h][:, :], 0.0), writes=[CTb[h]])
            nmxb = [self.sb(ps, "mnmxb%d" % i, [128, 128], F32) for i in range(2)]
            Dt = [self.sb(ps, "mDt%d" % i, [128, 128], F32) for i in range(2)]
            Wb = [self.sb(ps, "mWb%d" % i, [128, 128], F32) for i in range(2)]
            SD = [self.sb(ps, "mSD%d" % i, [128, 128], BF16) for i in range(2)]
            qw = [self.sb(ps, "mqw%d" % i, [128, 128], BF16) for i in range(2)]
            dn = [self.sb(ps, "mdn%d" % i, [128, 1], F32) for i in range(2)]
            hm = [self.sb(ps, "mhm%d" % i, [128, 256], F32) for i in range(2)]
            junk = self.sb(ps, "mjunk", [128, 256], F32)
            ss2 = [self.sb(ps, "mss2%d" % i, [128, 1], F32) for i in range(2)]
            t1 = [self.sb(ps, "mt1%d" % i, [128, 256], F32) for i in range(2)]
            vw = [self.sb(ps, "mvw%d" % i, [128, 257], BF16) for i in range(2)]
            soc = [self.sb(ps, "msoc%d" % i, [128, 1024], BF16) for i in range(2)]
            brat = [self.sb(ps, "mbrat%d" % i, [128, 1024], BF16) for i in range(2)]
            braTs = [self.sb(ps, "mbraTs%d" % i, [128, 8, 128], BF16) for i in range(2)]
            for c in range(NTB):
                cs = slice(c * 128, (c + 1) * 128)
                so_ = soc[c % 2]
                bt_ = brat[c % 2]
                kb.dma("sp", so_[:, :], self.so_tm[cs, :], writes=[so_])
                def mstage(h, st):
                    idx = c * 4 + h
                    i2 = idx % 2
                    X, N_, C_ = pX[i2], pN[i2], pC[i2]
                    nb_, D_, W_, S_, q_, dn_, hm_, ss_, t1_, vw_ = nmxb[i2], Dt[i2], Wb[i2], SD[i2], qw[i2], dn[i2], hm[i2], ss2[i2], t1[i2], vw[i2]
                    if st == 0:
                        kb.op("pool", lambda e, nb_=nb_, idx=idx: e.tensor_copy(out=nb_[:, :], in_=nmx[:, idx:idx + 1].to_broadcast([128, 128])), reads=[nmx], writes=[nb_])
                        kb.op("pe", lambda e, X=X, nb_=nb_: e.matmul(X[:, 0:128], lhsT=nb_[:, :], rhs=identF, start=True, stop=False), reads=[nb_, cf], writes=[X])
                        kb.op("pe", lambda e, X=X: e.matmul(X[:, 0:128], lhsT=identF, rhs=negT, start=False, stop=True), reads=[cf], wpart=[X])
                        kb.op("pe", lambda e, X=X, nb_=nb_: e.matmul(X[:, 128:256], lhsT=nb_[:, :], rhs=identF, start=True, stop=True), reads=[nb_, cf], wpart=[X])
                        kb.op("pe", lambda e, X=X, h=h, cs=cs: e.matmul(X[:, 256:384], lhsT=kT[:, h, cs], rhs=qT[:, h, cs], start=True, stop=True), reads=[kT, qT], wpart=[X])
                    if st == 1:
                        kb.op("act", lambda e, X=X, D_=D_, idx=idx: e.activation(out=D_[:, :], in_=X[:, 0:128], func=AF.Exp, bias=g[:, idx:idx + 1], scale=1.0),
                              reads=[X, g], writes=[D_])
                        kb.op("act", lambda e, X=X, W_=W_, idx=idx: e.activation(out=W_[:, :], in_=X[:, 128:256], func=AF.Exp, bias=mp[:, idx:idx + 1], scale=1.0),
                              reads=[X, mp], writes=[W_])
                    if st == 2:
                        kb.op("dve", lambda e, X=X, D_=D_, S_=S_: e.tensor_tensor(out=S_[:, :], in0=X[:, 256:384], in1=D_[:, :], op=ALU.mult), reads=[X, D_], writes=[S_])
                        kb.op("dve", lambda e, W_=W_, q_=q_, h=h, cs=cs: e.tensor_tensor(out=q_[:, :], in0=qT[:, h, cs], in1=W_[:, :], op=ALU.mult), reads=[qT, W_], writes=[q_])
                    if st == 3:
                        kb.op("pe", lambda e, N_=N_, q_=q_, h=h: e.matmul(N_[:, :], lhsT=q_[:, :], rhs=CTb[h][:, :], start=True, stop=False), reads=[q_, CTb[h]], writes=[N_])
                        kb.op("pe", lambda e, N_=N_, S_=S_, c=c, h=h: e.matmul(N_[:, :], lhsT=S_[:, :], rhs=vaug[:, c, h, :], start=False, stop=True), reads=[S_, vaug], wpart=[N_])
                        kb.op("pool", lambda e, vw_=vw_, c=c, h=h, idx=idx: e.tensor_scalar(out=vw_[:, :], in0=vaug[:, c, h, :], scalar1=wsc[:, idx:idx + 1], scalar2=None, op0=ALU.mult),
                              reads=[vaug, wsc], writes=[vw_])
                        kb.op("pe", lambda e, C_=C_, vw_=vw_, c=c, h=h: e.matmul(C_[:, :], lhsT=ktm[:, c, h * 128:(h + 1) * 128], rhs=vw_[:, :], start=True, stop=True),
                              reads=[ktm, vw_], writes=[C_])
                    if st == 4:
                        kb.op("dve", lambda e, C_=C_, h=h, idx=idx: e.scalar_tensor_tensor(out=CT[h][:, :], in0=CT[h][:, :], scalar=decay[:, idx:idx + 1], in1=C_[:, :],
                                                                                          op0=ALU.mult, op1=ALU.add), reads=[CT[h], decay, C_], writes=[CT[h]])
                        kb.op("act", lambda e, h=h: e.activation(out=CTb[h][:, :], in_=CT[h][:, :], func=AF.Identity), reads=[CT[h]], writes=[CTb[h]])

                    if st == 5:
                        kb.op("act", lambda e, N_=N_, dn_=dn_: e.activation(out=dn_[:, :], in_=N_[:, 256:257], func=AF.Abs), reads=[N_], writes=[dn_])
                        kb.op("dve", lambda e, dn_=dn_, idx=idx: e.tensor_tensor(out=dn_[:, :], in0=dn_[:, :], in1=em[:, idx:idx + 1], op=ALU.max), reads=[dn_, em], writes=[dn_])
                        kb.op("dve", lambda e, dn_=dn_: e.reciprocal(out=dn_[:, :], in_=dn_[:, :]), reads=[dn_], writes=[dn_])
                    if st == 6:
                        kb.op("act", lambda e, N_=N_, hm_=hm_, dn_=dn_: e.activation(out=hm_[:, :], in_=N_[:, 0:256], func=AF.Identity, scale=dn_[:, 0:1]), reads=[N_, dn_], writes=[hm_])
                        kb.op("act", lambda e, hm_=hm_, ss_=ss_: e.activation(out=junk[:, :], in_=hm_[:, :], func=AF.Square, accum_out=ss_[:, 0:1]), reads=[hm_], writes=[junk, ss_])
                        kb.op("act", lambda e, ss_=ss_: e.activation(out=ss_[:, :], in_=ss_[:, :], func=AF.Sqrt, bias=EPS, scale=1.0 / 256), reads=[ss_], writes=[ss_])
                        kb.op("dve", lambda e, ss_=ss_: e.reciprocal(out=ss_[:, :], in_=ss_[:, :]), reads=[ss_], writes=[ss_])
                        kb.op("dve", lambda e, hm_=hm_, ss_=ss_, t1_=t1_, h=h: e.scalar_tensor_tensor(out=t1_[:, :], in0=hm_[:, :], scalar=ss_[:, 0:1], in1=gmn[:, h * 256:(h + 1) * 256],
                                                                                                     op0=ALU.mult, op1=ALU.mult), reads=[hm_, ss_, gmn], writes=[t1_])
                        kb.op("pool", lambda e, t1_=t1_, h=h, so_=so_, bt_=bt_: e.tensor_tensor(out=bt_[:, h * 256:(h + 1) * 256], in0=t1_[:, :], in1=so_[:, h * 256:(h + 1) * 256], op=ALU.mult),
                              reads=[t1_, so_], writes=[bt_] if h == 0 else [], wpart=[bt_] if h else [])

                for pair in ((0, 1), (2, 3)):
                    for st in range(7):
                        for h in pair:
                            mstage(h, st)
                bs_ = braTs[c % 2]
                for q in range(8):
                    kb.op("pe", lambda e, q=q, bt_=bt_: e.transpose(pT[:, q * 128:(q + 1) * 128], bt_[:, q * 128:(q + 1) * 128], identB),
                          reads=[bt_, cbf], writes=[pT] if q == 0 else [], wpart=[pT] if q else [])
                kb.op("act", lambda e, bs_=bs_: e.activation(out=bs_[:, :, :], in_=pT[:, :].rearrange("p (q t) -> p q t", q=8), func=AF.Identity), reads=[pT], writes=[bs_])
                kb.dma("sp", self.braT[:, cs].rearrange("(q p) t -> p q t", p=128), bs_[:, :, :], reads=[bs_])

    def phase_mla(self, l):
        nc, kb = self.nc, self.kb
        with ExitStack() as ps:
            ones_b, tri = self.cB(5), self.cB(4)
            cbuf, cfb = self.cb, self.cf
            ql = self.sb(ps, "ql", [128, 4, T], BF16)
            kvl = self.sb(ps, "kvl", [128, 2, T], BF16)
            kr = self.sb(ps, "kr", [64, T], BF16)
            rc = self.sb(ps, "rc", [64, 2 * T + 64], F32)
            gql = self.sb(ps, "gql", [128, 4], F32)
            gkv = self.sb(ps, "gkv", [128, 2], F32)
            gqn = self.sb(ps, "gqn", [128, 2], F32)
            gkn = self.sb(ps, "gkn", [128, 2], F32)
            wuq = self.sb(ps, "wuq", [128, 4, 1536], BF16)
            wukv = self.sb(ps, "wukv", [128, 2, 2048], BF16)
            kb.dma("sp", ql[:, :, :], self.qlatT.rearrange("(kc p) t -> p kc t", p=128), writes=[ql])
            kb.dma("sp", kvl[:, :, :], self.kvlatT.rearrange("(kc p) t -> p kc t", p=128), writes=[kvl])
            kb.dma("sp", kr[:, :], self.krT[:, :], writes=[kr])
            kb.dma("sp", rc[:, :], self.ropec[:, :], writes=[rc])
            kb.dma("sp", gql[:, :], self.gqlT[l], writes=[gql])
            kb.dma("sp", gkv[:, :], self.gkvT[l], writes=[gkv])
            kb.dma("sp", gqn[:, :], self.gqnT[l], writes=[gqn])
            kb.dma("sp", gkn[:, :], self.gknT[l], writes=[gkn])
            kb.dma("pool", wuq[:, :, :], self.w_uq[l].rearrange("(kc p) n -> p kc n", p=128), writes=[wuq])
            kb.dma("pool", wukv[:, :, :], self.w_ukv[l].rearrange("(kc p) n -> p kc n", p=128), writes=[wukv])
            kb.op("dve", lambda e: e.tensor_scalar(out=gqn[:, :], in0=gqn[:, :], scalar1=float(192 ** -0.5), scalar2=None, op0=ALU.mult),
                  reads=[gqn], writes=[gqn])
            cosT = lambda a, b: rc[:, a:b]
            sinT = lambda a, b: rc[:, T + a:T + b]
            Pm = rc[:, 2 * T:2 * T + 64]
            PA, PB, PC, PD, S0, S1, PO, PR = [self.pm(ps, "pa%d" % i, [128, 512]) for i in range(8)]
            sq = [self.sb(ps, "sq%d" % i, [128, 512], BF16) for i in range(4)]
            rsd = [self.sb(ps, "rsd%d" % i, [128, 512], F32) for i in range(2)]
            cnt = {"sq": 0, "rs": 0}

            def rstd_from(pss, nfeat):
                r = rsd[cnt["rs"] % 2]
                cnt["rs"] += 1
                kb.op("act", lambda e: e.activation(out=r[:, :], in_=pss[:, :], func=AF.Sqrt, bias=EPS, scale=1.0 / nfeat), reads=[pss], writes=[r])
                kb.op("dve", lambda e: e.reciprocal(out=r[:, :], in_=r[:, :]), reads=[r], writes=[r])
                return r

            def fm_rmsnorm(src, KC, nfeat, gcol, dst):
                for tt in range(4):
                    sl = slice(tt * 512, (tt + 1) * 512)
                    for kc in range(KC):
                        q_ = sq[cnt["sq"] % 4]
                        cnt["sq"] += 1
                        kb.op("dve", lambda e, q_=q_, kc=kc: e.tensor_tensor(out=q_[:, :], in0=src[:, kc, sl], in1=src[:, kc, sl], op=ALU.mult),
                              reads=[src], writes=[q_])
                        kb.op("pe", lambda e, q_=q_, kc=kc: e.matmul(PC[:, :], lhsT=ones_b, rhs=q_[:, :], start=(kc == 0), stop=(kc == KC - 1)),
                              reads=[q_, cbuf], writes=[PC] if kc == 0 else [], wpart=[PC] if kc else [])
                    r = rstd_from(PC, nfeat)
                    for kc in range(KC):
                        kb.op("dve", lambda e, kc=kc, r=r: e.scalar_tensor_tensor(out=dst[:, kc, sl], in0=src[:, kc, sl], scalar=gcol[:, kc:kc + 1],
                                                                                in1=r[:, :], op0=ALU.mult, op1=ALU.mult),
                              reads=[src, gcol, r], wpart=[dst])

            qln = self.sb(ps, "qln", [128, 4, T], BF16)
            kvn = self.sb(ps, "kvn", [128, 2, T], BF16)
            fm_rmsnorm(ql, 4, 512, gql, qln)
            fm_rmsnorm(kvl, 2, 256, gkv, kvn)
            krr = self.sb(ps, "krr", [64, T], F32)
            sqkr = self.sb(ps, "sqkr", [64, T], BF16)
            krg = self.sb(ps, "krg", [64, 512], F32)
            ta = self.sb(ps, "ta", [64, 512], F32)
            tb_ = self.sb(ps, "tb_", [64, 512], F32)
            kb.op("dve", lambda e: e.tensor_tensor(out=sqkr[:, :], in0=kr[:, :], in1=kr[:, :], op=ALU.mult), reads=[kr], writes=[sqkr])

            def rope_apply(xf, sl0, sl1, out_ap, out_buf):
                kb.op("pe", lambda e: e.matmul(PD[0:64, :], lhsT=Pm, rhs=xf[:, :], start=True, stop=True), reads=[xf, rc], writes=[PD])
                kb.op("dve", lambda e: e.tensor_tensor(out=ta[:, :], in0=xf[:, :], in1=cosT(sl0, sl1), op=ALU.mult), reads=[xf, rc], writes=[ta])
                kb.op("dve", lambda e: e.tensor_tensor(out=tb_[:, :], in0=PD[0:64, :], in1=sinT(sl0, sl1), op=ALU.mult), reads=[PD, rc], writes=[tb_])
                kb.op("pool", lambda e: e.tensor_tensor(out=out_ap, in0=ta[:, :], in1=tb_[:, :], op=ALU.add), reads=[ta, tb_], wpart=[out_buf])

            for tt in range(4):
                a, b = tt * 512, (tt + 1) * 512
                kb.op("dve", lambda e, a=a, b=b: e.tensor_scalar(out=krg[:, :], in0=kr[:, a:b], scalar1=gkn[0:64, 1:2], scalar2=None, op0=ALU.mult),
                      reads=[kr, gkn], writes=[krg])
                rope_apply(krg, a, b, krr[:, a:b], krr)

            QnT = [self.sb(ps, "QnT%d" % i, [128, T], BF16) for i in range(2)]
            QrT = [self.sb(ps, "QrT%d" % i, [64, T], BF16) for i in range(2)]
            KnT = [self.sb(ps, "KnT%d" % i, [128, T], BF16) for i in range(2)]
            KrT = [self.sb(ps, "KrT%d" % i, [64, T], BF16) for i in range(2)]
            Vh = [self.sb(ps, "Vh%d" % i, [128, NTB, 128], BF16) for i in range(2)]
            qrf = self.sb(ps, "qrf", [64, 512], F32)
            pt = [self.sb(ps, "pt%d" % i, [128, 512], BF16) for i in range(3)]
            rr = self.sb(ps, "rr", [128, 512], F32)
            ob = [self.sb(ps, "ob%d" % i, [128, 512], BF16) for i in range(2)]
            npt = 0
            nob = 0
            for h in range(8):
                Qn, Qr, Kn, Kr, V = QnT[h % 2], QrT[h % 2], KnT[h % 2], KrT[h % 2], Vh[h % 2]
                for tt in range(4):
                    a, b = tt * 512, (tt + 1) * 512
                    s1, s2, s3 = sq[cnt["sq"] % 4], sq[(cnt["sq"] + 1) % 4], sq[(cnt["sq"] + 2) % 4]
                    cnt["sq"] += 3
                    rq, rk = rsd[0], rsd[1]
                    ones64 = self.cb[0:64, 640:768]
                    for kc in range(4):
                        kb.op("pe", lambda e, kc=kc: e.matmul(PA[:, :], lhsT=wuq[:, kc, h * 192:h * 192 + 128], rhs=qln[:, kc, a:b],
                                                             start=(kc == 0), stop=(kc == 3)), reads=[wuq, qln], writes=[PA] if kc == 0 else [], wpart=[PA] if kc else [])
                    for kc in range(4):
                        kb.op("pe", lambda e, kc=kc: e.matmul(PB[0:64, :], lhsT=wuq[:, kc, h * 192 + 128:h * 192 + 192], rhs=qln[:, kc, a:b],
                                                             start=(kc == 0), stop=(kc == 3)), reads=[wuq, qln], writes=[PB] if kc == 0 else [], wpart=[PB] if kc else [])
                    for kc in range(2):
                        kb.op("pe", lambda e, kc=kc: e.matmul(S0[:, :], lhsT=wukv[:, kc, h * 256:h * 256 + 128], rhs=kvn[:, kc, a:b],
                                                             start=(kc == 0), stop=(kc == 1)), reads=[wukv, kvn], writes=[S0] if kc == 0 else [], wpart=[S0] if kc else [])
                    kb.op("act", lambda e: e.activation(out=s1[:, :], in_=PA[:, :], func=AF.Square), reads=[PA], writes=[s1])
                    kb.op("act", lambda e: e.activation(out=s2[0:64, :], in_=PB[0:64, :], func=AF.Square), reads=[PB], writes=[s2])
                    kb.op("act", lambda e: e.activation(out=s3[:, :], in_=S0[:, :], func=AF.Square), reads=[S0], writes=[s3])
                    kb.op("pe", lambda e: e.matmul(PC[:, :], lhsT=ones_b, rhs=s1[:, :], start=True, stop=False), reads=[s1, cbuf], writes=[PC])
                    kb.op("pe", lambda e: e.matmul(PC[:, :], lhsT=ones64, rhs=s2[0:64, :], start=False, stop=True), reads=[s2, cbuf], wpart=[PC])
                    kb.op("pe", lambda e: e.matmul(S1[:, :], lhsT=ones_b, rhs=s3[:, :], start=True, stop=False), reads=[s3, cbuf], writes=[S1])
                    kb.op("pe", lambda e: e.matmul(S1[:, :], lhsT=ones64, rhs=sqkr[:, a:b], start=False, stop=True), reads=[sqkr, cbuf], wpart=[S1])
                    kb.op("act", lambda e: e.activation(out=rq[:, :], in_=PC[:, :], func=AF.Sqrt, bias=EPS, scale=1.0 / 192), reads=[PC], writes=[rq])
                    kb.op("act", lambda e: e.activation(out=rk[:, :], in_=S1[:, :], func=AF.Sqrt, bias=EPS, scale=1.0 / 192), reads=[S1], writes=[rk])
                    kb.op("dve", lambda e: e.reciprocal(out=rq[:, :], in_=rq[:, :]), reads=[rq], writes=[rq])
                    kb.op("dve", lambda e: e.reciprocal(out=rk[:, :], in_=rk[:, :]), reads=[rk], writes=[rk])
                    kb.op("dve", lambda e: e.scalar_tensor_tensor(out=Qn[:, a:b], in0=PA[:, :], scalar=gqn[:, 0:1], in1=rq[:, :], op0=ALU.mult, op1=ALU.mult),
                          reads=[PA, gqn, rq], wpart=[Qn])
                    kb.op("dve", lambda e: e.scalar_tensor_tensor(out=qrf[:, :], in0=PB[0:64, :], scalar=gqn[0:64, 1:2], in1=rq[0:64, :], op0=ALU.mult, op1=ALU.mult),
                          reads=[PB, gqn, rq], writes=[qrf])
                    kb.op("dve", lambda e: e.scalar_tensor_tensor(out=Kn[:, a:b], in0=S0[:, :], scalar=gkn[:, 0:1], in1=rk[:, :], op0=ALU.mult, op1=ALU.mult),
                          reads=[S0, gkn, rk], wpart=[Kn])
                    kb.op("pool", lambda e: e.tensor_tensor(out=Kr[:, a:b], in0=krr[:, a:b], in1=rk[0:64, :], op=ALU.mult), reads=[krr, rk], wpart=[Kr])
                    rope_apply(qrf, a, b, Qr[:, a:b], Qr)
                for g4 in range(4):
                    for j in range(4):
                        tb = g4 * 4 + j
                        for kc in range(2):
                            kb.op("pe", lambda e, kc=kc, tb=tb, j=j: e.matmul(PB[:, j * 128:(j + 1) * 128], lhsT=kvn[:, kc, tb * 128:(tb + 1) * 128],
                                                                             rhs=wukv[:, kc, h * 256 + 128:h * 256 + 256], start=(kc == 0), stop=(kc == 1)),
                                  reads=[kvn, wukv], writes=[PB] if (j == 0 and kc == 0) else [], wpart=[] if (j == 0 and kc == 0) else [PB])
                    kb.op("act", lambda e, g4=g4: e.activation(out=V[:, g4 * 4:(g4 + 1) * 4, :], in_=PB[:, :].rearrange("p (j d) -> p j d", j=4), func=AF.Identity),
                          reads=[PB], wpart=[V])
                for qt in range(4):
                    q0 = qt * 512
                    nkb = 4 * (qt + 1)
                    def emit_S(kbi):
                        off = max(0, kbi * 128 - q0)
                        S = S0 if kbi % 2 == 0 else S1
                        ks = slice(kbi * 128, (kbi + 1) * 128)
                        kb.op("pe", lambda e, S=S, off=off, ks=ks: e.matmul(S[:, off:512], lhsT=Kn[:, ks], rhs=Qn[:, q0 + off:q0 + 512], start=True, stop=False),
                              reads=[Kn, Qn], writes=[S])
                        kb.op("pe", lambda e, S=S, off=off, ks=ks: e.matmul(S[:, off:512], lhsT=Kr[:, ks], rhs=Qr[:, q0 + off:q0 + 512], start=False, stop=True),
                              reads=[Kr, Qr], wpart=[S])

                    emit_S(0)
                    for kbi in range(nkb):
                        if kbi + 1 < nkb:
                            emit_S(kbi + 1)
                        off = max(0, kbi * 128 - q0)
                        S = S0 if kbi % 2 == 0 else S1
                        p_ = pt[npt % 3]
                        npt += 1
                        kb.op("act", lambda e, S=S, off=off, p_=p_: e.activation(out=p_[:, off:512], in_=S[:, off:512], func=AF.Exp), reads=[S], writes=[p_])
                        if kbi * 128 >= q0:
                            kb.op("pool", lambda e, off=off, p_=p_: e.tensor_tensor(out=p_[:, off:off + 128], in0=p_[:, off:off + 128], in1=tri, op=ALU.mult),
                                  reads=[p_, cbuf], writes=[p_])
                        first, last = (kbi == 0), (kbi == nkb - 1)
                        kb.op("pe", lambda e, off=off, p_=p_, kbi=kbi, first=first, last=last: e.matmul(PO[:, off:512], lhsT=V[:, kbi, :], rhs=p_[:, off:512], start=first, stop=last),
                              reads=[V, p_], writes=[PO] if first else [], wpart=[] if first else [PO])
                        kb.op("pe", lambda e, off=off, p_=p_, first=first, last=last: e.matmul(PR[:, off:512], lhsT=ones_b, rhs=p_[:, off:512], start=first, stop=last),
                              reads=[p_, cbuf], writes=[PR] if first else [], wpart=[] if first else [PR])
                    o_ = ob[nob % 2]
                    nob += 1
                    kb.op("dve", lambda e: e.reciprocal(out=rr[:, :], in_=PR[:, :]), reads=[PR], writes=[rr])
                    kb.op("dve", lambda e, o_=o_: e.tensor_tensor(out=o_[:, :], in0=PO[:, :], in1=rr[:, :], op=ALU.mult), reads=[PO, rr], writes=[o_])
                    kb.dma("sp", self.brbT[h * 128:(h + 1) * 128, q0:q0 + 512], o_[:, :], reads=[o_])

    def phase_pool(self, l):
        nc, kb = self.nc, self.kb
        WIN = (2, 4, 8, 16)
        with ExitStack() as ps:
            inv = [self.sb(ps, "inv%d" % g, [128, T], F32) for g in range(4)]
            for g in range(4):
                kb.dma("sp", inv[g][:, :], self.poolinv[g].partition_broadcast(128), writes=[inv[g]])
            bp = self.sb(ps, "bp", [128, 8], F32)
            sp_ = self.sb(ps, "spl", [128, 8], F32)
            kb.dma("sp", bp[:, :], self.bpT[l], writes=[bp])
            kb.dma("sp", sp_[:, :], self.spT[l], writes=[sp_])
            kb.op("dve", lambda e: e.tensor_tensor(out=bp[:, :], in0=bp[:, :], in1=sp_[:, :], op=ALU.mult), reads=[bp, sp_], writes=[bp])
            pooled = self.sb(ps, "pooled", [128, 8, T], BF16)
            ub = [self.sb(ps, "ub%d" % i, [128, T], BF16) for i in range(2)]
            u32 = [self.sb(ps, "u32%d" % i, [128, T], F32) for i in range(2)]
            sa = [self.sb(ps, "sa%d" % i, [128, T], F32) for i in range(2)]
            for cc in range(8):
                g = cc // 2
                u_, f_ = ub[cc % 2], u32[cc % 2]
                kb.dma("sp", u_[:, :], self.upT[cc * 128:(cc + 1) * 128, :], writes=[u_])
                kb.op("act", lambda e, u_=u_, f_=f_: e.activation(out=f_[:, :], in_=u_[:, :], func=AF.Identity), reads=[u_], writes=[f_])
                cur = f_
                step = 1
                i = 0
                while step < WIN[g]:
                    nxt = sa[i % 2]
                    i += 1
                    kb.op("pool", lambda e, cur=cur, nxt=nxt, step=step: e.tensor_copy(out=nxt[:, 0:step], in_=cur[:, 0:step]),
                          reads=[cur], writes=[nxt])
                    kb.op("dve", lambda e, cur=cur, nxt=nxt, step=step: e.tensor_tensor(out=nxt[:, step:T], in0=cur[:, step:T], in1=cur[:, 0:T - step], op=ALU.add),
                          reads=[cur], wpart=[nxt])
                    cur = nxt
                    step *= 2
                kb.op("dve", lambda e, cur=cur, g=g: e.tensor_tensor(out=cur[:, :], in0=cur[:, :], in1=inv[g][:, :], op=ALU.mult),
                      reads=[cur, inv[g]], writes=[cur])
                kb.op("dve", lambda e, cur=cur, f_=f_, cc=cc: e.tensor_tensor(out=pooled[:, cc, :], in0=cur[:, :], in1=f_[:, :], op=ALU.subtract),
                      reads=[cur, f_], wpart=[pooled])
            wp = self.sb(ps, "wp", [128, 8, 256], BF16)
            kb.dma("pool", wp[:, :, :], self.w_pool[l].rearrange("g (kc p) e -> p (g kc) e", p=128), writes=[wp])
            pacc = [self.pm(ps, "ppool%d" % i, [128, 512]) for i in range(8)]
            ot = [self.sb(ps, "pot%d" % i, [128, T], BF16) for i in range(2)]
            n = 0
            for g in range(4):
                for eb in range(2):
                    j = g * 2 + eb
                    banks = pacc[(n % 2) * 4:(n % 2) * 4 + 4]
                    o = ot[n % 2]
                    n += 1
                    for kc in range(2):
                        for tt in range(4):
                            p = banks[tt]
                            kb.op("pe", lambda e, p=p, g=g, kc=kc, eb=eb, tt=tt: e.matmul(
                                p[:, :], lhsT=wp[:, g * 2 + kc, eb * 128:(eb + 1) * 128], rhs=pooled[:, g * 2 + kc, tt * 512:(tt + 1) * 512],
                                start=(kc == 0), stop=(kc == 1)), reads=[wp, pooled], writes=[p] if kc == 0 else [], wpart=[p] if kc else [])
                    for tt in range(4):
                        p = banks[tt]
                        kb.op("act", lambda e, p=p, o=o, tt=tt, j=j: e.activation(
                            out=o[:, tt * 512:(tt + 1) * 512], in_=p[:, :], func=AF.Identity, bias=bp[:, j:j + 1], scale=sp_[:, j:j + 1]),
                            reads=[p, bp, sp_], writes=[o] if tt == 0 else [], wpart=[o] if tt else [])
                    kb.dma("sp", self.brcT[j * 128:(j + 1) * 128, :], o[:, :], reads=[o])

    def phase_merge(self, l, xin):
        nc, kb = self.nc, self.kb
        with ExitStack() as ps:
            br = [self.sb(ps, "br%d" % j, [128, 8, T], BF16) for j in range(3)]
            for j, src in enumerate((self.braT, self.brbT, self.brcT)):
                kb.dma("sp", br[j][:, :, :], src.rearrange("(kc p) t -> p kc t", p=128), writes=[br[j]])
            ws = self.WStream(self, ps, "wb", 8, 512, 6)
            pacc = [self.pm(ps, "pmg%d" % i, [128, 512]) for i in range(8)]
            gt = [self.sb(ps, "gt%d" % i, [128, T], BF16) for i in range(3)]
            acc = [self.sb(ps, "macc%d" % i, [128, T], F32) for i in range(2)]
            tmp = [self.sb(ps, "mtmp%d" % i, [128, 512], F32) for i in range(3)]
            mo = [self.sb(ps, "mo%d" % i, [128, T], BF16) for i in range(2)]
            n = 0
            ng = 0
            nt = 0
            for dt in range(4):
                wts = [ws.load(self.w_branch[l, j][:, dt * 512:(dt + 1) * 512], 512) for j in range(3)]
                for f in range(4):
                    dblk = dt * 4 + f
                    a_ = acc[dblk % 2]
                    m_ = mo[dblk % 2]
                    for j in range(3):
                        banks = pacc[(n % 2) * 4:(n % 2) * 4 + 4]
                        n += 1
                        g_ = gt[ng % 3]
                        ng += 1
                        r0 = j * D + dblk * 128
                        kb.dma("sp", g_[:, :], self.gatesT[r0:r0 + 128, :], writes=[g_])
                        for k in range(8):
                            for tt in range(4):
                                p = banks[tt]
                                kb.op("pe", lambda e, p=p, k=k, tt=tt, f=f, j=j, w=wts[j]: e.matmul(
                                    p[:, :], lhsT=w[:, k, f * 128:(f + 1) * 128], rhs=br[j][:, k, tt * 512:(tt + 1) * 512],
                                    start=(k == 0), stop=(k == 7)), reads=[wts[j], br[j]], writes=[p] if k == 0 else [], wpart=[p] if k else [])
                        for tt in range(4):
                            p = banks[tt]
                            sl = slice(tt * 512, (tt + 1) * 512)
                            if j == 0:
                                kb.op("dve", lambda e, p=p, g_=g_, a_=a_, sl=sl: e.tensor_tensor(out=a_[:, sl], in0=p[:, :], in1=g_[:, sl], op=ALU.mult),
                                      reads=[p, g_], writes=[a_] if tt == 0 else [], wpart=[a_] if tt else [])
                            else:
                                t_ = tmp[nt % 3]
                                nt += 1
                                kb.op("dve", lambda e, p=p, g_=g_, t_=t_, sl=sl: e.tensor_tensor(out=t_[:, :], in0=p[:, :], in1=g_[:, sl], op=ALU.mult),
                                      reads=[p, g_], writes=[t_])
                                if j == 1:
                                    kb.op("pool", lambda e, a_=a_, t_=t_, sl=sl: e.tensor_tensor(out=a_[:, sl], in0=a_[:, sl], in1=t_[:, :], op=ALU.add),
                                          reads=[a_, t_], wpart=[a_])
                                else:
                                    kb.op("pool", lambda e, a_=a_, t_=t_, m_=m_, sl=sl: e.tensor_tensor(out=m_[:, sl], in0=a_[:, sl], in1=t_[:, :], op=ALU.add),
                                          reads=[a_, t_], writes=[m_] if tt == 0 else [], wpart=[m_] if tt else [])
                    kb.dma("act", self.mergedT[dblk * 128:(dblk + 1) * 128, :], m_[:, :], reads=[m_])
        kb.barrier()
        with ExitStack() as ps:
            mT = self.sb(ps, "mT", [128, 16, T], BF16)
            kb.dma("sp", mT[:, :, :], self.mergedT.rearrange("(kc p) t -> p kc t", p=128), writes=[mT])
            xdst = self.xmid
            self.gemm_resid(ps, mT, 16, 0, NTB, self.w_out[l], self.mod_d[l, 2 * D:3 * D], xin, xdst, 0, "wo")

    def gemm_resid(self, ps, aT, KC, tb0, ntb, W, gate_row, xsrc, xdst, row0, tag):
        nc, kb = self.nc, self.kb
        gb = self.sb(ps, tag + "gb", [128, D], F32)
        kb.dma("sp", gb[:, :], gate_row.partition_broadcast(128), writes=[gb])
        nsp = 2 if KC > 16 else 1
        kh = KC // nsp
        ws = self.WStream(self, ps, tag + "w", kh, 512, 3 * nsp if nsp == 1 else 4)
        pacc = [self.pm(ps, tag + "p%d" % i, [128, 512]) for i in range(4)]
        xr = [self.sb(ps, tag + "xr%d" % i, [128, 512], F32) for i in range(3)]
        yy = [self.sb(ps, tag + "yy%d" % i, [128, 512], F32) for i in range(3)]
        n = 0
        tiles = [(ct, tb) for ct in range(4) for tb in range(ntb)]

        def xload(i):
            ct_, tb_ = tiles[i]
            rr_ = row0 + tb_ * 128
            kb.dma("sp", xr[i % 3][:, :], xsrc[rr_:rr_ + 128, ct_ * 512:(ct_ + 1) * 512], writes=[xr[i % 3]])

        xload(0)
        xload(1)
        for ct in range(4):
            c0 = ct * 512
            wts = [ws.load(W[h * kh * 128:(h + 1) * kh * 128, c0:c0 + 512], 512) for h in range(nsp)]
            for tb in range(ntb):
                p = pacc[n % 4]
                x_ = xr[n % 3]
                y_ = yy[n % 3]
                if n + 2 < len(tiles):
                    xload(n + 2)
                n += 1
                r0 = row0 + tb * 128
                for k in range(KC):
                    w = wts[k // kh]
                    kb.op("pe", lambda e, p=p, k=k, tb=tb, w=w: e.matmul(
                        p[:, :], lhsT=aT[:, k, tb * 128:(tb + 1) * 128], rhs=w[:, k % kh, :], start=(k == 0), stop=(k == KC - 1)),
                        reads=[w, aT], writes=[p] if k == 0 else [], wpart=[p] if k else [])
                kb.op("dve", lambda e, p=p, y_=y_, c0=c0: e.tensor_tensor(out=y_[:, :], in0=p[:, :], in1=gb[:, c0:c0 + 512], op=ALU.mult),
                      reads=[p, gb], writes=[y_])
                kb.op("pool", lambda e, y_=y_, x_=x_: e.tensor_tensor(out=y_[:, :], in0=y_[:, :], in1=x_[:, :], op=ALU.add),
                      reads=[y_, x_], writes=[y_])
                kb.dma("sp", xdst[r0:r0 + 128, c0:c0 + 512], y_[:, :], reads=[y_])

    def phase_ffn(self, l, xo):
        nc, kb = self.nc, self.kb
        NH = 2
        TH = T // NH
        for th in range(NH):
            with ExitStack() as ps:
                actT = self.sb(ps, "actT", [128, 44, TH], BF16)
                with ExitStack() as ps2:
                    h2T = self.sb(ps2, "h2T", [128, 16, TH], BF16)
                    h2T.alt = h2T
                    with ExitStack() as ps3:
                        gs, sh = self.load_mod_cols(ps3, l, 2, self.g2T, "m2")
                        self.norm_to_hT(ps3, self.xmid, th * (TH // 128), TH // 128, h2T, gs, sh, "n2")
                    kb.barrier()
                    ws = self.WStream(self, ps2, "wf", 16, 512, 4)
                    pacc = [self.pm(ps2, "pf%d" % i, [128, 512]) for i in range(8)]
                    sg = [self.sb(ps2, "sg%d" % i, [128, 512], F32) for i in range(3)]
                    n = 0
                    ns = 0
                    W = self.w_ffn_in[l]
                    for ft in range(FFN // 512):
                        wg = ws.load(W[:, ft * 512:(ft + 1) * 512], 512)
                        wu = ws.load(W[:, FFN + ft * 512:FFN + (ft + 1) * 512], 512)
                        for f in range(4):
                            banks = pacc[(n % 2) * 4:(n % 2) * 4 + 4]
                            n += 1
                            for gi, w in enumerate((wg, wu)):
                                for k in range(16):
                                    for tt in range(TH // 512):
                                        p = banks[gi * 2 + tt]
                                        kb.op("pe", lambda e, p=p, k=k, tt=tt, f=f, w=w: e.matmul(
                                            p[:, :], lhsT=w[:, k, f * 128:(f + 1) * 128], rhs=h2T[:, k, tt * 512:(tt + 1) * 512],
                                            start=(k == 0), stop=(k == 15)), reads=[w, h2T, h2T.alt], writes=[p] if k == 0 else [], wpart=[p] if k else [])
                            for tt in range(TH // 512):
                                s_ = sg[ns % 3]
                                ns += 1
                                kb.op("act", lambda e, p=banks[tt], s_=s_: e.activation(out=s_[:, :], in_=p[:, :], func=AF.Silu), reads=[banks[tt]], writes=[s_])
                                kb.op("dve", lambda e, p=banks[2 + tt], s_=s_, fb=ft * 4 + f, tt=tt: e.tensor_tensor(
                                    out=actT[:, fb, tt * 512:(tt + 1) * 512], in0=p[:, :], in1=s_[:, :], op=ALU.mult),
                                    reads=[banks[2 + tt], s_], wpart=[actT])
                kb.barrier()
                with ExitStack() as ps2:
                    self.gemm_resid(ps2, actT, 44, 0, TH // 128, self.w_ffn_out[l], self.mod_d[l, 5 * D:6 * D], self.xmid, xo, th * TH, "fo")
            kb.barrier()


def _consts():
    i = np.arange(128)
    ident = np.eye(128, dtype=np.float32)
    U = (i[:, None] <= i[None, :]).astype(np.float32)
    negT = np.where(i[:, None] <= i[None, :], 0.0, NEG).astype(np.float32)
    neg = negT.T.copy()
    tri = U.copy()
    ones = np.ones((128, 128), np.float32)
    cst = np.concatenate([ident, U, negT, neg, tri, ones], axis=1)
    pos = np.arange(T, dtype=np.float32)
    freqs = (np.float32(10000.0) ** (-np.arange(0, 64, 2, dtype=np.float32) / np.float32(64))).astype(np.float32)
    ang = pos[:, None] * freqs[None, :]
    cos = np.cos(ang).astype(np.float32).T
    sin = np.sin(ang).astype(np.float32).T
    cosT = np.concatenate([cos, cos], axis=0)
    sinT = np.concatenate([sin, sin], axis=0)
    P = np.zeros((64, 64), np.float32)
    for m in range(32):
        P[m + 32, m] = -1.0
        P[m, m + 32] = 1.0
    ropec = np.concatenate([cosT, sinT, P], axis=1).astype(np.float32)
    tt = np.arange(T)
    poolinv = np.stack([1.0 / np.minimum(tt + 1, w) for w in (2, 4, 8, 16)]).astype(np.float32)
    return cst, ropec, poolinv


def _colT(v, n):
    return np.ascontiguousarray(v.reshape(n, 128).T)


def make_in_maps(inp, cores):
    f = lambda a: np.ascontiguousarray(np.asarray(a, dtype=np.float32))
    cst, ropec, poolinv = _consts()
    g = {k: f(v) for k, v in inp.items()}

    def colT_l(a, n):
        return np.stack([_colT(a[l], n) for l in range(DEPTH)])

    def qk192(a):
        o = np.zeros((DEPTH, 128, 2), np.float32)
        o[:, :, 0] = a[:, 0:128]
        o[:, 0:64, 1] = a[:, 128:192]
        return o

    shared = {
        "w_ada": g["w_ada"], "b_ada": g["b_ada"], "g1T": colT_l(g["g_norm1"], 16), "g2T": colT_l(g["g_norm2"], 16),
        "w_in": g["w_in"], "b_mgate": g["b_mgate"].reshape(DEPTH, 8), "g_mnorm": g["g_mnorm"].reshape(DEPTH, 1024),
        "gqlT": colT_l(g["g_qlat"], 4), "w_uq": g["w_uq"], "gkvT": colT_l(g["g_kvlat"], 2), "w_ukv": g["w_ukv"],
        "gqnT": qk192(g["g_qn"]), "gknT": qk192(g["g_kn"]), "w_pool": g["w_pool"],
        "bpT": colT_l(g["b_pool"].reshape(DEPTH, 1024), 8), "spT": colT_l(g["s_pool"], 8),
        "w_branch": g["w_branch"], "w_out": g["w_out"], "w_ffn_in": g["w_ffn_in"], "w_ffn_out": g["w_ffn_out"],
        "cst": cst, "ropec": ropec, "poolinv": poolinv,
    }
    maps = []
    for b in cores:
        m = dict(shared)
        m["x"] = g["x"][b]
        m["cT"] = _colT(g["c"][b], 16)
        maps.append(m)
    return maps


_CACHE = {}


def kernel(**inputs):
    cores = list(range(8))
    if "nc" not in _CACHE:
        _CACHE["nc"] = Prog().build()
    nc = _CACHE["nc"]
    maps = make_in_maps(inputs, cores)
    res = run_bass_kernel_spmd(nc, maps, core_ids=cores)
    return np.stack([np.asarray(r["out"], dtype=np.float32) for r in res.results], axis=0)
```

```python
import types
import numpy as np
from contextlib import ExitStack
import concourse.bass as bass
import concourse.mybir as mybir
from concourse.bass_utils import run_bass_kernel_spmd

F32 = mybir.dt.float32
BF16 = mybir.dt.bfloat16
AF = mybir.ActivationFunctionType
ALU = mybir.AluOpType
AX = mybir.AxisListType

D = 2048
T = 2048
DEPTH = 2
NTB = T // 128
FFN = 5632
IN_W = 11080
EPS = 1e-6
C_QM, C_KM, C_VM, C_OM, C_IF, C_QL, C_KV, C_KR, C_UP, C_GT = 0, 512, 1024, 2048, 3072, 3080, 3592, 3848, 3912, 4936
NEG = -30000.0


class Buf:
    __slots__ = ("t", "w", "r", "name", "alt")

    def __init__(self, t, name=""):
        self.t = t
        self.w = {}
        self.r = {}
        self.name = name
        self.alt = None

    def __getitem__(self, k):
        return self.t[k]


def _freeze(fn):
    if fn.__closure__ is None:
        return fn
    cells = []
    for c in fn.__closure__:
        try:
            cells.append(types.CellType(c.cell_contents))
        except ValueError:
            cells.append(c)
    return types.FunctionType(fn.__code__, fn.__globals__, fn.__name__, fn.__defaults__, tuple(cells))


class Op:
    __slots__ = ("fn", "waits", "dma")

    def __init__(self, fn, waits, dma=None):
        self.fn = fn
        self.waits = waits
        self.dma = dma


ENGS = ["pe", "act", "dve", "pool", "sp"]


class KB:
    def __init__(self, nc, es, n_dma=24):
        self.nc = nc
        self.ops = {e: [] for e in ENGS}
        self.known = {e: {} for e in ENGS}
        self.sigset = {e: set() for e in ENGS}
        self.lastc = {e: 0 for e in ENGS}
        self.n_dma = n_dma
        self.dma_val = [0] * n_dma
        self.dma_rr2 = {True: 0, False: 0}
        self.csem = {e: es.enter_context(nc.semaphore("c_" + e)) for e in ENGS if e != "sp"}
        self.dsem = [es.enter_context(nc.semaphore("d%d" % i)) for i in range(n_dma)]

    def _collect(self, eng, reads, writes, wpart=()):
        need = {}
        own = ("c", eng)
        for b in reads:
            for k, v in b.w.items():
                if v > need.get(k, 0):
                    need[k] = v
        for b in writes:
            for dct in (b.w, b.r):
                for k, v in dct.items():
                    if v > need.get(k, 0):
                        need[k] = v
        for b in wpart:
            for k, v in b.w.items():
                if k == own:
                    continue
                if v > need.get(k, 0):
                    need[k] = v
        if eng == "pe":
            need.pop(own, None)
        waits = []
        kn = self.known[eng]
        for k, v in need.items():
            if kn.get(k, 0) >= v:
                continue
            kn[k] = v
            waits.append((k, v))
            if k[0] == "c":
                self.sigset[k[1]].add(v)
        return waits

    def op(self, eng, fn, reads=(), writes=(), wpart=()):
        waits = self._collect(eng, reads, writes, wpart)
        self.ops[eng].append(Op(_freeze(fn), waits))
        idx = len(self.ops[eng])
        self.lastc[eng] = idx
        key = ("c", eng)
        for b in reads:
            b.r[key] = idx
        for b in writes:
            b.w = {key: idx}
            b.r = {}
        for b in wpart:
            b.w[key] = idx
        return idx

    def dma(self, q, out_ap, in_ap, reads=(), writes=(), wpart=(), slow=False):
        lo, hi = (0, 6) if q == "pool" else (6, self.n_dma)
        k = lo + self.dma_rr2[q == "pool"] % (hi - lo)
        self.dma_rr2[q == "pool"] += 1
        prev = self.dma_val[k]
        waits = self._collect(q, reads, writes, wpart)
        key = ("d", k)
        if prev > 0 and self.known[q].get(key, 0) < prev:
            waits.append((key, prev))
            self.known[q][key] = prev
        new = prev + 16
        self.dma_val[k] = new
        if slow:
            fn = lambda e: e.dma_start(out=out_ap, in_=in_ap, allow_slow_non_contiguous=True)
        else:
            fn = lambda e: e.dma_start(out=out_ap, in_=in_ap)
        self.ops[q].append(Op(fn, waits, dma=k))
        for b in reads:
            b.r[key] = new
        for b in writes:
            b.w = {key: new}
            b.r = {}
        for b in wpart:
            b.w[key] = new

    def barrier(self):
        outstanding = {}
        for e in ENGS:
            if e != "sp" and self.lastc[e] > 0:
                outstanding[("c", e)] = self.lastc[e]
        for k in range(self.n_dma):
            if self.dma_val[k] > 0:
                outstanding[("d", k)] = self.dma_val[k]
        for e in ENGS:
            waits = []
            kn = self.known[e]
            for k, v in outstanding.items():
                if kn.get(k, 0) >= v:
                    continue
                kn[k] = v
                waits.append((k, v))
                if k[0] == "c":
                    self.sigset[k[1]].add(v)
            if waits:
                self.ops[e].append(Op(lambda eng: eng.nop(), waits))

    def emit(self):
        nc = self.nc
        rank = {}
        for e in ENGS:
            rank[e] = {idx: i + 1 for i, idx in enumerate(sorted(self.sigset[e]))}

        def run(e, engobj):
            rk = rank[e]
            for i, op in enumerate(self.ops[e], start=1):
                for (k, v) in op.waits:
                    if k[0] == "c":
                        engobj.wait_ge(self.csem[k[1]], rank[k[1]][v])
                    else:
                        engobj.wait_ge(self.dsem[k[1]], v)
                ins = op.fn(engobj)
                if op.dma is not None:
                    ins.then_inc(self.dsem[op.dma], 16)
                elif i in rk:
                    ins.then_inc(self.csem[e], 1)

        with nc.Block() as block:
            @block.tensor
            def _(pe):
                run("pe", pe)

            @block.scalar
            def _(act):
                run("act", act)

            @block.vector
            def _(dve):
                run("dve", dve)

            @block.gpsimd
            def _(pool):
                run("pool", pool)

            @block.sync
            def _(sp):
                run("sp", sp)


class Prog:
    def __init__(self, debug=None, stop_after=None):
        self.debug = debug or []
        self.stop_after = stop_after
        self.nc = bass.Bass("TRN2", target_bir_lowering=False)
        self.es = ExitStack()

    def din(self, name, shape, dt=F32):
        return self.nc.dram_tensor(name, list(shape), dt, kind="ExternalInput").ap()

    def dscr(self, name, shape, dt=BF16):
        kind = "ExternalOutput" if name in self.debug else "Internal"
        return self.nc.dram_tensor(name, list(shape), dt, kind=kind).ap()

    def sb(self, ps, name, shape, dt):
        self.uid = getattr(self, "uid", 0) + 1
        name = "%s_%d" % (name, self.uid)
        return Buf(ps.enter_context(self.nc.sbuf_tensor(name, list(shape), dt)), name)

    def pm(self, ps, name, shape, dt=F32):
        self.uid = getattr(self, "uid", 0) + 1
        name = "%s_%d" % (name, self.uid)
        return Buf(ps.enter_context(self.nc.psum_tensor(name, list(shape), dt)), name)

    def build(self):
        nc = self.nc
        es = self.es
        kb = self.kb = KB(nc, es)
        self.x = self.din("x", [T, D])
        self.cT = self.din("cT", [128, 16])
        self.w_ada = self.din("w_ada", [DEPTH, D, 6 * D])
        self.b_ada = self.din("b_ada", [DEPTH, 6 * D])
        self.g1T = self.din("g1T", [DEPTH, 128, 16])
        self.g2T = self.din("g2T", [DEPTH, 128, 16])
        self.w_in = self.din("w_in", [DEPTH, D, IN_W])
        self.b_mgate = self.din("b_mgate", [DEPTH, 8])
        self.g_mnorm = self.din("g_mnorm", [DEPTH, 1024])
        self.gqlT = self.din("gqlT", [DEPTH, 128, 4])
        self.w_uq = self.din("w_uq", [DEPTH, 512, 1536])
        self.gkvT = self.din("gkvT", [DEPTH, 128, 2])
        self.w_ukv = self.din("w_ukv", [DEPTH, 256, 2048])
        self.gqnT = self.din("gqnT", [DEPTH, 128, 2])
        self.gknT = self.din("gknT", [DEPTH, 128, 2])
        self.w_pool = self.din("w_pool", [DEPTH, 4, 256, 256])
        self.bpT = self.din("bpT", [DEPTH, 128, 8])
        self.spT = self.din("spT", [DEPTH, 128, 8])
        self.w_branch = self.din("w_branch", [DEPTH, 3, 1024, D])
        self.w_out = self.din("w_out", [DEPTH, D, D])
        self.w_ffn_in = self.din("w_ffn_in", [DEPTH, D, 2 * FFN])
        self.w_ffn_out = self.din("w_ffn_out", [DEPTH, FFN, D])
        self.cst = self.din("cst", [128, 128 * 6])
        self.ropec = self.din("ropec", [64, 2 * T + 64])
        self.poolinv = self.din("poolinv", [4, T])
        self.out = self.nc.dram_tensor("out", [T, D], F32, kind="ExternalOutput").ap()
        self.mod_d = self.dscr("mod_d", [DEPTH, 6 * D], F32)
        self.qmT = self.dscr("qmT", [512, T])
        self.kmT = self.dscr("kmT", [512, T])
        self.k_tm = self.dscr("k_tm", [T, 512])
        self.v_tm = self.dscr("v_tm", [T, 1024])
        self.so_tm = self.dscr("so_tm", [T, 1024])
        self.if_d = self.dscr("if_d", [T, 8], F32)
        self.qlatT = self.dscr("qlatT", [512, T])
        self.kvlatT = self.dscr("kvlatT", [256, T])
        self.krT = self.dscr("krT", [64, T])
        self.upT = self.dscr("upT", [1024, T])
        self.gatesT = self.dscr("gatesT", [6144, T])
        self.braT = self.dscr("braT", [1024, T])
        self.brbT = self.dscr("brbT", [1024, T])
        self.brcT = self.dscr("brcT", [1024, T])
        self.mergedT = self.dscr("mergedT", [D, T])
        self.xmid = self.dscr("xmid", [T, D], F32)
        self.x1 = self.dscr("x1", [T, D], F32)

        self.cf = self.sb(es, "cf", [128, 128 * 6], F32)
        self.cb = self.sb(es, "cb", [128, 128 * 6], BF16)
        kb.dma("sp", self.cf[:, :], self.cst[:, :], writes=[self.cf])
        kb.dma("pool", self.cb[:, :], self.cst[:, :], writes=[self.cb])
        self.stages = [
            ("mod", lambda: self.phase_mod()),
        ]
        xin = self.x
        for l in range(DEPTH):
            xo = self.out if l == DEPTH - 1 else self.x1
            self.stages += [
                ("A%d" % l, lambda l=l, xin=xin: self.phase_win(l, xin)),
                ("C%d" % l, lambda l=l: self.phase_mlstm(l)),
                ("D%d" % l, lambda l=l: self.phase_mla(l)),
                ("P%d" % l, lambda l=l: self.phase_pool(l)),
                ("E%d" % l, lambda l=l, xin=xin: self.phase_merge(l, xin)),
                ("F%d" % l, lambda l=l, xo=xo: self.phase_ffn(l, xo)),
            ]
            xin = xo
        for name, fn in self.stages:
            fn()
            kb.barrier()
            if self.stop_after == name:
                break
        kb.barrier()
        kb.emit()
        return nc

    def cF(self, i):
        return self.cf[:, i * 128:(i + 1) * 128]

    def cB(self, i):
        return self.cb[:, i * 128:(i + 1) * 128]

    def phase_mod(self):
        nc, kb = self.nc, self.kb
        with ExitStack() as ps:
            ct = self.sb(ps, "ct", [128, 16], F32)
            ca = self.sb(ps, "ca", [128, 16], F32)
            wt = [self.sb(ps, "wada%d" % i, [128, 16, 512], F32) for i in range(2)]
            pr = [self.pm(ps, "pmod%d" % i, [1, 512]) for i in range(2)]
            brow = self.sb(ps, "brow", [1, DEPTH * 6 * D], F32)
            orow = [self.sb(ps, "orow%d" % i, [1, 512], F32) for i in range(2)]
            kb.dma("sp", ct[:, :], self.cT[:, :], writes=[ct])
            kb.dma("sp", brow[:, :], self.b_ada.rearrange("l n -> (l n)").rearrange("(o n) -> o n", o=1), writes=[brow])
            kb.op("act", lambda e: e.activation(out=ca[:, :], in_=ct[:, :], func=AF.Silu), reads=[ct], writes=[ca])
            tiles = [(l, cbk) for l in range(DEPTH) for cbk in range(24)]

            def load(i):
                l, cbk = tiles[i]
                src = self.w_ada[l, :, cbk * 512:(cbk + 1) * 512].rearrange("(kc p) n -> p kc n", p=128)
                kb.dma("sp", wt[i % 2][:, :, :], src, writes=[wt[i % 2]])

            load(0)
            for i, (l, cbk) in enumerate(tiles):
                if i + 1 < len(tiles):
                    load(i + 1)
                w = wt[i % 2]
                p = pr[i % 2]
                o = orow[i % 2]
                for kc in range(16):
                    kb.op("pe", lambda e, w=w, p=p, kc=kc: e.matmul(p[0:1, :], lhsT=ca[:, kc:kc + 1], rhs=w[:, kc, :],
                                                                    start=(kc == 0), stop=(kc == 15)),
                          reads=[ca, w], writes=[p] if kc == 0 else [], wpart=[p] if kc else [])
                off = l * 6 * D + cbk * 512
                kb.op("dve", lambda e, p=p, o=o, off=off: e.tensor_tensor(out=o[0:1, :], in0=p[0:1, :], in1=brow[0:1, off:off + 512], op=ALU.add),
                      reads=[p, brow], writes=[o])
                kb.dma("sp", self.mod_d[l:l + 1, cbk * 512:(cbk + 1) * 512], o[0:1, :], reads=[o])

    def load_mod_cols(self, ps, l, which, gT, name):
        nc, kb = self.nc, self.kb
        sh = self.sb(ps, name + "sh", [128, 16], F32)
        sc = self.sb(ps, name + "sc", [128, 16], F32)
        g = self.sb(ps, name + "g", [128, 16], F32)
        gs = self.sb(ps, name + "gs", [128, 16], F32)
        base = 0 if which == 1 else 3 * D
        kb.dma("sp", sh[:, :], self.mod_d[l, base:base + D].rearrange("(j p) -> p j", p=128), writes=[sh], slow=True)
        kb.dma("sp", sc[:, :], self.mod_d[l, base + D:base + 2 * D].rearrange("(j p) -> p j", p=128), writes=[sc], slow=True)
        kb.dma("sp", g[:, :], gT[l], writes=[g])
        kb.op("dve", lambda e: e.tensor_scalar(out=sc[:, :], in0=sc[:, :], scalar1=1.0, scalar2=1.0, op0=ALU.add, op1=ALU.mult),
              reads=[sc], writes=[sc])
        kb.op("dve", lambda e: e.tensor_tensor(out=gs[:, :], in0=sc[:, :], in1=g[:, :], op=ALU.mult), reads=[sc, g], writes=[gs])
        return gs, sh

    def norm_to_hT(self, ps, xsrc, tb0, ntb, hT, gs, sh, tag):
        nc, kb = self.nc, self.kb
        xt = [self.sb(ps, tag + "xt%d" % i, [128, D], F32) for i in range(2)]
        junk = self.sb(ps, tag + "junk", [128, D], BF16)
        xn = [self.sb(ps, tag + "xn%d" % i, [128, D], BF16) for i in range(2)]
        ss = [self.sb(ps, tag + "ss%d" % i, [128, 1], F32) for i in range(2)]
        rs = [self.sb(ps, tag + "rs%d" % i, [128, 1], F32) for i in range(2)]
        pt = [self.pm(ps, tag + "pt%d" % i, [128, 1024], BF16) for i in range(4)]
        ident = self.cB(0)
        for j in range(ntb):
            tb = tb0 + j
            x_, xn_, ss_, rs_ = xt[j % 2], xn[j % 2], ss[j % 2], rs[j % 2]
            kb.dma("sp", x_[:, :], xsrc[tb * 128:(tb + 1) * 128, :], writes=[x_])
            kb.op("act", lambda e, x_=x_, ss_=ss_: e.activation(out=junk[:, :], in_=x_[:, :], func=AF.Square, accum_out=ss_[:, 0:1]),
                  reads=[x_], writes=[junk, ss_])
            kb.op("act", lambda e, ss_=ss_: e.activation(out=ss_[:, :], in_=ss_[:, :], func=AF.Sqrt, bias=EPS, scale=1.0 / D),
                  reads=[ss_], writes=[ss_])
            kb.op("dve", lambda e, ss_=ss_, rs_=rs_: e.reciprocal(out=rs_[:, :], in_=ss_[:, :]), reads=[ss_], writes=[rs_])
            kb.op("act", lambda e, x_=x_, xn_=xn_, rs_=rs_: e.activation(out=xn_[:, :], in_=x_[:, :], func=AF.Identity, scale=rs_[:, 0:1]),
                  reads=[x_, rs_], writes=[xn_])
            for half in range(2):
                p = pt[(j * 2 + half) % 4]
                for q in range(8):
                    dc = half * 8 + q
                    kb.op("pe", lambda e, p=p, q=q, dc=dc, xn_=xn_: e.transpose(p[:, q * 128:(q + 1) * 128], xn_[:, dc * 128:(dc + 1) * 128], ident),
                          reads=[xn_, self.cb], writes=[p] if q == 0 else [], wpart=[p] if q else [])
                for q in range(8):
                    dc = half * 8 + q
                    if half == 0:
                        kb.op("dve", lambda e, p=p, q=q, dc=dc, j=j: e.tensor_scalar(
                            out=hT[:, dc, j * 128:(j + 1) * 128], in0=p[:, q * 128:(q + 1) * 128],
                            scalar1=gs[:, dc:dc + 1], scalar2=sh[:, dc:dc + 1], op0=ALU.mult, op1=ALU.add),
                            reads=[p, gs, sh], wpart=[hT])
                    else:
                        kb.op("act", lambda e, p=p, q=q, dc=dc, j=j: e.activation(
                            out=hT[:, dc, j * 128:(j + 1) * 128], in_=p[:, q * 128:(q + 1) * 128],
                            func=AF.Identity, bias=sh[:, dc:dc + 1], scale=gs[:, dc:dc + 1]),
                            reads=[p, gs, sh], wpart=[hT.alt])

    class WStream:
        def __init__(self, prog, ps, name, kc, ncols, nbuf=3):
            self.prog = prog
            self.bufs = [prog.sb(ps, "%s%d" % (name, i), [128, kc, ncols], BF16) for i in range(nbuf)]
            self.i = 0
            self.kc = kc

        def load(self, src2d, n):
            b = self.bufs[self.i % len(self.bufs)]
            self.i += 1
            self.prog.kb.dma("pool", b[:, :, 0:n], src2d.rearrange("(kc p) n -> p kc n", p=128), writes=[b])
            return b

    def phase_win(self, l, xin):
        nc, kb = self.nc, self.kb
        with ExitStack() as ps:
            hT = self.sb(ps, "hT", [128, 16, T], BF16)
            hT.alt = hT
            with ExitStack() as ps2:
                gs, sh = self.load_mod_cols(ps2, l, 1, self.g1T, "m1")
                self.norm_to_hT(ps2, xin, 0, NTB, hT, gs, sh, "n1")
            kb.barrier()
            ws = self.WStream(self, ps, "wi", 16, 512, 3)
            pacc = [self.pm(ps, "pacc%d" % i, [128, 512]) for i in range(8)]
            ot = [self.sb(ps, "ot%d" % i, [128, T], BF16) for i in range(3)]
            otm = [self.sb(ps, "otm%d" % i, [128, 512], BF16) for i in range(3)]
            ifs = self.sb(ps, "ifs", [128, NTB, 8], F32)
            W = self.w_in[l]
            state = {"pset": 0, "ot": 0, "pb": 0, "otm": 0, "ev": 0}

            def fm_tile(c0, n, dst, func, scale):
                wt = ws.load(W[:, c0:c0 + n], n)
                for f0 in range(0, n, 128):
                    m = min(128, n - f0)
                    pset = state["pset"]
                    state["pset"] ^= 1
                    banks = pacc[pset * 4:(pset + 1) * 4]
                    for k in range(16):
                        for tt in range(4):
                            p = banks[tt]
                            kb.op("pe", lambda e, p=p, k=k, tt=tt, f0=f0, m=m, wt=wt: e.matmul(
                                p[0:m, :], lhsT=wt[:, k, f0:f0 + m], rhs=hT[:, k, tt * 512:(tt + 1) * 512],
                                start=(k == 0), stop=(k == 15)),
                                reads=[wt, hT, hT.alt], writes=[p] if k == 0 else [], wpart=[p] if k else [])
                    o = ot[state["ot"] % 3]
                    state["ot"] += 1
                    for tt in range(4):
                        p = banks[tt]
                        eng = "act" if (func is not None or tt % 2 == 1) else "dve"
                        first = (tt == 0)
                        if eng == "act":
                            kb.op("act", lambda e, p=p, o=o, tt=tt, m=m: e.activation(
                                out=o[0:m, tt * 512:(tt + 1) * 512], in_=p[0:m, :], func=(func or AF.Identity), scale=scale),
                                reads=[p], writes=[o] if first else [], wpart=[] if first else [o])
                        else:
                            kb.op("dve", lambda e, p=p, o=o, tt=tt, m=m: e.tensor_scalar(
                                out=o[0:m, tt * 512:(tt + 1) * 512], in0=p[0:m, :], scalar1=scale, scalar2=None, op0=ALU.mult),
                                reads=[p], writes=[o] if first else [], wpart=[] if first else [o])
                    r0 = c0 - dst[1] + f0
                    kb.dma("sp", dst[0][r0:r0 + m, :], o[0:m, :], reads=[o])

            def tm_tile(c0, n, dst, func, scale):
                wt = ws.load(W[:, c0:c0 + n], n)
                for tb in range(NTB):
                    p = pacc[state["pb"] % 8]
                    state["pb"] += 1
                    for k in range(16):
                        kb.op("pe", lambda e, p=p, k=k, tb=tb, wt=wt: e.matmul(
                            p[:, 0:n], lhsT=hT[:, k, tb * 128:(tb + 1) * 128], rhs=wt[:, k, 0:n],
                            start=(k == 0), stop=(k == 15)),
                            reads=[wt, hT, hT.alt], writes=[p] if k == 0 else [], wpart=[p] if k else [])
                    if dst is None:
                        kb.op("dve", lambda e, p=p, tb=tb: e.tensor_copy(out=ifs[:, tb, :], in_=p[:, 0:8]), reads=[p], wpart=[ifs])
                        continue
                    o = otm[state["otm"] % 3]
                    state["otm"] += 1
                    state["ev"] += 1
                    if func is not None or state["ev"] % 2:
                        kb.op("act", lambda e, p=p, o=o: e.activation(out=o[:, 0:n], in_=p[:, 0:n], func=(func or AF.Identity), scale=scale),
                              reads=[p], writes=[o])
                    else:
                        kb.op("dve", lambda e, p=p, o=o: e.tensor_scalar(out=o[:, 0:n], in0=p[:, 0:n], scalar1=scale, scalar2=None, op0=ALU.mult),
                              reads=[p], writes=[o])
                    cc = c0 - dst[1]
                    kb.dma("sp", dst[0][tb * 128:(tb + 1) * 128, cc:cc + n], o[:, 0:n], reads=[o])

            ksc = float(128 ** -0.5)
            tm_tile(C_IF, 8, None, None, 1.0)
            tm_tile(C_KM, 512, (self.k_tm, C_KM), None, ksc)
            for c0 in (C_VM, C_VM + 512):
                tm_tile(c0, 512, (self.v_tm, C_VM), None, 1.0)
            for c0 in (C_OM, C_OM + 512):
                tm_tile(c0, 512, (self.so_tm, C_OM), AF.Sigmoid, 1.0)
            kb.dma("sp", self.if_d.rearrange("(c p) e -> p c e", p=128), ifs[:, :, :], reads=[ifs])
            fm_tile(C_QM, 512, (self.qmT, C_QM), None, 1.0)
            fm_tile(C_KM, 512, (self.kmT, C_KM), None, ksc)
            fm_tile(C_QL, 512, (self.qlatT, C_QL), None, 1.0)
            fm_tile(C_KV, 256, (self.kvlatT, C_KV), None, 1.0)
            fm_tile(C_KR, 64, (self.krT, C_KR), None, 1.0)
            for c0 in (C_UP, C_UP + 512):
                fm_tile(c0, 512, (self.upT, C_UP), None, 1.0)
            for c0 in range(C_GT, IN_W, 512):
                fm_tile(c0, 512, (self.gatesT, C_GT), AF.Sigmoid, 1.0)

    def phase_mlstm(self, l):
        nc, kb = self.nc, self.kb
        with ExitStack() as ps:
            cf, cbf = self.cf, self.cb
            identF, U, negT, neg, onesF = self.cF(0), self.cF(1), self.cF(2), self.cF(3), self.cF(5)
            identB = self.cB(0)
            qT = self.sb(ps, "mqT", [128, 4, T], BF16)
            kT = self.sb(ps, "mkT", [128, 4, T], BF16)
            ktm = self.sb(ps, "mktm", [128, NTB, 512], BF16)
            vaug = self.sb(ps, "mvaug", [128, NTB, 4, 257], BF16)
            ifs = self.sb(ps, "mifs", [128, NTB, 8], F32)
            bmg = self.sb(ps, "mbmg", [128, 8], F32)
            gmn = self.sb(ps, "mgmn", [128, 1024], F32)
            kb.dma("sp", qT[:, :, :], self.qmT.rearrange("(h d) t -> d h t", d=128), writes=[qT])
            kb.dma("sp", kT[:, :, :], self.kmT.rearrange("(h d) t -> d h t", d=128), writes=[kT])
            kb.dma("sp", ktm[:, :, :], self.k_tm.rearrange("(c p) e -> p c e", p=128), writes=[ktm])
            kb.op("pool", lambda e: e.memset(vaug[:, :, :, :], 1.0), writes=[vaug])
            for c in range(NTB):
                kb.dma("sp", vaug[:, c, :, 0:256], self.v_tm[c * 128:(c + 1) * 128, :].rearrange("p (h v) -> p h v", h=4), reads=[], wpart=[vaug])
            kb.dma("sp", ifs[:, :, :], self.if_d.rearrange("(c p) e -> p c e", p=128), writes=[ifs])
            kb.dma("sp", bmg[:, :], self.b_mgate[l].partition_broadcast(128), writes=[bmg])
            kb.dma("sp", gmn[:, :], self.g_mnorm[l].partition_broadcast(128), writes=[gmn])
            NI = NTB * 4
            mk = lambda n, w=NI: self.sb(ps, n, [128, w], F32)
            ipre, fpre, logf, bcs, bL, g, cm, gmax = [mk("m" + n) for n in ("ipre", "fpre", "logf", "bcs", "bL", "g", "cm", "gmax")]
            mprev = mk("mprev", NI + 4)
            mxL, decay, wsc, mx, nmx, em = [mk("m" + n) for n in ("mxL", "decay", "wsc", "mx", "nmx", "em")]
            pP = self.pm(ps, "mpP", [128, 512])
            pT = self.pm(ps, "mpT", [128, 1024], BF16)
            pX = [self.pm(ps, "mpX%d" % i, [128, 384]) for i in range(2)]
            pN = [self.pm(ps, "mpN%d" % i, [128, 257]) for i in range(2)]
            pC = [self.pm(ps, "mpC%d" % i, [128, 257]) for i in range(2)]
            for c in range(NTB):
                kb.op("dve", lambda e, c=c: e.tensor_tensor(out=ipre[:, c * 4:(c + 1) * 4], in0=ifs[:, c, 0:4], in1=bmg[:, 0:4], op=ALU.add),
                      reads=[ifs, bmg], wpart=[ipre])
                kb.op("dve", lambda e, c=c: e.tensor_tensor(out=fpre[:, c * 4:(c + 1) * 4], in0=ifs[:, c, 4:8], in1=bmg[:, 4:8], op=ALU.add),
                      reads=[ifs, bmg], wpart=[fpre])
            kb.op("act", lambda e: e.activation(out=logf[:, :], in_=fpre[:, :], func=AF.Exp, scale=-1.0), reads=[fpre], writes=[logf])
            kb.op("act", lambda e: e.activation(out=logf[:, :], in_=logf[:, :], func=AF.Ln, bias=1.0, scale=1.0), reads=[logf], writes=[logf])
            kb.op("dve", lambda e: e.tensor_scalar(out=logf[:, :], in0=logf[:, :], scalar1=-1.0, scalar2=None, op0=ALU.mult), reads=[logf], writes=[logf])
            kb.op("pe", lambda e: e.matmul(pP[:, 0:NI], lhsT=U, rhs=logf[:, :], start=True, stop=True), reads=[logf, cf], writes=[pP])
            kb.op("dve", lambda e: e.tensor_copy(out=bcs[:, :], in_=pP[:, 0:NI]), reads=[pP], writes=[bcs])
            kb.op("pe", lambda e: e.matmul(pP[:, 0:NI], lhsT=onesF, rhs=logf[:, :], start=True, stop=True), reads=[logf, cf], writes=[pP])
            kb.op("dve", lambda e: e.tensor_copy(out=bL[:, :], in_=pP[:, 0:NI]), reads=[pP], writes=[bL])
            kb.op("dve", lambda e: e.tensor_tensor(out=g[:, :], in0=ipre[:, :], in1=bcs[:, :], op=ALU.subtract), reads=[ipre, bcs], writes=[g])
            gb = [self.sb(ps, "mgb%d" % i, [128, 128], F32) for i in range(2)]
            tmpm = [self.sb(ps, "mtmpm%d" % i, [128, 128], F32) for i in range(2)]
            for idx in range(NI):
                gb_ = gb[idx % 2]
                tm_ = tmpm[idx % 2]
                pg = pX[idx % 2]
                kb.op("pool", lambda e, gb_=gb_, idx=idx: e.tensor_copy(out=gb_[:, :], in_=g[:, idx:idx + 1].to_broadcast([128, 128])), reads=[g], writes=[gb_])
                kb.op("pe", lambda e, gb_=gb_, pg=pg: e.matmul(pg[:, 0:128], lhsT=gb_[:, :], rhs=identF, start=True, stop=True), reads=[gb_, cf], writes=[pg])
                kb.op("dve", lambda e, pg=pg, tm_=tm_: e.tensor_tensor(out=tm_[:, :], in0=pg[:, 0:128], in1=neg, op=ALU.add), reads=[pg, cf], writes=[tm_])
                kb.op("dve", lambda e, tm_=tm_, idx=idx: e.reduce_max(out=cm[:, idx:idx + 1], in_=tm_[:, :], axis=AX.X), reads=[tm_], wpart=[cm])
                kb.op("dve", lambda e, pg=pg, idx=idx: e.reduce_max(out=gmax[:, idx:idx + 1], in_=pg[:, 0:128], axis=AX.X), reads=[pg], wpart=[gmax])
            kb.op("dve", lambda e: e.memset(mprev[:, 0:4], 0.0), writes=[mprev])
            for c in range(NTB):
                sl = slice(c * 4, (c + 1) * 4)
                sl2 = slice((c + 1) * 4, (c + 2) * 4)
                kb.op("dve", lambda e, sl=sl: e.tensor_tensor(out=mxL[:, sl], in0=mprev[:, sl], in1=gmax[:, sl], op=ALU.max), reads=[mprev, gmax], writes=[mxL])
                kb.op("dve", lambda e, sl=sl, sl2=sl2: e.tensor_tensor(out=mprev[:, sl2], in0=bL[:, sl], in1=mxL[:, sl], op=ALU.add), reads=[bL, mxL], writes=[mprev])
            mp = mprev
            kb.op("dve", lambda e: e.tensor_tensor(out=decay[:, :], in0=mp[:, 0:NI], in1=mxL[:, :], op=ALU.subtract), reads=[mp, mxL], writes=[decay])
            kb.op("act", lambda e: e.activation(out=decay[:, :], in_=decay[:, :], func=AF.Exp), reads=[decay], writes=[decay])
            kb.op("dve", lambda e: e.tensor_tensor(out=wsc[:, :], in0=g[:, :], in1=mxL[:, :], op=ALU.subtract), reads=[g, mxL], writes=[wsc])
            kb.op("act", lambda e: e.activation(out=wsc[:, :], in_=wsc[:, :], func=AF.Exp), reads=[wsc], writes=[wsc])
            kb.op("dve", lambda e: e.tensor_tensor(out=mx[:, :], in0=mp[:, 0:NI], in1=cm[:, :], op=ALU.max), reads=[mp, cm], writes=[mx])
            kb.op("dve", lambda e: e.tensor_scalar(out=nmx[:, :], in0=mx[:, :], scalar1=-1.0, scalar2=None, op0=ALU.mult), reads=[mx], writes=[nmx])
            kb.op("dve", lambda e: e.tensor_tensor(out=em[:, :], in0=bcs[:, :], in1=mx[:, :], op=ALU.add), reads=[bcs, mx], writes=[em])
            kb.op("act", lambda e: e.activation(out=em[:, :], in_=em[:, :], func=AF.Exp, scale=-1.0), reads=[em], writes=[em])
            CT = [self.sb(ps, "mCT%d" % h, [128, 257], F32) for h in range(4)]
            CTb = [self.sb(ps, "mCTb%d" % h, [128, 257], BF16) for h in range(4)]
            for h in range(4):
                kb.op("pool", lambda e, h=h: e.memset(CT[h][:, :], 0.0), writes=[CT[h]])
                kb.op("pool", lambda e, h=h: e.memset(CTb[h][:, :], 0.0), writes=[CTb[h]])
            nmxb = [self.sb(ps, "mnmxb%d" % i, [128, 128], F32) for i in range(2)]
            Dt = [self.sb(ps, "mDt%d" % i, [128, 128], F32) for i in range(2)]
            Wb = [self.sb(ps, "mWb%d" % i, [128, 128], F32) for i in range(2)]
            SD = [self.sb(ps, "mSD%d" % i, [128, 128], BF16) for i in range(2)]
            qw = [self.sb(ps, "mqw%d" % i, [128, 128], BF16) for i in range(2)]
            dn = [self.sb(ps, "mdn%d" % i, [128, 1], F32) for i in range(2)]
            hm = [self.sb(ps, "mhm%d" % i, [128, 256], F32) for i in range(2)]
            junk = self.sb(ps, "mjunk", [128, 256], F32)
            ss2 = [self.sb(ps, "mss2%d" % i, [128, 1], F32) for i in range(2)]
            t1 = [self.sb(ps, "mt1%d" % i, [128, 256], F32) for i in range(2)]
            vw = [self.sb(ps, "mvw%d" % i, [128, 257], BF16) for i in range(2)]
            soc = [self.sb(ps, "msoc%d" % i, [128, 1024], BF16) for i in range(2)]
            brat = [self.sb(ps, "mbrat%d" % i, [128, 1024], BF16) for i in range(2)]
            braTs = [self.sb(ps, "mbraTs%d" % i, [128, 8, 128], BF16) for i in range(2)]
            for c in range(NTB):
                cs = slice(c * 128, (c + 1) * 128)
                so_ = soc[c % 2]
                bt_ = brat[c % 2]
                kb.dma("sp", so_[:, :], self.so_tm[cs, :], writes=[so_])
                def mstage(h, st):
                    idx = c * 4 + h
                    i2 = idx % 2
                    X, N_, C_ = pX[i2], pN[i2], pC[i2]
                    nb_, D_, W_, S_, q_, dn_, hm_, ss_, t1_, vw_ = nmxb[i2], Dt[i2], Wb[i2], SD[i2], qw[i2], dn[i2], hm[i2], ss2[i2], t1[i2], vw[i2]
                    if st == 0:
                        kb.op("pool", lambda e, nb_=nb_, idx=idx: e.tensor_copy(out=nb_[:, :], in_=nmx[:, idx:idx + 1].to_broadcast([128, 128])), reads=[nmx], writes=[nb_])
                        kb.op("pe", lambda e, X=X, nb_=nb_: e.matmul(X[:, 0:128], lhsT=nb_[:, :], rhs=identF, start=True, stop=False), reads=[nb_, cf], writes=[X])
                        kb.op("pe", lambda e, X=X: e.matmul(X[:, 0:128], lhsT=identF, rhs=negT, start=False, stop=True), reads=[cf], wpart=[X])
                        kb.op("pe", lambda e, X=X, nb_=nb_: e.matmul(X[:, 128:256], lhsT=nb_[:, :], rhs=identF, start=True, stop=True), reads=[nb_, cf], wpart=[X])
                        kb.op("pe", lambda e, X=X, h=h, cs=cs: e.matmul(X[:, 256:384], lhsT=kT[:, h, cs], rhs=qT[:, h, cs], start=True, stop=True), reads=[kT, qT], wpart=[X])
                    if st == 1:
                        kb.op("act", lambda e, X=X, D_=D_, idx=idx: e.activation(out=D_[:, :], in_=X[:, 0:128], func=AF.Exp, bias=g[:, idx:idx + 1], scale=1.0),
                              reads=[X, g], writes=[D_])
                        kb.op("act", lambda e, X=X, W_=W_, idx=idx: e.activation(out=W_[:, :], in_=X[:, 128:256], func=AF.Exp, bias=mp[:, idx:idx + 1], scale=1.0),
                              reads=[X, mp], writes=[W_])
                    if st == 2:
                        kb.op("dve", lambda e, X=X, D_=D_, S_=S_: e.tensor_tensor(out=S_[:, :], in0=X[:, 256:384], in1=D_[:, :], op=ALU.mult), reads=[X, D_], writes=[S_])
                        kb.op("dve", lambda e, W_=W_, q_=q_, h=h, cs=cs: e.tensor_tensor(out=q_[:, :], in0=qT[:, h, cs], in1=W_[:, :], op=ALU.mult), reads=[qT, W_], writes=[q_])
                    if st == 3:
                        kb.op("pe", lambda e, N_=N_, q_=q_, h=h: e.matmul(N_[:, :], lhsT=q_[:, :], rhs=CTb[h][:, :], start=True, stop=False), reads=[q_, CTb[h]], writes=[N_])
                        kb.op("pe", lambda e, N_=N_, S_=S_, c=c, h=h: e.matmul(N_[:, :], lhsT=S_[:, :], rhs=vaug[:, c, h, :], start=False, stop=True), reads=[S_, vaug], wpart=[N_])
                        kb.op("pool", lambda e, vw_=vw_, c=c, h=h, idx=idx: e.tensor_scalar(out=vw_[:, :], in0=vaug[:, c, h, :], scalar1=wsc[:, idx:idx + 1], scalar2=None, op0=ALU.mult),
                              reads=[vaug, wsc], writes=[vw_])
                        kb.op("pe", lambda e, C_=C_, vw_=vw_, c=c, h=h: e.matmul(C_[:, :], lhsT=ktm[:, c, h * 128:(h + 1) * 128], rhs=vw_[:, :], start=True, stop=True),
                              reads=[ktm, vw_], writes=[C_])
                    if st == 4:
                        kb.op("dve", lambda e, C_=C_, h=h, idx=idx: e.scalar_tensor_tensor(out=CT[h][:, :], in0=CT[h][:, :], scalar=decay[:, idx:idx + 1], in1=C_[:, :],
                                                                                          op0=ALU.mult, op1=ALU.add), reads=[CT[h], decay, C_], writes=[CT[h]])
                        kb.op("act", lambda e, h=h: e.activation(out=CTb[h][:, :], in_=CT[h][:, :], func=AF.Identity), reads=[CT[h]], writes=[CTb[h]])

                    if st == 5:
                        kb.op("act", lambda e, N_=N_, dn_=dn_: e.activation(out=dn_[:, :], in_=N_[:, 256:257], func=AF.Abs), reads=[N_], writes=[dn_])
                        kb.op("dve", lambda e, dn_=dn_, idx=idx: e.tensor_tensor(out=dn_[:, :], in0=dn_[:, :], in1=em[:, idx:idx + 1], op=ALU.max), reads=[dn_, em], writes=[dn_])
                        kb.op("dve", lambda e, dn_=dn_: e.reciprocal(out=dn_[:, :], in_=dn_[:, :]), reads=[dn_], writes=[dn_])
                    if st == 6:
                        kb.op("act", lambda e, N_=N_, hm_=hm_, dn_=dn_: e.activation(out=hm_[:, :], in_=N_[:, 0:256], func=AF.Identity, scale=dn_[:, 0:1]), reads=[N_, dn_], writes=[hm_])
                        kb.op("act", lambda e, hm_=hm_, ss_=ss_: e.activation(out=junk[:, :], in_=hm_[:, :], func=AF.Square, accum_out=ss_[:, 0:1]), reads=[hm_], writes=[junk, ss_])
                        kb.op("act", lambda e, ss_=ss_: e.activation(out=ss_[:, :], in_=ss_[:, :], func=AF.Sqrt, bias=EPS, scale=1.0 / 256), reads=[ss_], writes=[ss_])
                        kb.op("dve", lambda e, ss_=ss_: e.reciprocal(out=ss_[:, :], in_=ss_[:, :]), reads=[ss_], writes=[ss_])
                        kb.op("dve", lambda e, hm_=hm_, ss_=ss_, t1_=t1_, h=h: e.scalar_tensor_tensor(out=t1_[:, :], in0=hm_[:, :], scalar=ss_[:, 0:1], in1=gmn[:, h * 256:(h + 1) * 256],
                                                                                                     op0=ALU.mult, op1=ALU.mult), reads=[hm_, ss_, gmn], writes=[t1_])
                        kb.op("pool", lambda e, t1_=t1_, h=h, so_=so_, bt_=bt_: e.tensor_tensor(out=bt_[:, h * 256:(h + 1) * 256], in0=t1_[:, :], in1=so_[:, h * 256:(h + 1) * 256], op=ALU.mult),
                              reads=[t1_, so_], writes=[bt_] if h == 0 else [], wpart=[bt_] if h else [])

                for pair in ((0, 1), (2, 3)):
                    for st in range(7):
                        for h in pair:
                            mstage(h, st)
                bs_ = braTs[c % 2]
                for q in range(8):
                    kb.op("pe", lambda e, q=q, bt_=bt_: e.transpose(pT[:, q * 128:(q + 1) * 128], bt_[:, q * 128:(q + 1) * 128], identB),
                          reads=[bt_, cbf], writes=[pT] if q == 0 else [], wpart=[pT] if q else [])
                kb.op("act", lambda e, bs_=bs_: e.activation(out=bs_[:, :, :], in_=pT[:, :].rearrange("p (q t) -> p q t", q=8), func=AF.Identity), reads=[pT], writes=[bs_])
                kb.dma("sp", self.braT[:, cs].rearrange("(q p) t -> p q t", p=128), bs_[:, :, :], reads=[bs_])

    def phase_mla(self, l):
        nc, kb = self.nc, self.kb
        with ExitStack() as ps:
            ones_b, tri = self.cB(5), self.cB(4)
            cbuf, cfb = self.cb, self.cf
            ql = self.sb(ps, "ql", [128, 4, T], BF16)
            kvl = self.sb(ps, "kvl", [128, 2, T], BF16)
            kr = self.sb(ps, "kr", [64, T], BF16)
            rc = self.sb(ps, "rc", [64, 2 * T + 64], F32)
            gql = self.sb(ps, "gql", [128, 4], F32)
            gkv = self.sb(ps, "gkv", [128, 2], F32)
            gqn = self.sb(ps, "gqn", [128, 2], F32)
            gkn = self.sb(ps, "gkn", [128, 2], F32)
            wuq = self.sb(ps, "wuq", [128, 4, 1536], BF16)
            wukv = self.sb(ps, "wukv", [128, 2, 2048], BF16)
            kb.dma("sp", ql[:, :, :], self.qlatT.rearrange("(kc p) t -> p kc t", p=128), writes=[ql])
            kb.dma("sp", kvl[:, :, :], self.kvlatT.rearrange("(kc p) t -> p kc t", p=128), writes=[kvl])
            kb.dma("sp", kr[:, :], self.krT[:, :], writes=[kr])
            kb.dma("sp", rc[:, :], self.ropec[:, :], writes=[rc])
            kb.dma("sp", gql[:, :], self.gqlT[l], writes=[gql])
            kb.dma("sp", gkv[:, :], self.gkvT[l], writes=[gkv])
            kb.dma("sp", gqn[:, :], self.gqnT[l], writes=[gqn])
            kb.dma("sp", gkn[:, :], self.gknT[l], writes=[gkn])
            kb.dma("pool", wuq[:, :, :], self.w_uq[l].rearrange("(kc p) n -> p kc n", p=128), writes=[wuq])
            kb.dma("pool", wukv[:, :, :], self.w_ukv[l].rearrange("(kc p) n -> p kc n", p=128), writes=[wukv])
            kb.op("dve", lambda e: e.tensor_scalar(out=gqn[:, :], in0=gqn[:, :], scalar1=float(192 ** -0.5), scalar2=None, op0=ALU.mult),
                  reads=[gqn], writes=[gqn])
            cosT = lambda a, b: rc[:, a:b]
            sinT = lambda a, b: rc[:, T + a:T + b]
            Pm = rc[:, 2 * T:2 * T + 64]
            PA, PB, PC, PD, S0, S1, PO, PR = [self.pm(ps, "pa%d" % i, [128, 512]) for i in range(8)]
            sq = [self.sb(ps, "sq%d" % i, [128, 512], BF16) for i in range(4)]
            rsd = [self.sb(ps, "rsd%d" % i, [128, 512], F32) for i in range(2)]
            cnt = {"sq": 0, "rs": 0}

            def rstd_from(pss, nfeat):
                r = rsd[cnt["rs"] % 2]
                cnt["rs"] += 1
                kb.op("act", lambda e: e.activation(out=r[:, :], in_=pss[:, :], func=AF.Sqrt, bias=EPS, scale=1.0 / nfeat), reads=[pss], writes=[r])
                kb.op("dve", lambda e: e.reciprocal(out=r[:, :], in_=r[:, :]), reads=[r], writes=[r])
                return r

            def fm_rmsnorm(src, KC, nfeat, gcol, dst):
                for tt in range(4):
                    sl = slice(tt * 512, (tt + 1) * 512)
                    for kc in range(KC):
                        q_ = sq[cnt["sq"] % 4]
                        cnt["sq"] += 1
                        kb.op("dve", lambda e, q_=q_, kc=kc: e.tensor_tensor(out=q_[:, :], in0=src[:, kc, sl], in1=src[:, kc, sl], op=ALU.mult),
                              reads=[src], writes=[q_])
                        kb.op("pe", lambda e, q_=q_, kc=kc: e.matmul(PC[:, :], lhsT=ones_b, rhs=q_[:, :], start=(kc == 0), stop=(kc == KC - 1)),
                              reads=[q_, cbuf], writes=[PC] if kc == 0 else [], wpart=[PC] if kc else [])
                    r = rstd_from(PC, nfeat)
                    for kc in range(KC):
                        kb.op("dve", lambda e, kc=kc, r=r: e.scalar_tensor_tensor(out=dst[:, kc, sl], in0=src[:, kc, sl], scalar=gcol[:, kc:kc + 1],
                                                                                in1=r[:, :], op0=ALU.mult, op1=ALU.mult),
                              reads=[src, gcol, r], wpart=[dst])

            qln = self.sb(ps, "qln", [128, 4, T], BF16)
            kvn = self.sb(ps, "kvn", [128, 2, T], BF16)
            fm_rmsnorm(ql, 4, 512, gql, qln)
            fm_rmsnorm(kvl, 2, 256, gkv, kvn)
            krr = self.sb(ps, "krr", [64, T], F32)
            sqkr = self.sb(ps, "sqkr", [64, T], BF16)
            krg = self.sb(ps, "krg", [64, 512], F32)
            ta = self.sb(ps, "ta", [64, 512], F32)
            tb_ = self.sb(ps, "tb_", [64, 512], F32)
            kb.op("dve", lambda e: e.tensor_tensor(out=sqkr[:, :], in0=kr[:, :], in1=kr[:, :], op=ALU.mult), reads=[kr], writes=[sqkr])

            def rope_apply(xf, sl0, sl1, out_ap, out_buf):
                kb.op("pe", lambda e: e.matmul(PD[0:64, :], lhsT=Pm, rhs=xf[:, :], start=True, stop=True), reads=[xf, rc], writes=[PD])
                kb.op("dve", lambda e: e.tensor_tensor(out=ta[:, :], in0=xf[:, :], in1=cosT(sl0, sl1), op=ALU.mult), reads=[xf, rc], writes=[ta])
                kb.op("dve", lambda e: e.tensor_tensor(out=tb_[:, :], in0=PD[0:64, :], in1=sinT(sl0, sl1), op=ALU.mult), reads=[PD, rc], writes=[tb_])
                kb.op("pool", lambda e: e.tensor_tensor(out=out_ap, in0=ta[:, :], in1=tb_[:, :], op=ALU.add), reads=[ta, tb_], wpart=[out_buf])

            for tt in range(4):
                a, b = tt * 512, (tt + 1) * 512
                kb.op("dve", lambda e, a=a, b=b: e.tensor_scalar(out=krg[:, :], in0=kr[:, a:b], scalar1=gkn[0:64, 1:2], scalar2=None, op0=ALU.mult),
                      reads=[kr, gkn], writes=[krg])
                rope_apply(krg, a, b, krr[:, a:b], krr)

            QnT = [self.sb(ps, "QnT%d" % i, [128, T], BF16) for i in range(2)]
            QrT = [self.sb(ps, "QrT%d" % i, [64, T], BF16) for i in range(2)]
            KnT = [self.sb(ps, "KnT%d" % i, [128, T], BF16) for i in range(2)]
            KrT = [self.sb(ps, "KrT%d" % i, [64, T], BF16) for i in range(2)]
            Vh = [self.sb(ps, "Vh%d" % i, [128, NTB, 128], BF16) for i in range(2)]
            qrf = self.sb(ps, "qrf", [64, 512], F32)
            pt = [self.sb(ps, "pt%d" % i, [128, 512], BF16) for i in range(3)]
            rr = self.sb(ps, "rr", [128, 512], F32)
            ob = [self.sb(ps, "ob%d" % i, [128, 512], BF16) for i in range(2)]
            npt = 0
            nob = 0
            for h in range(8):
                Qn, Qr, Kn, Kr, V = QnT[h % 2], QrT[h % 2], KnT[h % 2], KrT[h % 2], Vh[h % 2]
                for tt in range(4):
                    a, b = tt * 512, (tt + 1) * 512
                    s1, s2, s3 = sq[cnt["sq"] % 4], sq[(cnt["sq"] + 1) % 4], sq[(cnt["sq"] + 2) % 4]
                    cnt["sq"] += 3
                    rq, rk = rsd[0], rsd[1]
                    ones64 = self.cb[0:64, 640:768]
                    for kc in range(4):
                        kb.op("pe", lambda e, kc=kc: e.matmul(PA[:, :], lhsT=wuq[:, kc, h * 192:h * 192 + 128], rhs=qln[:, kc, a:b],
                                                             start=(kc == 0), stop=(kc == 3)), reads=[wuq, qln], writes=[PA] if kc == 0 else [], wpart=[PA] if kc else [])
                    for kc in range(4):
                        kb.op("pe", lambda e, kc=kc: e.matmul(PB[0:64, :], lhsT=wuq[:, kc, h * 192 + 128:h * 192 + 192], rhs=qln[:, kc, a:b],
                                                             start=(kc == 0), stop=(kc == 3)), reads=[wuq, qln], writes=[PB] if kc == 0 else [], wpart=[PB] if kc else [])
                    for kc in range(2):
                        kb.op("pe", lambda e, kc=kc: e.matmul(S0[:, :], lhsT=wukv[:, kc, h * 256:h * 256 + 128], rhs=kvn[:, kc, a:b],
                                                             start=(kc == 0), stop=(kc == 1)), reads=[wukv, kvn], writes=[S0] if kc == 0 else [], wpart=[S0] if kc else [])
                    kb.op("act", lambda e: e.activation(out=s1[:, :], in_=PA[:, :], func=AF.Square), reads=[PA], writes=[s1])
                    kb.op("act", lambda e: e.activation(out=s2[0:64, :], in_=PB[0:64, :], func=AF.Square), reads=[PB], writes=[s2])
                    kb.op("act", lambda e: e.activation(out=s3[:, :], in_=S0[:, :], func=AF.Square), reads=[S0], writes=[s3])
                    kb.op("pe", lambda e: e.matmul(PC[:, :], lhsT=ones_b, rhs=s1[:, :], start=True, stop=False), reads=[s1, cbuf], writes=[PC])
                    kb.op("pe", lambda e: e.matmul(PC[:, :], lhsT=ones64, rhs=s2[0:64, :], start=False, stop=True), reads=[s2, cbuf], wpart=[PC])
                    kb.op("pe", lambda e: e.matmul(S1[:, :], lhsT=ones_b, rhs=s3[:, :], start=True, stop=False), reads=[s3, cbuf], writes=[S1])
                    kb.op("pe", lambda e: e.matmul(S1[:, :], lhsT=ones64, rhs=sqkr[:, a:b], start=False, stop=True), reads=[sqkr, cbuf], wpart=[S1])
                    kb.op("act", lambda e: e.activation(out=rq[:, :], in_=PC[:, :], func=AF.Sqrt, bias=EPS, scale=1.0 / 192), reads=[PC], writes=[rq])
                    kb.op("act", lambda e: e.activation(out=rk[:, :], in_=S1[:, :], func=AF.Sqrt, bias=EPS, scale=1.0 / 192), reads=[S1], writes=[rk])
                    kb.op("dve", lambda e: e.reciprocal(out=rq[:, :], in_=rq[:, :]), reads=[rq], writes=[rq])
                    kb.op("dve", lambda e: e.reciprocal(out=rk[:, :], in_=rk[:, :]), reads=[rk], writes=[rk])
                    kb.op("dve", lambda e: e.scalar_tensor_tensor(out=Qn[:, a:b], in0=PA[:, :], scalar=gqn[:, 0:1], in1=rq[:, :], op0=ALU.mult, op1=ALU.mult),
                          reads=[PA, gqn, rq], wpart=[Qn])
                    kb.op("dve", lambda e: e.scalar_tensor_tensor(out=qrf[:, :], in0=PB[0:64, :], scalar=gqn[0:64, 1:2], in1=rq[0:64, :], op0=ALU.mult, op1=ALU.mult),
                          reads=[PB, gqn, rq], writes=[qrf])
                    kb.op("dve", lambda e: e.scalar_tensor_tensor(out=Kn[:, a:b], in0=S0[:, :], scalar=gkn[:, 0:1], in1=rk[:, :], op0=ALU.mult, op1=ALU.mult),
                          reads=[S0, gkn, rk], wpart=[Kn])
                    kb.op("pool", lambda e: e.tensor_tensor(out=Kr[:, a:b], in0=krr[:, a:b], in1=rk[0:64, :], op=ALU.mult), reads=[krr, rk], wpart=[Kr])
                    rope_apply(qrf, a, b, Qr[:, a:b], Qr)
                for g4 in range(4):
                    for j in range(4):
                        tb = g4 * 4 + j
                        for kc in range(2):
                            kb.op("pe", lambda e, kc=kc, tb=tb, j=j: e.matmul(PB[:, j * 128:(j + 1) * 128], lhsT=kvn[:, kc, tb * 128:(tb + 1) * 128],
                                                                             rhs=wukv[:, kc, h * 256 + 128:h * 256 + 256], start=(kc == 0), stop=(kc == 1)),
                                  reads=[kvn, wukv], writes=[PB] if (j == 0 and kc == 0) else [], wpart=[] if (j == 0 and kc == 0) else [PB])
                    kb.op("act", lambda e, g4=g4: e.activation(out=V[:, g4 * 4:(g4 + 1) * 4, :], in_=PB[:, :].rearrange("p (j d) -> p j d", j=4), func=AF.Identity),
                          reads=[PB], wpart=[V])
                for qt in range(4):
                    q0 = qt * 512
                    nkb = 4 * (qt + 1)
                    def emit_S(kbi):
                        off = max(0, kbi * 128 - q0)
                        S = S0 if kbi % 2 == 0 else S1
                        ks = slice(kbi * 128, (kbi + 1) * 128)
                        kb.op("pe", lambda e, S=S, off=off, ks=ks: e.matmul(S[:, off:512], lhsT=Kn[:, ks], rhs=Qn[:, q0 + off:q0 + 512], start=True, stop=False),
                              reads=[Kn, Qn], writes=[S])
                        kb.op("pe", lambda e, S=S, off=off, ks=ks: e.matmul(S[:, off:512], lhsT=Kr[:, ks], rhs=Qr[:, q0 + off:q0 + 512], start=False, stop=True),
                              reads=[Kr, Qr], wpart=[S])

                    emit_S(0)
                    for kbi in range(nkb):
                        if kbi + 1 < nkb:
                            emit_S(kbi + 1)
                        off = max(0, kbi * 128 - q0)
                        S = S0 if kbi % 2 == 0 else S1
                        p_ = pt[npt % 3]
                        npt += 1
                        kb.op("act", lambda e, S=S, off=off, p_=p_: e.activation(out=p_[:, off:512], in_=S[:, off:512], func=AF.Exp), reads=[S], writes=[p_])
                        if kbi * 128 >= q0:
                            kb.op("pool", lambda e, off=off, p_=p_: e.tensor_tensor(out=p_[:, off:off + 128], in0=p_[:, off:off + 128], in1=tri, op=ALU.mult),
                                  reads=[p_, cbuf], writes=[p_])
                        first, last = (kbi == 0), (kbi == nkb - 1)
                        kb.op("pe", lambda e, off=off, p_=p_, kbi=kbi, first=first, last=last: e.matmul(PO[:, off:512], lhsT=V[:, kbi, :], rhs=p_[:, off:512], start=first, stop=last),
                              reads=[V, p_], writes=[PO] if first else [], wpart=[] if first else [PO])
                        kb.op("pe", lambda e, off=off, p_=p_, first=first, last=last: e.matmul(PR[:, off:512], lhsT=ones_b, rhs=p_[:, off:512], start=first, stop=last),
                              reads=[p_, cbuf], writes=[PR] if first else [], wpart=[] if first else [PR])
                    o_ = ob[nob % 2]
                    nob += 1
                    kb.op("dve", lambda e: e.reciprocal(out=rr[:, :], in_=PR[:, :]), reads=[PR], writes=[rr])
                    kb.op("dve", lambda e, o_=o_: e.tensor_tensor(out=o_[:, :], in0=PO[:, :], in1=rr[:, :], op=ALU.mult), reads=[PO, rr], writes=[o_])
                    kb.dma("sp", self.brbT[h * 128:(h + 1) * 128, q0:q0 + 512], o_[:, :], reads=[o_])

    def phase_pool(self, l):
        nc, kb = self.nc, self.kb
        WIN = (2, 4, 8, 16)
        with ExitStack() as ps:
            inv = [self.sb(ps, "inv%d" % g, [128, T], F32) for g in range(4)]
            for g in range(4):
                kb.dma("sp", inv[g][:, :], self.poolinv[g].partition_broadcast(128), writes=[inv[g]])
            bp = self.sb(ps, "bp", [128, 8], F32)
            sp_ = self.sb(ps, "spl", [128, 8], F32)
            kb.dma("sp", bp[:, :], self.bpT[l], writes=[bp])
            kb.dma("sp", sp_[:, :], self.spT[l], writes=[sp_])
            kb.op("dve", lambda e: e.tensor_tensor(out=bp[:, :], in0=bp[:, :], in1=sp_[:, :], op=ALU.mult), reads=[bp, sp_], writes=[bp])
            pooled = self.sb(ps, "pooled", [128, 8, T], BF16)
            ub = [self.sb(ps, "ub%d" % i, [128, T], BF16) for i in range(2)]
            u32 = [self.sb(ps, "u32%d" % i, [128, T], F32) for i in range(2)]
            sa = [self.sb(ps, "sa%d" % i, [128, T], F32) for i in range(2)]
            for cc in range(8):
                g = cc // 2
                u_, f_ = ub[cc % 2], u32[cc % 2]
                kb.dma("sp", u_[:, :], self.upT[cc * 128:(cc + 1) * 128, :], writes=[u_])
                kb.op("act", lambda e, u_=u_, f_=f_: e.activation(out=f_[:, :], in_=u_[:, :], func=AF.Identity), reads=[u_], writes=[f_])
                cur = f_
                step = 1
                i = 0
                while step < WIN[g]:
                    nxt = sa[i % 2]
                    i += 1
                    kb.op("pool", lambda e, cur=cur, nxt=nxt, step=step: e.tensor_copy(out=nxt[:, 0:step], in_=cur[:, 0:step]),
                          reads=[cur], writes=[nxt])
                    kb.op("dve", lambda e, cur=cur, nxt=nxt, step=step: e.tensor_tensor(out=nxt[:, step:T], in0=cur[:, step:T], in1=cur[:, 0:T - step], op=ALU.add),
                          reads=[cur], wpart=[nxt])
                    cur = nxt
                    step *= 2
                kb.op("dve", lambda e, cur=cur, g=g: e.tensor_tensor(out=cur[:, :], in0=cur[:, :], in1=inv[g][:, :], op=ALU.mult),
                      reads=[cur, inv[g]], writes=[cur])
                kb.op("dve", lambda e, cur=cur, f_=f_, cc=cc: e.tensor_tensor(out=pooled[:, cc, :], in0=cur[:, :], in1=f_[:, :], op=ALU.subtract),
                      reads=[cur, f_], wpart=[pooled])
            wp = self.sb(ps, "wp", [128, 8, 256], BF16)
            kb.dma("pool", wp[:, :, :], self.w_pool[l].rearrange("g (kc p) e -> p (g kc) e", p=128), writes=[wp])
            pacc = [self.pm(ps, "ppool%d" % i, [128, 512]) for i in range(8)]
            ot = [self.sb(ps, "pot%d" % i, [128, T], BF16) for i in range(2)]
            n = 0
            for g in range(4):
                for eb in range(2):
                    j = g * 2 + eb
                    banks = pacc[(n % 2) * 4:(n % 2) * 4 + 4]
                    o = ot[n % 2]
                    n += 1
                    for kc in range(2):
                        for tt in range(4):
                            p = banks[tt]
                            kb.op("pe", lambda e, p=p, g=g, kc=kc, eb=eb, tt=tt: e.matmul(
                                p[:, :], lhsT=wp[:, g * 2 + kc, eb * 128:(eb + 1) * 128], rhs=pooled[:, g * 2 + kc, tt * 512:(tt + 1) * 512],
                                start=(kc == 0), stop=(kc == 1)), reads=[wp, pooled], writes=[p] if kc == 0 else [], wpart=[p] if kc else [])
                    for tt in range(4):
                        p = banks[tt]
                        kb.op("act", lambda e, p=p, o=o, tt=tt, j=j: e.activation(
                            out=o[:, tt * 512:(tt + 1) * 512], in_=p[:, :], func=AF.Identity, bias=bp[:, j:j + 1], scale=sp_[:, j:j + 1]),
                            reads=[p, bp, sp_], writes=[o] if tt == 0 else [], wpart=[o] if tt else [])
                    kb.dma("sp", self.brcT[j * 128:(j + 1) * 128, :], o[:, :], reads=[o])

    def phase_merge(self, l, xin):
        nc, kb = self.nc, self.kb
        with ExitStack() as ps:
            br = [self.sb(ps, "br%d" % j, [128, 8, T], BF16) for j in range(3)]
            for j, src in enumerate((self.braT, self.brbT, self.brcT)):
                kb.dma("sp", br[j][:, :, :], src.rearrange("(kc p) t -> p kc t", p=128), writes=[br[j]])
            ws = self.WStream(self, ps, "wb", 8, 512, 6)
            pacc = [self.pm(ps, "pmg%d" % i, [128, 512]) for i in range(8)]
            gt = [self.sb(ps, "gt%d" % i, [128, T], BF16) for i in range(3)]
            acc = [self.sb(ps, "macc%d" % i, [128, T], F32) for i in range(2)]
            tmp = [self.sb(ps, "mtmp%d" % i, [128, 512], F32) for i in range(3)]
            mo = [self.sb(ps, "mo%d" % i, [128, T], BF16) for i in range(2)]
            n = 0
            ng = 0
            nt = 0
            def wl(dt_):
                return [ws.load(self.w_branch[l, j][:, dt_ * 512:(dt_ + 1) * 512], 512) for j in range(3)]

            pend = {0: wl(0)}
            for dt in range(4):
                if dt + 1 < 4:
                    pend[dt + 1] = wl(dt + 1)
                wts = pend.pop(dt)
                for f in range(4):
                    dblk = dt * 4 + f
                    a_ = acc[dblk % 2]
                    m_ = mo[dblk % 2]
                    for j in range(3):
                        banks = pacc[(n % 2) * 4:(n % 2) * 4 + 4]
                        n += 1
                        g_ = gt[ng % 3]
                        ng += 1
                        r0 = j * D + dblk * 128
                        kb.dma("sp", g_[:, :], self.gatesT[r0:r0 + 128, :], writes=[g_])
                        for k in range(8):
                            for tt in range(4):
                                p = banks[tt]
                                kb.op("pe", lambda e, p=p, k=k, tt=tt, f=f, j=j, w=wts[j]: e.matmul(
                                    p[:, :], lhsT=w[:, k, f * 128:(f + 1) * 128], rhs=br[j][:, k, tt * 512:(tt + 1) * 512],
                                    start=(k == 0), stop=(k == 7)), reads=[wts[j], br[j]], writes=[p] if k == 0 else [], wpart=[p] if k else [])
                        for tt in range(4):
                            p = banks[tt]
                            sl = slice(tt * 512, (tt + 1) * 512)
                            if j == 0:
                                kb.op("dve", lambda e, p=p, g_=g_, a_=a_, sl=sl: e.tensor_tensor(out=a_[:, sl], in0=p[:, :], in1=g_[:, sl], op=ALU.mult),
                                      reads=[p, g_], writes=[a_] if tt == 0 else [], wpart=[a_] if tt else [])
                            else:
                                t_ = tmp[nt % 3]
                                nt += 1
                                kb.op("dve", lambda e, p=p, g_=g_, t_=t_, sl=sl: e.tensor_tensor(out=t_[:, :], in0=p[:, :], in1=g_[:, sl], op=ALU.mult),
                                      reads=[p, g_], writes=[t_])
                                if j == 1:
                                    kb.op("pool", lambda e, a_=a_, t_=t_, sl=sl: e.tensor_tensor(out=a_[:, sl], in0=a_[:, sl], in1=t_[:, :], op=ALU.add),
                                          reads=[a_, t_], wpart=[a_])
                                else:
                                    kb.op("pool", lambda e, a_=a_, t_=t_, m_=m_, sl=sl: e.tensor_tensor(out=m_[:, sl], in0=a_[:, sl], in1=t_[:, :], op=ALU.add),
                                          reads=[a_, t_], writes=[m_] if tt == 0 else [], wpart=[m_] if tt else [])
                    kb.dma("act", self.mergedT[dblk * 128:(dblk + 1) * 128, :], m_[:, :], reads=[m_])
        kb.barrier()
        with ExitStack() as ps:
            mT = self.sb(ps, "mT", [128, 16, T], BF16)
            kb.dma("sp", mT[:, :, :], self.mergedT.rearrange("(kc p) t -> p kc t", p=128), writes=[mT])
            xdst = self.xmid
            self.gemm_resid(ps, mT, 16, 0, NTB, self.w_out[l], self.mod_d[l, 2 * D:3 * D], xin, xdst, 0, "wo")

    def gemm_resid(self, ps, aT, KC, tb0, ntb, W, gate_row, xsrc, xdst, row0, tag):
        nc, kb = self.nc, self.kb
        gb = self.sb(ps, tag + "gb", [128, D], F32)
        kb.dma("sp", gb[:, :], gate_row.partition_broadcast(128), writes=[gb])
        nsp = 2 if KC > 16 else 1
        kh = KC // nsp
        ws = self.WStream(self, ps, tag + "w", kh, 512, 3 * nsp if nsp == 1 else 4)
        pacc = [self.pm(ps, tag + "p%d" % i, [128, 512]) for i in range(4)]
        xr = [self.sb(ps, tag + "xr%d" % i, [128, 512], F32) for i in range(3)]
        yy = [self.sb(ps, tag + "yy%d" % i, [128, 512], F32) for i in range(3)]
        n = 0
        tiles = [(ct, tb) for ct in range(4) for tb in range(ntb)]

        def xload(i):
            ct_, tb_ = tiles[i]
            rr_ = row0 + tb_ * 128
            kb.dma("sp", xr[i % 3][:, :], xsrc[rr_:rr_ + 128, ct_ * 512:(ct_ + 1) * 512], writes=[xr[i % 3]])

        xload(0)
        xload(1)
        def wload(ct_):
            return [ws.load(W[h * kh * 128:(h + 1) * kh * 128, ct_ * 512:(ct_ + 1) * 512], 512) for h in range(nsp)]

        pend = {0: wload(0)}
        for ct in range(4):
            c0 = ct * 512
            if ct + 1 < 4:
                pend[ct + 1] = wload(ct + 1)
            wts = pend.pop(ct)
            for tb in range(ntb):
                p = pacc[n % 4]
                x_ = xr[n % 3]
                y_ = yy[n % 3]
                if n + 2 < len(tiles):
                    xload(n + 2)
                n += 1
                r0 = row0 + tb * 128
                for k in range(KC):
                    w = wts[k // kh]
                    kb.op("pe", lambda e, p=p, k=k, tb=tb, w=w: e.matmul(
                        p[:, :], lhsT=aT[:, k, tb * 128:(tb + 1) * 128], rhs=w[:, k % kh, :], start=(k == 0), stop=(k == KC - 1)),
                        reads=[w, aT], writes=[p] if k == 0 else [], wpart=[p] if k else [])
                kb.op("dve", lambda e, p=p, y_=y_, c0=c0: e.tensor_tensor(out=y_[:, :], in0=p[:, :], in1=gb[:, c0:c0 + 512], op=ALU.mult),
                      reads=[p, gb], writes=[y_])
                kb.op("pool", lambda e, y_=y_, x_=x_: e.tensor_tensor(out=y_[:, :], in0=y_[:, :], in1=x_[:, :], op=ALU.add),
                      reads=[y_, x_], writes=[y_])
                kb.dma("sp", xdst[r0:r0 + 128, c0:c0 + 512], y_[:, :], reads=[y_])

    def phase_ffn(self, l, xo):
        nc, kb = self.nc, self.kb
        NH = 2
        TH = T // NH
        for th in range(NH):
            with ExitStack() as ps:
                actT = self.sb(ps, "actT", [128, 44, TH], BF16)
                with ExitStack() as ps2:
                    h2T = self.sb(ps2, "h2T", [128, 16, TH], BF16)
                    h2T.alt = h2T
                    with ExitStack() as ps3:
                        gs, sh = self.load_mod_cols(ps3, l, 2, self.g2T, "m2")
                        self.norm_to_hT(ps3, self.xmid, th * (TH // 128), TH // 128, h2T, gs, sh, "n2")
                    kb.barrier()
                    ws = self.WStream(self, ps2, "wf", 16, 512, 4)
                    pacc = [self.pm(ps2, "pf%d" % i, [128, 512]) for i in range(8)]
                    sg = [self.sb(ps2, "sg%d" % i, [128, 512], F32) for i in range(3)]
                    n = 0
                    ns = 0
                    W = self.w_ffn_in[l]
                    for ft in range(FFN // 512):
                        wg = ws.load(W[:, ft * 512:(ft + 1) * 512], 512)
                        wu = ws.load(W[:, FFN + ft * 512:FFN + (ft + 1) * 512], 512)
                        for f in range(4):
                            banks = pacc[(n % 2) * 4:(n % 2) * 4 + 4]
                            n += 1
                            for gi, w in enumerate((wg, wu)):
                                for k in range(16):
                                    for tt in range(TH // 512):
                                        p = banks[gi * 2 + tt]
                                        kb.op("pe", lambda e, p=p, k=k, tt=tt, f=f, w=w: e.matmul(
                                            p[:, :], lhsT=w[:, k, f * 128:(f + 1) * 128], rhs=h2T[:, k, tt * 512:(tt + 1) * 512],
                                            start=(k == 0), stop=(k == 15)), reads=[w, h2T, h2T.alt], writes=[p] if k == 0 else [], wpart=[p] if k else [])
                            for tt in range(TH // 512):
                                s_ = sg[ns % 3]
                                ns += 1
                                kb.op("act", lambda e, p=banks[tt], s_=s_: e.activation(out=s_[:, :], in_=p[:, :], func=AF.Silu), reads=[banks[tt]], writes=[s_])
                                kb.op("dve", lambda e, p=banks[2 + tt], s_=s_, fb=ft * 4 + f, tt=tt: e.tensor_tensor(
                                    out=actT[:, fb, tt * 512:(tt + 1) * 512], in0=p[:, :], in1=s_[:, :], op=ALU.mult),
                                    reads=[banks[2 + tt], s_], wpart=[actT])
                kb.barrier()
                with ExitStack() as ps2:
                    self.gemm_resid(ps2, actT, 44, 0, TH // 128, self.w_ffn_out[l], self.mod_d[l, 5 * D:6 * D], self.xmid, xo, th * TH, "fo")
            kb.barrier()


def _consts():
    i = np.arange(128)
    ident = np.eye(128, dtype=np.float32)
    U = (i[:, None] <= i[None, :]).astype(np.float32)
    negT = np.where(i[:, None] <= i[None, :], 0.0, NEG).astype(np.float32)
    neg = negT.T.copy()
    tri = U.copy()
    ones = np.ones((128, 128), np.float32)
    cst = np.concatenate([ident, U, negT, neg, tri, ones], axis=1)
    pos = np.arange(T, dtype=np.float32)
    freqs = (np.float32(10000.0) ** (-np.arange(0, 64, 2, dtype=np.float32) / np.float32(64))).astype(np.float32)
    ang = pos[:, None] * freqs[None, :]
    cos = np.cos(ang).astype(np.float32).T
    sin = np.sin(ang).astype(np.float32).T
    cosT = np.concatenate([cos, cos], axis=0)
    sinT = np.concatenate([sin, sin], axis=0)
    P = np.zeros((64, 64), np.float32)
    for m in range(32):
        P[m + 32, m] = -1.0
        P[m, m + 32] = 1.0
    ropec = np.concatenate([cosT, sinT, P], axis=1).astype(np.float32)
    tt = np.arange(T)
    poolinv = np.stack([1.0 / np.minimum(tt + 1, w) for w in (2, 4, 8, 16)]).astype(np.float32)
    return cst, ropec, poolinv


def _colT(v, n):
    return np.ascontiguousarray(v.reshape(n, 128).T)


def make_in_maps(inp, cores):
    f = lambda a: np.ascontiguousarray(np.asarray(a, dtype=np.float32))
    cst, ropec, poolinv = _consts()
    g = {k: f(v) for k, v in inp.items()}

    def colT_l(a, n):
        return np.stack([_colT(a[l], n) for l in range(DEPTH)])

    def qk192(a):
        o = np.zeros((DEPTH, 128, 2), np.float32)
        o[:, :, 0] = a[:, 0:128]
        o[:, 0:64, 1] = a[:, 128:192]
        return o

    shared = {
        "w_ada": g["w_ada"], "b_ada": g["b_ada"], "g1T": colT_l(g["g_norm1"], 16), "g2T": colT_l(g["g_norm2"], 16),
        "w_in": g["w_in"], "b_mgate": g["b_mgate"].reshape(DEPTH, 8), "g_mnorm": g["g_mnorm"].reshape(DEPTH, 1024),
        "gqlT": colT_l(g["g_qlat"], 4), "w_uq": g["w_uq"], "gkvT": colT_l(g["g_kvlat"], 2), "w_ukv": g["w_ukv"],
        "gqnT": qk192(g["g_qn"]), "gknT": qk192(g["g_kn"]), "w_pool": g["w_pool"],
        "bpT": colT_l(g["b_pool"].reshape(DEPTH, 1024), 8), "spT": colT_l(g["s_pool"], 8),
        "w_branch": g["w_branch"], "w_out": g["w_out"], "w_ffn_in": g["w_ffn_in"], "w_ffn_out": g["w_ffn_out"],
        "cst": cst, "ropec": ropec, "poolinv": poolinv,
    }
    maps = []
    for b in cores:
        m = dict(shared)
        m["x"] = g["x"][b]
        m["cT"] = _colT(g["c"][b], 16)
        maps.append(m)
    return maps


_CACHE = {}


def kernel(**inputs):
    cores = list(range(8))
    if "nc" not in _CACHE:
        _CACHE["nc"] = Prog().build()
    nc = _CACHE["nc"]
    maps = make_in_maps(inputs, cores)
    res = run_bass_kernel_spmd(nc, maps, core_ids=cores)
    return np.stack([np.asarray(r["out"], dtype=np.float32) for r in res.results], axis=0)
```

```python
import types
import numpy as np
from contextlib import ExitStack
import concourse.bass as bass
import concourse.mybir as mybir
from concourse.bass_utils import run_bass_kernel_spmd

F32 = mybir.dt.float32
BF16 = mybir.dt.bfloat16
AF = mybir.ActivationFunctionType
ALU = mybir.AluOpType
AX = mybir.AxisListType

D = 2048
T = 2048
DEPTH = 2
NTB = T // 128
FFN = 5632
IN_W = 11080
EPS = 1e-6
C_QM, C_KM, C_VM, C_OM, C_IF, C_QL, C_KV, C_KR, C_UP, C_GT = 0, 512, 1024, 2048, 3072, 3080, 3592, 3848, 3912, 4936
NEG = -30000.0


class Buf:
    __slots__ = ("t", "w", "r", "name", "alt")

    def __init__(self, t, name=""):
        self.t = t
        self.w = {}
        self.r = {}
        self.name = name
        self.alt = None

    def __getitem__(self, k):
        return self.t[k]


def _freeze(fn):
    if fn.__closure__ is None:
        return fn
    cells = []
    for c in fn.__closure__:
        try:
            cells.append(types.CellType(c.cell_contents))
        except ValueError:
            cells.append(c)
    return types.FunctionType(fn.__code__, fn.__globals__, fn.__name__, fn.__defaults__, tuple(cells))


class Op:
    __slots__ = ("fn", "waits", "dma")

    def __init__(self, fn, waits, dma=None):
        self.fn = fn
        self.waits = waits
        self.dma = dma


ENGS = ["pe", "act", "dve", "pool", "sp"]


class KB:
    def __init__(self, nc, es, n_dma=24):
        self.nc = nc
        self.ops = {e: [] for e in ENGS}
        self.known = {e: {} for e in ENGS}
        self.sigset = {e: set() for e in ENGS}
        self.lastc = {e: 0 for e in ENGS}
        self.n_dma = n_dma
        self.dma_val = [0] * n_dma
        self.dma_rr2 = {True: 0, False: 0}
        self.csem = {e: es.enter_context(nc.semaphore("c_" + e)) for e in ENGS if e != "sp"}
        self.dsem = [es.enter_context(nc.semaphore("d%d" % i)) for i in range(n_dma)]

    def _collect(self, eng, reads, writes, wpart=()):
        need = {}
        own = ("c", eng)
        for b in reads:
            for k, v in b.w.items():
                if v > need.get(k, 0):
                    need[k] = v
        for b in writes:
            for dct in (b.w, b.r):
                for k, v in dct.items():
                    if v > need.get(k, 0):
                        need[k] = v
        for b in wpart:
            for k, v in b.w.items():
                if k == own:
                    continue
                if v > need.get(k, 0):
                    need[k] = v
        if eng == "pe":
            need.pop(own, None)
        waits = []
        kn = self.known[eng]
        for k, v in need.items():
            if kn.get(k, 0) >= v:
                continue
            kn[k] = v
            waits.append((k, v))
            if k[0] == "c":
                self.sigset[k[1]].add(v)
        return waits

    def op(self, eng, fn, reads=(), writes=(), wpart=()):
        waits = self._collect(eng, reads, writes, wpart)
        self.ops[eng].append(Op(_freeze(fn), waits))
        idx = len(self.ops[eng])
        self.lastc[eng] = idx
        key = ("c", eng)
        for b in reads:
            b.r[key] = idx
        for b in writes:
            b.w = {key: idx}
            b.r = {}
        for b in wpart:
            b.w[key] = idx
        return idx

    def dma(self, q, out_ap, in_ap, reads=(), writes=(), wpart=(), slow=False):
        lo, hi = (0, 6) if q == "pool" else (6, self.n_dma)
        k = lo + self.dma_rr2[q == "pool"] % (hi - lo)
        self.dma_rr2[q == "pool"] += 1
        prev = self.dma_val[k]
        waits = self._collect(q, reads, writes, wpart)
        key = ("d", k)
        if prev > 0 and self.known[q].get(key, 0) < prev:
            waits.append((key, prev))
            self.known[q][key] = prev
        new = prev + 16
        self.dma_val[k] = new
        if slow:
            fn = lambda e: e.dma_start(out=out_ap, in_=in_ap, allow_slow_non_contiguous=True)
        else:
            fn = lambda e: e.dma_start(out=out_ap, in_=in_ap)
        self.ops[q].append(Op(fn, waits, dma=k))
        for b in reads:
            b.r[key] = new
        for b in writes:
            b.w = {key: new}
            b.r = {}
        for b in wpart:
            b.w[key] = new

    def barrier(self):
        outstanding = {}
        for e in ENGS:
            if e != "sp" and self.lastc[e] > 0:
                outstanding[("c", e)] = self.lastc[e]
        for k in range(self.n_dma):
            if self.dma_val[k] > 0:
                outstanding[("d", k)] = self.dma_val[k]
        for e in ENGS:
            waits = []
            kn = self.known[e]
            for k, v in outstanding.items():
                if kn.get(k, 0) >= v:
                    continue
                kn[k] = v
                waits.append((k, v))
                if k[0] == "c":
                    self.sigset[k[1]].add(v)
            if waits:
                self.ops[e].append(Op(lambda eng: eng.nop(), waits))

    def emit(self):
        nc = self.nc
        rank = {}
        for e in ENGS:
            rank[e] = {idx: i + 1 for i, idx in enumerate(sorted(self.sigset[e]))}

        def run(e, engobj):
            rk = rank[e]
            for i, op in enumerate(self.ops[e], start=1):
                for (k, v) in op.waits:
                    if k[0] == "c":
                        engobj.wait_ge(self.csem[k[1]], rank[k[1]][v])
                    else:
                        engobj.wait_ge(self.dsem[k[1]], v)
                ins = op.fn(engobj)
                if op.dma is not None:
                    ins.then_inc(self.dsem[op.dma], 16)
                elif i in rk:
                    ins.then_inc(self.csem[e], 1)

        with nc.Block() as block:
            @block.tensor
            def _(pe):
                run("pe", pe)

            @block.scalar
            def _(act):
                run("act", act)

            @block.vector
            def _(dve):
                run("dve", dve)

            @block.gpsimd
            def _(pool):
                run("pool", pool)

            @block.sync
            def _(sp):
                run("sp", sp)


class Prog:
    def __init__(self, debug=None, stop_after=None):
        self.debug = debug or []
        self.stop_after = stop_after
        self.nc = bass.Bass("TRN2", target_bir_lowering=False)
        self.es = ExitStack()

    def din(self, name, shape, dt=F32):
        return self.nc.dram_tensor(name, list(shape), dt, kind="ExternalInput").ap()

    def dscr(self, name, shape, dt=BF16):
        kind = "ExternalOutput" if name in self.debug else "Internal"
        return self.nc.dram_tensor(name, list(shape), dt, kind=kind).ap()

    def sb(self, ps, name, shape, dt):
        self.uid = getattr(self, "uid", 0) + 1
        name = "%s_%d" % (name, self.uid)
        return Buf(ps.enter_context(self.nc.sbuf_tensor(name, list(shape), dt)), name)

    def pm(self, ps, name, shape, dt=F32):
        self.uid = getattr(self, "uid", 0) + 1
        name = "%s_%d" % (name, self.uid)
        return Buf(ps.enter_context(self.nc.psum_tensor(name, list(shape), dt)), name)

    def build(self):
        nc = self.nc
        es = self.es
        kb = self.kb = KB(nc, es)
        self.x = self.din("x", [T, D])
        self.cT = self.din("cT", [128, 16])
        self.w_ada = self.din("w_ada", [DEPTH, D, 6 * D])
        self.b_ada = self.din("b_ada", [DEPTH, 6 * D])
        self.g1T = self.din("g1T", [DEPTH, 128, 16])
        self.g2T = self.din("g2T", [DEPTH, 128, 16])
        self.w_in = self.din("w_in", [DEPTH, D, IN_W])
        self.b_mgate = self.din("b_mgate", [DEPTH, 8])
        self.g_mnorm = self.din("g_mnorm", [DEPTH, 1024])
        self.gqlT = self.din("gqlT", [DEPTH, 128, 4])
        self.w_uq = self.din("w_uq", [DEPTH, 512, 1536])
        self.gkvT = self.din("gkvT", [DEPTH, 128, 2])
        self.w_ukv = self.din("w_ukv", [DEPTH, 256, 2048])
        self.gqnT = self.din("gqnT", [DEPTH, 128, 2])
        self.gknT = self.din("gknT", [DEPTH, 128, 2])
        self.w_pool = self.din("w_pool", [DEPTH, 4, 256, 256])
        self.bpT = self.din("bpT", [DEPTH, 128, 8])
        self.spT = self.din("spT", [DEPTH, 128, 8])
        self.w_branch = self.din("w_branch", [DEPTH, 3, 1024, D])
        self.w_out = self.din("w_out", [DEPTH, D, D])
        self.w_ffn_in = self.din("w_ffn_in", [DEPTH, D, 2 * FFN])
        self.w_ffn_out = self.din("w_ffn_out", [DEPTH, FFN, D])
        self.cst = self.din("cst", [128, 128 * 6])
        self.ropec = self.din("ropec", [64, 2 * T + 64])
        self.poolinv = self.din("poolinv", [4, T])
        self.out = self.nc.dram_tensor("out", [T, D], F32, kind="ExternalOutput").ap()
        self.mod_d = self.dscr("mod_d", [DEPTH, 6 * D], F32)
        self.qmT = self.dscr("qmT", [512, T])
        self.kmT = self.dscr("kmT", [512, T])
        self.k_tm = self.dscr("k_tm", [T, 512])
        self.v_tm = self.dscr("v_tm", [T, 1024])
        self.so_tm = self.dscr("so_tm", [T, 1024])
        self.if_d = self.dscr("if_d", [T, 8], F32)
        self.qlatT = self.dscr("qlatT", [512, T])
        self.kvlatT = self.dscr("kvlatT", [256, T])
        self.krT = self.dscr("krT", [64, T])
        self.upT = self.dscr("upT", [1024, T])
        self.gatesT = self.dscr("gatesT", [6144, T])
        self.braT = self.dscr("braT", [1024, T])
        self.brbT = self.dscr("brbT", [1024, T])
        self.brcT = self.dscr("brcT", [1024, T])
        self.mergedT = self.dscr("mergedT", [D, T])
        self.xmid = self.dscr("xmid", [T, D], F32)
        self.x1 = self.dscr("x1", [T, D], F32)

        self.cf = self.sb(es, "cf", [128, 128 * 6], F32)
        self.cb = self.sb(es, "cb", [128, 128 * 6], BF16)
        kb.dma("sp", self.cf[:, :], self.cst[:, :], writes=[self.cf])
        kb.dma("pool", self.cb[:, :], self.cst[:, :], writes=[self.cb])
        self.stages = [
            ("mod", lambda: self.phase_mod()),
        ]
        xin = self.x
        for l in range(DEPTH):
            xo = self.out if l == DEPTH - 1 else self.x1
            self.stages += [
                ("A%d" % l, lambda l=l, xin=xin: self.phase_win(l, xin)),
                ("C%d" % l, lambda l=l: self.phase_mlstm(l)),
                ("D%d" % l, lambda l=l: self.phase_mla(l)),
                ("P%d" % l, lambda l=l: self.phase_pool(l)),
                ("E%d" % l, lambda l=l, xin=xin: self.phase_merge(l, xin)),
                ("F%d" % l, lambda l=l, xo=xo: self.phase_ffn(l, xo)),
            ]
            xin = xo
        for name, fn in self.stages:
            fn()
            kb.barrier()
            if self.stop_after == name:
                break
        kb.barrier()
        kb.emit()
        return nc

    def cF(self, i):
        return self.cf[:, i * 128:(i + 1) * 128]

    def cB(self, i):
        return self.cb[:, i * 128:(i + 1) * 128]

    def phase_mod(self):
        nc, kb = self.nc, self.kb
        with ExitStack() as ps:
            ct = self.sb(ps, "ct", [128, 16], F32)
            ca = self.sb(ps, "ca", [128, 16], F32)
            wt = [self.sb(ps, "wada%d" % i, [128, 16, 512], F32) for i in range(2)]
            pr = [self.pm(ps, "pmod%d" % i, [1, 512]) for i in range(2)]
            brow = self.sb(ps, "brow", [1, DEPTH * 6 * D], F32)
            orow = [self.sb(ps, "orow%d" % i, [1, 512], F32) for i in range(2)]
            kb.dma("sp", ct[:, :], self.cT[:, :], writes=[ct])
            kb.dma("sp", brow[:, :], self.b_ada.rearrange("l n -> (l n)").rearrange("(o n) -> o n", o=1), writes=[brow])
            kb.op("act", lambda e: e.activation(out=ca[:, :], in_=ct[:, :], func=AF.Silu), reads=[ct], writes=[ca])
            tiles = [(l, cbk) for l in range(DEPTH) for cbk in range(24)]

            def load(i):
                l, cbk = tiles[i]
                src = self.w_ada[l, :, cbk * 512:(cbk + 1) * 512].rearrange("(kc p) n -> p kc n", p=128)
                kb.dma("sp", wt[i % 2][:, :, :], src, writes=[wt[i % 2]])

            load(0)
            for i, (l, cbk) in enumerate(tiles):
                if i + 1 < len(tiles):
                    load(i + 1)
                w = wt[i % 2]
                p = pr[i % 2]
                o = orow[i % 2]
                for kc in range(16):
                    kb.op("pe", lambda e, w=w, p=p, kc=kc: e.matmul(p[0:1, :], lhsT=ca[:, kc:kc + 1], rhs=w[:, kc, :],
                                                                    start=(kc == 0), stop=(kc == 15)),
                          reads=[ca, w], writes=[p] if kc == 0 else [], wpart=[p] if kc else [])
                off = l * 6 * D + cbk * 512
                kb.op("dve", lambda e, p=p, o=o, off=off: e.tensor_tensor(out=o[0:1, :], in0=p[0:1, :], in1=brow[0:1, off:off + 512], op=ALU.add),
                      reads=[p, brow], writes=[o])
                kb.dma("sp", self.mod_d[l:l + 1, cbk * 512:(cbk + 1) * 512], o[0:1, :], reads=[o])

    def load_mod_cols(self, ps, l, which, gT, name):
        nc, kb = self.nc, self.kb
        sh = self.sb(ps, name + "sh", [128, 16], F32)
        sc = self.sb(ps, name + "sc", [128, 16], F32)
        g = self.sb(ps, name + "g", [128, 16], F32)
        gs = self.sb(ps, name + "gs", [128, 16], F32)
        base = 0 if which == 1 else 3 * D
        kb.dma("sp", sh[:, :], self.mod_d[l, base:base + D].rearrange("(j p) -> p j", p=128), writes=[sh], slow=True)
        kb.dma("sp", sc[:, :], self.mod_d[l, base + D:base + 2 * D].rearrange("(j p) -> p j", p=128), writes=[sc], slow=True)
        kb.dma("sp", g[:, :], gT[l], writes=[g])
        kb.op("dve", lambda e: e.tensor_scalar(out=sc[:, :], in0=sc[:, :], scalar1=1.0, scalar2=1.0, op0=ALU.add, op1=ALU.mult),
              reads=[sc], writes=[sc])
        kb.op("dve", lambda e: e.tensor_tensor(out=gs[:, :], in0=sc[:, :], in1=g[:, :], op=ALU.mult), reads=[sc, g], writes=[gs])
        return gs, sh

    def norm_to_hT(self, ps, xsrc, tb0, ntb, hT, gs, sh, tag):
        nc, kb = self.nc, self.kb
        xt = [self.sb(ps, tag + "xt%d" % i, [128, D], F32) for i in range(2)]
        junk = self.sb(ps, tag + "junk", [128, D], BF16)
        xn = [self.sb(ps, tag + "xn%d" % i, [128, D], BF16) for i in range(2)]
        ss = [self.sb(ps, tag + "ss%d" % i, [128, 1], F32) for i in range(2)]
        rs = [self.sb(ps, tag + "rs%d" % i, [128, 1], F32) for i in range(2)]
        pt = [self.pm(ps, tag + "pt%d" % i, [128, 1024], BF16) for i in range(4)]
        ident = self.cB(0)
        for j in range(ntb):
            tb = tb0 + j
            x_, xn_, ss_, rs_ = xt[j % 2], xn[j % 2], ss[j % 2], rs[j % 2]
            kb.dma("sp", x_[:, :], xsrc[tb * 128:(tb + 1) * 128, :], writes=[x_])
            kb.op("act", lambda e, x_=x_, ss_=ss_: e.activation(out=junk[:, :], in_=x_[:, :], func=AF.Square, accum_out=ss_[:, 0:1]),
                  reads=[x_], writes=[junk, ss_])
            kb.op("act", lambda e, ss_=ss_: e.activation(out=ss_[:, :], in_=ss_[:, :], func=AF.Sqrt, bias=EPS, scale=1.0 / D),
                  reads=[ss_], writes=[ss_])
            kb.op("dve", lambda e, ss_=ss_, rs_=rs_: e.reciprocal(out=rs_[:, :], in_=ss_[:, :]), reads=[ss_], writes=[rs_])
            kb.op("act", lambda e, x_=x_, xn_=xn_, rs_=rs_: e.activation(out=xn_[:, :], in_=x_[:, :], func=AF.Identity, scale=rs_[:, 0:1]),
                  reads=[x_, rs_], writes=[xn_])
            for half in range(2):
                p = pt[(j * 2 + half) % 4]
                for q in range(8):
                    dc = half * 8 + q
                    kb.op("pe", lambda e, p=p, q=q, dc=dc, xn_=xn_: e.transpose(p[:, q * 128:(q + 1) * 128], xn_[:, dc * 128:(dc + 1) * 128], ident),
                          reads=[xn_, self.cb], writes=[p] if q == 0 else [], wpart=[p] if q else [])
                for q in range(8):
                    dc = half * 8 + q
                    if half == 0:
                        kb.op("dve", lambda e, p=p, q=q, dc=dc, j=j: e.tensor_scalar(
                            out=hT[:, dc, j * 128:(j + 1) * 128], in0=p[:, q * 128:(q + 1) * 128],
                            scalar1=gs[:, dc:dc + 1], scalar2=sh[:, dc:dc + 1], op0=ALU.mult, op1=ALU.add),
                            reads=[p, gs, sh], wpart=[hT])
                    else:
                        kb.op("act", lambda e, p=p, q=q, dc=dc, j=j: e.activation(
                            out=hT[:, dc, j * 128:(j + 1) * 128], in_=p[:, q * 128:(q + 1) * 128],
                            func=AF.Identity, bias=sh[:, dc:dc + 1], scale=gs[:, dc:dc + 1]),
                            reads=[p, gs, sh], wpart=[hT.alt])

    class WStream:
        def __init__(self, prog, ps, name, kc, ncols, nbuf=3):
            self.prog = prog
            self.bufs = [prog.sb(ps, "%s%d" % (name, i), [128, kc, ncols], BF16) for i in range(nbuf)]
            self.i = 0
            self.kc = kc

        def load(self, src2d, n):
            b = self.bufs[self.i % len(self.bufs)]
            self.i += 1
            self.prog.kb.dma("pool", b[:, :, 0:n], src2d.rearrange("(kc p) n -> p kc n", p=128), writes=[b])
            return b

    def phase_win(self, l, xin):
        nc, kb = self.nc, self.kb
        with ExitStack() as ps:
            hT = self.sb(ps, "hT", [128, 16, T], BF16)
            hT.alt = hT
            with ExitStack() as ps2:
                gs, sh = self.load_mod_cols(ps2, l, 1, self.g1T, "m1")
                self.norm_to_hT(ps2, xin, 0, NTB, hT, gs, sh, "n1")
            kb.barrier()
            ws = self.WStream(self, ps, "wi", 16, 512, 3)
            pacc = [self.pm(ps, "pacc%d" % i, [128, 512]) for i in range(8)]
            ot = [self.sb(ps, "ot%d" % i, [128, T], BF16) for i in range(3)]
            otm = [self.sb(ps, "otm%d" % i, [128, 512], BF16) for i in range(3)]
            ifs = self.sb(ps, "ifs", [128, NTB, 8], F32)
            W = self.w_in[l]
            state = {"pset": 0, "ot": 0, "pb": 0, "otm": 0, "ev": 0}

            def fm_tile(c0, n, dst, func, scale):
                wt = ws.load(W[:, c0:c0 + n], n)
                for f0 in range(0, n, 128):
                    m = min(128, n - f0)
                    pset = state["pset"]
                    state["pset"] ^= 1
                    banks = pacc[pset * 4:(pset + 1) * 4]
                    for k in range(16):
                        for tt in range(4):
                            p = banks[tt]
                            kb.op("pe", lambda e, p=p, k=k, tt=tt, f0=f0, m=m, wt=wt: e.matmul(
                                p[0:m, :], lhsT=wt[:, k, f0:f0 + m], rhs=hT[:, k, tt * 512:(tt + 1) * 512],
                                start=(k == 0), stop=(k == 15)),
                                reads=[wt, hT, hT.alt], writes=[p] if k == 0 else [], wpart=[p] if k else [])
                    o = ot[state["ot"] % 3]
                    state["ot"] += 1
                    for tt in range(4):
                        p = banks[tt]
                        eng = "act" if (func is not None or tt % 2 == 1) else "dve"
                        first = (tt == 0)
                        if eng == "act":
                            kb.op("act", lambda e, p=p, o=o, tt=tt, m=m: e.activation(
                                out=o[0:m, tt * 512:(tt + 1) * 512], in_=p[0:m, :], func=(func or AF.Identity), scale=scale),
                                reads=[p], writes=[o] if first else [], wpart=[] if first else [o])
                        else:
                            kb.op("dve", lambda e, p=p, o=o, tt=tt, m=m: e.tensor_scalar(
                                out=o[0:m, tt * 512:(tt + 1) * 512], in0=p[0:m, :], scalar1=scale, scalar2=None, op0=ALU.mult),
                                reads=[p], writes=[o] if first else [], wpart=[] if first else [o])
                    r0 = c0 - dst[1] + f0
                    kb.dma("sp", dst[0][r0:r0 + m, :], o[0:m, :], reads=[o])

            def tm_tile(c0, n, dst, func, scale):
                wt = ws.load(W[:, c0:c0 + n], n)
                for tb in range(NTB):
                    p = pacc[state["pb"] % 8]
                    state["pb"] += 1
                    for k in range(16):
                        kb.op("pe", lambda e, p=p, k=k, tb=tb, wt=wt: e.matmul(
                            p[:, 0:n], lhsT=hT[:, k, tb * 128:(tb + 1) * 128], rhs=wt[:, k, 0:n],
                            start=(k == 0), stop=(k == 15)),
                            reads=[wt, hT, hT.alt], writes=[p] if k == 0 else [], wpart=[p] if k else [])
                    if dst is None:
                        kb.op("dve", lambda e, p=p, tb=tb: e.tensor_copy(out=ifs[:, tb, :], in_=p[:, 0:8]), reads=[p], wpart=[ifs])
                        continue
                    o = otm[state["otm"] % 3]
                    state["otm"] += 1
                    state["ev"] += 1
                    if func is not None or state["ev"] % 2:
                        kb.op("act", lambda e, p=p, o=o: e.activation(out=o[:, 0:n], in_=p[:, 0:n], func=(func or AF.Identity), scale=scale),
                              reads=[p], writes=[o])
                    else:
                        kb.op("dve", lambda e, p=p, o=o: e.tensor_scalar(out=o[:, 0:n], in0=p[:, 0:n], scalar1=scale, scalar2=None, op0=ALU.mult),
                              reads=[p], writes=[o])
                    cc = c0 - dst[1]
                    kb.dma("sp", dst[0][tb * 128:(tb + 1) * 128, cc:cc + n], o[:, 0:n], reads=[o])

            ksc = float(128 ** -0.5)
            tm_tile(C_IF, 8, None, None, 1.0)
            tm_tile(C_KM, 512, (self.k_tm, C_KM), None, ksc)
            for c0 in (C_VM, C_VM + 512):
                tm_tile(c0, 512, (self.v_tm, C_VM), None, 1.0)
            for c0 in (C_OM, C_OM + 512):
                tm_tile(c0, 512, (self.so_tm, C_OM), AF.Sigmoid, 1.0)
            kb.dma("sp", self.if_d.rearrange("(c p) e -> p c e", p=128), ifs[:, :, :], reads=[ifs])
            fm_tile(C_QM, 512, (self.qmT, C_QM), None, 1.0)
            fm_tile(C_KM, 512, (self.kmT, C_KM), None, ksc)
            fm_tile(C_QL, 512, (self.qlatT, C_QL), None, 1.0)
            fm_tile(C_KV, 256, (self.kvlatT, C_KV), None, 1.0)
            fm_tile(C_KR, 64, (self.krT, C_KR), None, 1.0)
            for c0 in (C_UP, C_UP + 512):
                fm_tile(c0, 512, (self.upT, C_UP), None, 1.0)
            for c0 in range(C_GT, IN_W, 512):
                fm_tile(c0, 512, (self.gatesT, C_GT), AF.Sigmoid, 1.0)

    def phase_mlstm(self, l):
        nc, kb = self.nc, self.kb
        with ExitStack() as ps:
            cf, cbf = self.cf, self.cb
            identF, U, negT, neg, onesF = self.cF(0), self.cF(1), self.cF(2), self.cF(3), self.cF(5)
            identB = self.cB(0)
            qT = self.sb(ps, "mqT", [128, 4, T], BF16)
            kT = self.sb(ps, "mkT", [128, 4, T], BF16)
            ktm = self.sb(ps, "mktm", [128, NTB, 512], BF16)
            vaug = self.sb(ps, "mvaug", [128, NTB, 4, 257], BF16)
            ifs = self.sb(ps, "mifs", [128, NTB, 8], F32)
            bmg = self.sb(ps, "mbmg", [128, 8], F32)
            gmn = self.sb(ps, "mgmn", [128, 1024], F32)
            kb.dma("sp", qT[:, :, :], self.qmT.rearrange("(h d) t -> d h t", d=128), writes=[qT])
            kb.dma("sp", kT[:, :, :], self.kmT.rearrange("(h d) t -> d h t", d=128), writes=[kT])
            kb.dma("sp", ktm[:, :, :], self.k_tm.rearrange("(c p) e -> p c e", p=128), writes=[ktm])
            kb.op("pool", lambda e: e.memset(vaug[:, :, :, :], 1.0), writes=[vaug])
            for c in range(NTB):
                kb.dma("sp", vaug[:, c, :, 0:256], self.v_tm[c * 128:(c + 1) * 128, :].rearrange("p (h v) -> p h v", h=4), reads=[], wpart=[vaug])
            kb.dma("sp", ifs[:, :, :], self.if_d.rearrange("(c p) e -> p c e", p=128), writes=[ifs])
            kb.dma("sp", bmg[:, :], self.b_mgate[l].partition_broadcast(128), writes=[bmg])
            kb.dma("sp", gmn[:, :], self.g_mnorm[l].partition_broadcast(128), writes=[gmn])
            NI = NTB * 4
            mk = lambda n, w=NI: self.sb(ps, n, [128, w], F32)
            ipre, fpre, logf, bcs, bL, g, cm, gmax = [mk("m" + n) for n in ("ipre", "fpre", "logf", "bcs", "bL", "g", "cm", "gmax")]
            mprev = mk("mprev", NI + 4)
            mxL, decay, wsc, mx, nmx, em = [mk("m" + n) for n in ("mxL", "decay", "wsc", "mx", "nmx", "em")]
            pP = self.pm(ps, "mpP", [128, 512])
            pT = self.pm(ps, "mpT", [128, 1024], BF16)
            pX = [self.pm(ps, "mpX%d" % i, [128, 384]) for i in range(2)]
            pN = [self.pm(ps, "mpN%d" % i, [128, 257]) for i in range(2)]
            pC = [self.pm(ps, "mpC%d" % i, [128, 257]) for i in range(2)]
            for c in range(NTB):
                kb.op("dve", lambda e, c=c: e.tensor_tensor(out=ipre[:, c * 4:(c + 1) * 4], in0=ifs[:, c, 0:4], in1=bmg[:, 0:4], op=ALU.add),
                      reads=[ifs, bmg], wpart=[ipre])
                kb.op("dve", lambda e, c=c: e.tensor_tensor(out=fpre[:, c * 4:(c + 1) * 4], in0=ifs[:, c, 4:8], in1=bmg[:, 4:8], op=ALU.add),
                      reads=[ifs, bmg], wpart=[fpre])
            kb.op("act", lambda e: e.activation(out=logf[:, :], in_=fpre[:, :], func=AF.Exp, scale=-1.0), reads=[fpre], writes=[logf])
            kb.op("act", lambda e: e.activation(out=logf[:, :], in_=logf[:, :], func=AF.Ln, bias=1.0, scale=1.0), reads=[logf], writes=[logf])
            kb.op("dve", lambda e: e.tensor_scalar(out=logf[:, :], in0=logf[:, :], scalar1=-1.0, scalar2=None, op0=ALU.mult), reads=[logf], writes=[logf])
            kb.op("pe", lambda e: e.matmul(pP[:, 0:NI], lhsT=U, rhs=logf[:, :], start=True, stop=True), reads=[logf, cf], writes=[pP])
            kb.op("dve", lambda e: e.tensor_copy(out=bcs[:, :], in_=pP[:, 0:NI]), reads=[pP], writes=[bcs])
            kb.op("pe", lambda e: e.matmul(pP[:, 0:NI], lhsT=onesF, rhs=logf[:, :], start=True, stop=True), reads=[logf, cf], writes=[pP])
            kb.op("dve", lambda e: e.tensor_copy(out=bL[:, :], in_=pP[:, 0:NI]), reads=[pP], writes=[bL])
            kb.op("dve", lambda e: e.tensor_tensor(out=g[:, :], in0=ipre[:, :], in1=bcs[:, :], op=ALU.subtract), reads=[ipre, bcs], writes=[g])
            gb = [self.sb(ps, "mgb%d" % i, [128, 128], F32) for i in range(2)]
            tmpm = [self.sb(ps, "mtmpm%d" % i, [128, 128], F32) for i in range(2)]
            for idx in range(NI):
                gb_ = gb[idx % 2]
                tm_ = tmpm[idx % 2]
                pg = pX[idx % 2]
                kb.op("pool", lambda e, gb_=gb_, idx=idx: e.tensor_copy(out=gb_[:, :], in_=g[:, idx:idx + 1].to_broadcast([128, 128])), reads=[g], writes=[gb_])
                kb.op("pe", lambda e, gb_=gb_, pg=pg: e.matmul(pg[:, 0:128], lhsT=gb_[:, :], rhs=identF, start=True, stop=True), reads=[gb_, cf], writes=[pg])
                kb.op("dve", lambda e, pg=pg, tm_=tm_: e.tensor_tensor(out=tm_[:, :], in0=pg[:, 0:128], in1=neg, op=ALU.add), reads=[pg, cf], writes=[tm_])
                kb.op("dve", lambda e, tm_=tm_, idx=idx: e.reduce_max(out=cm[:, idx:idx + 1], in_=tm_[:, :], axis=AX.X), reads=[tm_], wpart=[cm])
                kb.op("dve", lambda e, pg=pg, idx=idx: e.reduce_max(out=gmax[:, idx:idx + 1], in_=pg[:, 0:128], axis=AX.X), reads=[pg], wpart=[gmax])
            kb.op("dve", lambda e: e.memset(mprev[:, 0:4], 0.0), writes=[mprev])
            for c in range(NTB):
                sl = slice(c * 4, (c + 1) * 4)
                sl2 = slice((c + 1) * 4, (c + 2) * 4)
                kb.op("dve", lambda e, sl=sl: e.tensor_tensor(out=mxL[:, sl], in0=mprev[:, sl], in1=gmax[:, sl], op=ALU.max), reads=[mprev, gmax], writes=[mxL])
                kb.op("dve", lambda e, sl=sl, sl2=sl2: e.tensor_tensor(out=mprev[:, sl2], in0=bL[:, sl], in1=mxL[:, sl], op=ALU.add), reads=[bL, mxL], writes=[mprev])
            mp = mprev
            kb.op("dve", lambda e: e.tensor_tensor(out=decay[:, :], in0=mp[:, 0:NI], in1=mxL[:, :], op=ALU.subtract), reads=[mp, mxL], writes=[decay])
            kb.op("act", lambda e: e.activation(out=decay[:, :], in_=decay[:, :], func=AF.Exp), reads=[decay], writes=[decay])
            kb.op("dve", lambda e: e.tensor_tensor(out=wsc[:, :], in0=g[:, :], in1=mxL[:, :], op=ALU.subtract), reads=[g, mxL], writes=[wsc])
            kb.op("act", lambda e: e.activation(out=wsc[:, :], in_=wsc[:, :], func=AF.Exp), reads=[wsc], writes=[wsc])
            kb.op("dve", lambda e: e.tensor_tensor(out=mx[:, :], in0=mp[:, 0:NI], in1=cm[:, :], op=ALU.max), reads=[mp, cm], writes=[mx])
            kb.op("dve", lambda e: e.tensor_scalar(out=nmx[:, :], in0=mx[:, :], scalar1=-1.0, scalar2=None, op0=ALU.mult), reads=[mx], writes=[nmx])
            kb.op("dve", lambda e: e.tensor_tensor(out=em[:, :], in0=bcs[:, :], in1=mx[:, :], op=ALU.add), reads=[bcs, mx], writes=[em])
            kb.op("act", lambda e: e.activation(out=em[:, :], in_=em[:, :], func=AF.Exp, scale=-1.0), reads=[em], writes=[em])
            CT = [self.sb(ps, "mCT%d" % h, [128, 257], F32) for h in range(4)]
            CTb = [self.sb(ps, "mCTb%d" % h, [128, 257], BF16) for h in range(4)]
            for h in range(4):
                kb.op("pool", lambda e, h=h: e.memset(CT[h][:, :], 0.0), writes=[CT[h]])
                kb.op("pool", lambda e, h=h: e.memset(CTb[h][:, :], 0.0), writes=[CTb[h]])
            nmxb = [self.sb(ps, "mnmxb%d" % i, [128, 128], F32) for i in range(2)]
            Dt = [self.sb(ps, "mDt%d" % i, [128, 128], F32) for i in range(2)]
            Wb = [self.sb(ps, "mWb%d" % i, [128, 128], F32) for i in range(2)]
            SD = [self.sb(ps, "mSD%d" % i, [128, 128], BF16) for i in range(2)]
            qw = [self.sb(ps, "mqw%d" % i, [128, 128], BF16) for i in range(2)]
            dn = [self.sb(ps, "mdn%d" % i, [128, 1], F32) for i in range(2)]
            hm = [self.sb(ps, "mhm%d" % i, [128, 256], F32) for i in range(2)]
            junk = self.sb(ps, "mjunk", [128, 256], F32)
            ss2 = [self.sb(ps, "mss2%d" % i, [128, 1], F32) for i in range(2)]
            t1 = [self.sb(ps, "mt1%d" % i, [128, 256], F32) for i in range(2)]
            vw = [self.sb(ps, "mvw%d" % i, [128, 257], BF16) for i in range(2)]
            soc = [self.sb(ps, "msoc%d" % i, [128, 1024], BF16) for i in range(2)]
            brat = [self.sb(ps, "mbrat%d" % i, [128, 1024], BF16) for i in range(2)]
            braTs = [self.sb(ps, "mbraTs%d" % i, [128, 8, 128], BF16) for i in range(2)]
            for c in range(NTB):
                cs = slice(c * 128, (c + 1) * 128)
                so_ = soc[c % 2]
                bt_ = brat[c % 2]
                kb.dma("sp", so_[:, :], self.so_tm[cs, :], writes=[so_])
                def mstage(h, st):
                    idx = c * 4 + h
                    i2 = idx % 2
                    X, N_, C_ = pX[i2], pN[i2], pC[i2]
                    nb_, D_, W_, S_, q_, dn_, hm_, ss_, t1_, vw_ = nmxb[i2], Dt[i2], Wb[i2], SD[i2], qw[i2], dn[i2], hm[i2], ss2[i2], t1[i2], vw[i2]
                    if st == 0:
                        kb.op("pool", lambda e, nb_=nb_, idx=idx: e.tensor_copy(out=nb_[:, :], in_=nmx[:, idx:idx + 1].to_broadcast([128, 128])), reads=[nmx], writes=[nb_])
                        kb.op("pe", lambda e, X=X, nb_=nb_: e.matmul(X[:, 0:128], lhsT=nb_[:, :], rhs=identF, start=True, stop=False), reads=[nb_, cf], writes=[X])
                        kb.op("pe", lambda e, X=X: e.matmul(X[:, 0:128], lhsT=identF, rhs=negT, start=False, stop=True), reads=[cf], wpart=[X])
                        kb.op("pe", lambda e, X=X, nb_=nb_: e.matmul(X[:, 128:256], lhsT=nb_[:, :], rhs=identF, start=True, stop=True), reads=[nb_, cf], wpart=[X])
                        kb.op("pe", lambda e, X=X, h=h, cs=cs: e.matmul(X[:, 256:384], lhsT=kT[:, h, cs], rhs=qT[:, h, cs], start=True, stop=True), reads=[kT, qT], wpart=[X])
                    if st == 1:
                        kb.op("act", lambda e, X=X, D_=D_, idx=idx: e.activation(out=D_[:, :], in_=X[:, 0:128], func=AF.Exp, bias=g[:, idx:idx + 1], scale=1.0),
                              reads=[X, g], writes=[D_])
                        kb.op("act", lambda e, X=X, W_=W_, idx=idx: e.activation(out=W_[:, :], in_=X[:, 128:256], func=AF.Exp, bias=mp[:, idx:idx + 1], scale=1.0),
                              reads=[X, mp], writes=[W_])
                    if st == 2:
                        kb.op("dve", lambda e, X=X, D_=D_, S_=S_: e.tensor_tensor(out=S_[:, :], in0=X[:, 256:384], in1=D_[:, :], op=ALU.mult), reads=[X, D_], writes=[S_])
                        kb.op("dve", lambda e, W_=W_, q_=q_, h=h, cs=cs: e.tensor_tensor(out=q_[:, :], in0=qT[:, h, cs], in1=W_[:, :], op=ALU.mult), reads=[qT, W_], writes=[q_])
                    if st == 3:
                        kb.op("pe", lambda e, N_=N_, q_=q_, h=h: e.matmul(N_[:, :], lhsT=q_[:, :], rhs=CTb[h][:, :], start=True, stop=False), reads=[q_, CTb[h]], writes=[N_])
                        kb.op("pe", lambda e, N_=N_, S_=S_, c=c, h=h: e.matmul(N_[:, :], lhsT=S_[:, :], rhs=vaug[:, c, h, :], start=False, stop=True), reads=[S_, vaug], wpart=[N_])
                        kb.op("act", lambda e, vw_=vw_, c=c, h=h, idx=idx: e.activation(out=vw_[:, :], in_=vaug[:, c, h, :], func=AF.Identity, scale=wsc[:, idx:idx + 1]),
                              reads=[vaug, wsc], writes=[vw_])
                        kb.op("pe", lambda e, C_=C_, vw_=vw_, c=c, h=h: e.matmul(C_[:, :], lhsT=ktm[:, c, h * 128:(h + 1) * 128], rhs=vw_[:, :], start=True, stop=True),
                              reads=[ktm, vw_], writes=[C_])
                    if st == 4:
                        kb.op("dve", lambda e, C_=C_, h=h, idx=idx: e.scalar_tensor_tensor(out=CT[h][:, :], in0=CT[h][:, :], scalar=decay[:, idx:idx + 1], in1=C_[:, :],
                                                                                          op0=ALU.mult, op1=ALU.add), reads=[CT[h], decay, C_], writes=[CT[h]])
                        kb.op("act", lambda e, h=h: e.activation(out=CTb[h][:, :], in_=CT[h][:, :], func=AF.Identity), reads=[CT[h]], writes=[CTb[h]])

                    if st == 5:
                        kb.op("act", lambda e, N_=N_, dn_=dn_: e.activation(out=dn_[:, :], in_=N_[:, 256:257], func=AF.Abs), reads=[N_], writes=[dn_])
                        kb.op("dve", lambda e, dn_=dn_, idx=idx: e.tensor_tensor(out=dn_[:, :], in0=dn_[:, :], in1=em[:, idx:idx + 1], op=ALU.max), reads=[dn_, em], writes=[dn_])
                        kb.op("dve", lambda e, dn_=dn_: e.reciprocal(out=dn_[:, :], in_=dn_[:, :]), reads=[dn_], writes=[dn_])
                    if st == 6:
                        kb.op("act", lambda e, N_=N_, hm_=hm_, dn_=dn_: e.activation(out=hm_[:, :], in_=N_[:, 0:256], func=AF.Identity, scale=dn_[:, 0:1]), reads=[N_, dn_], writes=[hm_])
                        kb.op("act", lambda e, hm_=hm_, ss_=ss_: e.activation(out=junk[:, :], in_=hm_[:, :], func=AF.Square, accum_out=ss_[:, 0:1]), reads=[hm_], writes=[junk, ss_])
                        kb.op("act", lambda e, ss_=ss_: e.activation(out=ss_[:, :], in_=ss_[:, :], func=AF.Sqrt, bias=EPS, scale=1.0 / 256), reads=[ss_], writes=[ss_])
                        kb.op("dve", lambda e, ss_=ss_: e.reciprocal(out=ss_[:, :], in_=ss_[:, :]), reads=[ss_], writes=[ss_])
                        kb.op("dve", lambda e, hm_=hm_, ss_=ss_, t1_=t1_, h=h: e.scalar_tensor_tensor(out=t1_[:, :], in0=hm_[:, :], scalar=ss_[:, 0:1], in1=gmn[:, h * 256:(h + 1) * 256],
                                                                                                     op0=ALU.mult, op1=ALU.mult), reads=[hm_, ss_, gmn], writes=[t1_])
                        kb.op("pool", lambda e, t1_=t1_, h=h, so_=so_, bt_=bt_: e.tensor_tensor(out=bt_[:, h * 256:(h + 1) * 256], in0=t1_[:, :], in1=so_[:, h * 256:(h + 1) * 256], op=ALU.mult),
                              reads=[t1_, so_], writes=[bt_] if h == 0 else [], wpart=[bt_] if h else [])

                for pair in ((0, 1), (2, 3)):
                    for st in range(7):
                        for h in pair:
                            mstage(h, st)
                bs_ = braTs[c % 2]
                for q in range(8):
                    kb.op("pe", lambda e, q=q, bt_=bt_: e.transpose(pT[:, q * 128:(q + 1) * 128], bt_[:, q * 128:(q + 1) * 128], identB),
                          reads=[bt_, cbf], writes=[pT] if q == 0 else [], wpart=[pT] if q else [])
                kb.op("act", lambda e, bs_=bs_: e.activation(out=bs_[:, :, :], in_=pT[:, :].rearrange("p (q t) -> p q t", q=8), func=AF.Identity), reads=[pT], writes=[bs_])
                kb.dma("sp", self.braT[:, cs].rearrange("(q p) t -> p q t", p=128), bs_[:, :, :], reads=[bs_])

    def phase_mla(self, l):
        nc, kb = self.nc, self.kb
        with ExitStack() as ps:
            ones_b, tri = self.cB(5), self.cB(4)
            cbuf, cfb = self.cb, self.cf
            ql = self.sb(ps, "ql", [128, 4, T], BF16)
            kvl = self.sb(ps, "kvl", [128, 2, T], BF16)
            kr = self.sb(ps, "kr", [64, T], BF16)
            rc = self.sb(ps, "rc", [64, 2 * T + 64], F32)
            gql = self.sb(ps, "gql", [128, 4], F32)
            gkv = self.sb(ps, "gkv", [128, 2], F32)
            gqn = self.sb(ps, "gqn", [128, 2], F32)
            gkn = self.sb(ps, "gkn", [128, 2], F32)
            wuq = self.sb(ps, "wuq", [128, 4, 1536], BF16)
            wukv = self.sb(ps, "wukv", [128, 2, 2048], BF16)
            kb.dma("sp", ql[:, :, :], self.qlatT.rearrange("(kc p) t -> p kc t", p=128), writes=[ql])
            kb.dma("sp", kvl[:, :, :], self.kvlatT.rearrange("(kc p) t -> p kc t", p=128), writes=[kvl])
            kb.dma("sp", kr[:, :], self.krT[:, :], writes=[kr])
            kb.dma("sp", rc[:, :], self.ropec[:, :], writes=[rc])
            kb.dma("sp", gql[:, :], self.gqlT[l], writes=[gql])
            kb.dma("sp", gkv[:, :], self.gkvT[l], writes=[gkv])
            kb.dma("sp", gqn[:, :], self.gqnT[l], writes=[gqn])
            kb.dma("sp", gkn[:, :], self.gknT[l], writes=[gkn])
            kb.dma("pool", wuq[:, :, :], self.w_uq[l].rearrange("(kc p) n -> p kc n", p=128), writes=[wuq])
            kb.dma("pool", wukv[:, :, :], self.w_ukv[l].rearrange("(kc p) n -> p kc n", p=128), writes=[wukv])
            kb.op("dve", lambda e: e.tensor_scalar(out=gqn[:, :], in0=gqn[:, :], scalar1=float(192 ** -0.5), scalar2=None, op0=ALU.mult),
                  reads=[gqn], writes=[gqn])
            cosT = lambda a, b: rc[:, a:b]
            sinT = lambda a, b: rc[:, T + a:T + b]
            Pm = rc[:, 2 * T:2 * T + 64]
            PA, PB, PC, PD, S0, S1, PO, PR = [self.pm(ps, "pa%d" % i, [128, 512]) for i in range(8)]
            sq = [self.sb(ps, "sq%d" % i, [128, 512], BF16) for i in range(4)]
            rsd = [self.sb(ps, "rsd%d" % i, [128, 512], F32) for i in range(2)]
            cnt = {"sq": 0, "rs": 0}

            def rstd_from(pss, nfeat):
                r = rsd[cnt["rs"] % 2]
                cnt["rs"] += 1
                kb.op("act", lambda e: e.activation(out=r[:, :], in_=pss[:, :], func=AF.Sqrt, bias=EPS, scale=1.0 / nfeat), reads=[pss], writes=[r])
                kb.op("dve", lambda e: e.reciprocal(out=r[:, :], in_=r[:, :]), reads=[r], writes=[r])
                return r

            def fm_rmsnorm(src, KC, nfeat, gcol, dst):
                for tt in range(4):
                    sl = slice(tt * 512, (tt + 1) * 512)
                    for kc in range(KC):
                        q_ = sq[cnt["sq"] % 4]
                        cnt["sq"] += 1
                        kb.op("dve", lambda e, q_=q_, kc=kc: e.tensor_tensor(out=q_[:, :], in0=src[:, kc, sl], in1=src[:, kc, sl], op=ALU.mult),
                              reads=[src], writes=[q_])
                        kb.op("pe", lambda e, q_=q_, kc=kc: e.matmul(PC[:, :], lhsT=ones_b, rhs=q_[:, :], start=(kc == 0), stop=(kc == KC - 1)),
                              reads=[q_, cbuf], writes=[PC] if kc == 0 else [], wpart=[PC] if kc else [])
                    r = rstd_from(PC, nfeat)
                    for kc in range(KC):
                        kb.op("dve", lambda e, kc=kc, r=r: e.scalar_tensor_tensor(out=dst[:, kc, sl], in0=src[:, kc, sl], scalar=gcol[:, kc:kc + 1],
                                                                                in1=r[:, :], op0=ALU.mult, op1=ALU.mult),
                              reads=[src, gcol, r], wpart=[dst])

            qln = self.sb(ps, "qln", [128, 4, T], BF16)
            kvn = self.sb(ps, "kvn", [128, 2, T], BF16)
            fm_rmsnorm(ql, 4, 512, gql, qln)
            fm_rmsnorm(kvl, 2, 256, gkv, kvn)
            krr = self.sb(ps, "krr", [64, T], F32)
            sqkr = self.sb(ps, "sqkr", [64, T], BF16)
            krg = self.sb(ps, "krg", [64, 512], F32)
            ta = self.sb(ps, "ta", [64, 512], F32)
            tb_ = self.sb(ps, "tb_", [64, 512], F32)
            kb.op("dve", lambda e: e.tensor_tensor(out=sqkr[:, :], in0=kr[:, :], in1=kr[:, :], op=ALU.mult), reads=[kr], writes=[sqkr])

            def rope_apply(xf, sl0, sl1, out_ap, out_buf):
                kb.op("pe", lambda e: e.matmul(PD[0:64, :], lhsT=Pm, rhs=xf[:, :], start=True, stop=True), reads=[xf, rc], writes=[PD])
                kb.op("dve", lambda e: e.tensor_tensor(out=ta[:, :], in0=xf[:, :], in1=cosT(sl0, sl1), op=ALU.mult), reads=[xf, rc], writes=[ta])
                kb.op("dve", lambda e: e.tensor_tensor(out=tb_[:, :], in0=PD[0:64, :], in1=sinT(sl0, sl1), op=ALU.mult), reads=[PD, rc], writes=[tb_])
                kb.op("pool", lambda e: e.tensor_tensor(out=out_ap, in0=ta[:, :], in1=tb_[:, :], op=ALU.add), reads=[ta, tb_], wpart=[out_buf])

            for tt in range(4):
                a, b = tt * 512, (tt + 1) * 512
                kb.op("dve", lambda e, a=a, b=b: e.tensor_scalar(out=krg[:, :], in0=kr[:, a:b], scalar1=gkn[0:64, 1:2], scalar2=None, op0=ALU.mult),
                      reads=[kr, gkn], writes=[krg])
                rope_apply(krg, a, b, krr[:, a:b], krr)

            QnT = [self.sb(ps, "QnT%d" % i, [128, T], BF16) for i in range(2)]
            QrT = [self.sb(ps, "QrT%d" % i, [64, T], BF16) for i in range(2)]
            KnT = [self.sb(ps, "KnT%d" % i, [128, T], BF16) for i in range(2)]
            KrT = [self.sb(ps, "KrT%d" % i, [64, T], BF16) for i in range(2)]
            Vh = [self.sb(ps, "Vh%d" % i, [128, NTB, 128], BF16) for i in range(2)]
            qrf = self.sb(ps, "qrf", [64, 512], F32)
            pt = [self.sb(ps, "pt%d" % i, [128, 512], BF16) for i in range(3)]
            rr = self.sb(ps, "rr", [128, 512], F32)
            ob = [self.sb(ps, "ob%d" % i, [128, 512], BF16) for i in range(2)]
            npt = 0
            nob = 0
            for h in range(8):
                Qn, Qr, Kn, Kr, V = QnT[h % 2], QrT[h % 2], KnT[h % 2], KrT[h % 2], Vh[h % 2]
                for tt in range(4):
                    a, b = tt * 512, (tt + 1) * 512
                    s1, s2, s3 = sq[cnt["sq"] % 4], sq[(cnt["sq"] + 1) % 4], sq[(cnt["sq"] + 2) % 4]
                    cnt["sq"] += 3
                    rq, rk = rsd[0], rsd[1]
                    ones64 = self.cb[0:64, 640:768]
                    for kc in range(4):
                        kb.op("pe", lambda e, kc=kc: e.matmul(PA[:, :], lhsT=wuq[:, kc, h * 192:h * 192 + 128], rhs=qln[:, kc, a:b],
                                                             start=(kc == 0), stop=(kc == 3)), reads=[wuq, qln], writes=[PA] if kc == 0 else [], wpart=[PA] if kc else [])
                    for kc in range(4):
                        kb.op("pe", lambda e, kc=kc: e.matmul(PB[0:64, :], lhsT=wuq[:, kc, h * 192 + 128:h * 192 + 192], rhs=qln[:, kc, a:b],
                                                             start=(kc == 0), stop=(kc == 3)), reads=[wuq, qln], writes=[PB] if kc == 0 else [], wpart=[PB] if kc else [])
                    for kc in range(2):
                        kb.op("pe", lambda e, kc=kc: e.matmul(S0[:, :], lhsT=wukv[:, kc, h * 256:h * 256 + 128], rhs=kvn[:, kc, a:b],
                                                             start=(kc == 0), stop=(kc == 1)), reads=[wukv, kvn], writes=[S0] if kc == 0 else [], wpart=[S0] if kc else [])
                    kb.op("act", lambda e: e.activation(out=s1[:, :], in_=PA[:, :], func=AF.Square), reads=[PA], writes=[s1])
                    kb.op("act", lambda e: e.activation(out=s2[0:64, :], in_=PB[0:64, :], func=AF.Square), reads=[PB], writes=[s2])
                    kb.op("act", lambda e: e.activation(out=s3[:, :], in_=S0[:, :], func=AF.Square), reads=[S0], writes=[s3])
                    kb.op("pe", lambda e: e.matmul(PC[:, :], lhsT=ones_b, rhs=s1[:, :], start=True, stop=False), reads=[s1, cbuf], writes=[PC])
                    kb.op("pe", lambda e: e.matmul(PC[:, :], lhsT=ones64, rhs=s2[0:64, :], start=False, stop=True), reads=[s2, cbuf], wpart=[PC])
                    kb.op("pe", lambda e: e.matmul(S1[:, :], lhsT=ones_b, rhs=s3[:, :], start=True, stop=False), reads=[s3, cbuf], writes=[S1])
                    kb.op("pe", lambda e: e.matmul(S1[:, :], lhsT=ones64, rhs=sqkr[:, a:b], start=False, stop=True), reads=[sqkr, cbuf], wpart=[S1])
                    kb.op("act", lambda e: e.activation(out=rq[:, :], in_=PC[:, :], func=AF.Sqrt, bias=EPS, scale=1.0 / 192), reads=[PC], writes=[rq])
                    kb.op("act", lambda e: e.activation(out=rk[:, :], in_=S1[:, :], func=AF.Sqrt, bias=EPS, scale=1.0 / 192), reads=[S1], writes=[rk])
                    kb.op("dve", lambda e: e.reciprocal(out=rq[:, :], in_=rq[:, :]), reads=[rq], writes=[rq])
                    kb.op("dve", lambda e: e.reciprocal(out=rk[:, :], in_=rk[:, :]), reads=[rk], writes=[rk])
                    kb.op("dve", lambda e: e.scalar_tensor_tensor(out=Qn[:, a:b], in0=PA[:, :], scalar=gqn[:, 0:1], in1=rq[:, :], op0=ALU.mult, op1=ALU.mult),
                          reads=[PA, gqn, rq], wpart=[Qn])
                    kb.op("dve", lambda e: e.scalar_tensor_tensor(out=qrf[:, :], in0=PB[0:64, :], scalar=gqn[0:64, 1:2], in1=rq[0:64, :], op0=ALU.mult, op1=ALU.mult),
                          reads=[PB, gqn, rq], writes=[qrf])
                    kb.op("dve", lambda e: e.scalar_tensor_tensor(out=Kn[:, a:b], in0=S0[:, :], scalar=gkn[:, 0:1], in1=rk[:, :], op0=ALU.mult, op1=ALU.mult),
                          reads=[S0, gkn, rk], wpart=[Kn])
                    kb.op("pool", lambda e: e.tensor_tensor(out=Kr[:, a:b], in0=krr[:, a:b], in1=rk[0:64, :], op=ALU.mult), reads=[krr, rk], wpart=[Kr])
                    rope_apply(qrf, a, b, Qr[:, a:b], Qr)
                for g4 in range(4):
                    for j in range(4):
                        tb = g4 * 4 + j
                        for kc in range(2):
                            kb.op("pe", lambda e, kc=kc, tb=tb, j=j: e.matmul(PB[:, j * 128:(j + 1) * 128], lhsT=kvn[:, kc, tb * 128:(tb + 1) * 128],
                                                                             rhs=wukv[:, kc, h * 256 + 128:h * 256 + 256], start=(kc == 0), stop=(kc == 1)),
                                  reads=[kvn, wukv], writes=[PB] if (j == 0 and kc == 0) else [], wpart=[] if (j == 0 and kc == 0) else [PB])
                    kb.op("act", lambda e, g4=g4: e.activation(out=V[:, g4 * 4:(g4 + 1) * 4, :], in_=PB[:, :].rearrange("p (j d) -> p j d", j=4), func=AF.Identity),
                          reads=[PB], wpart=[V])
                for qt in range(4):
                    q0 = qt * 512
                    nkb = 4 * (qt + 1)
                    def emit_S(kbi):
                        off = max(0, kbi * 128 - q0)
                        S = S0 if kbi % 2 == 0 else S1
                        ks = slice(kbi * 128, (kbi + 1) * 128)
                        kb.op("pe", lambda e, S=S, off=off, ks=ks: e.matmul(S[:, off:512], lhsT=Kn[:, ks], rhs=Qn[:, q0 + off:q0 + 512], start=True, stop=False),
                              reads=[Kn, Qn], writes=[S])
                        kb.op("pe", lambda e, S=S, off=off, ks=ks: e.matmul(S[:, off:512], lhsT=Kr[:, ks], rhs=Qr[:, q0 + off:q0 + 512], start=False, stop=True),
                              reads=[Kr, Qr], wpart=[S])

                    emit_S(0)
                    for kbi in range(nkb):
                        if kbi + 1 < nkb:
                            emit_S(kbi + 1)
                        off = max(0, kbi * 128 - q0)
                        S = S0 if kbi % 2 == 0 else S1
                        p_ = pt[npt % 3]
                        npt += 1
                        kb.op("act", lambda e, S=S, off=off, p_=p_: e.activation(out=p_[:, off:512], in_=S[:, off:512], func=AF.Exp), reads=[S], writes=[p_])
                        if kbi * 128 >= q0:
                            kb.op("pool", lambda e, off=off, p_=p_: e.tensor_tensor(out=p_[:, off:off + 128], in0=p_[:, off:off + 128], in1=tri, op=ALU.mult),
                                  reads=[p_, cbuf], writes=[p_])
                        first, last = (kbi == 0), (kbi == nkb - 1)
                        kb.op("pe", lambda e, off=off, p_=p_, kbi=kbi, first=first, last=last: e.matmul(PO[:, off:512], lhsT=V[:, kbi, :], rhs=p_[:, off:512], start=first, stop=last),
                              reads=[V, p_], writes=[PO] if first else [], wpart=[] if first else [PO])
                        kb.op("pe", lambda e, off=off, p_=p_, first=first, last=last: e.matmul(PR[:, off:512], lhsT=ones_b, rhs=p_[:, off:512], start=first, stop=last),
                              reads=[p_, cbuf], writes=[PR] if first else [], wpart=[] if first else [PR])
                    o_ = ob[nob % 2]
                    nob += 1
                    kb.op("dve", lambda e: e.reciprocal(out=rr[:, :], in_=PR[:, :]), reads=[PR], writes=[rr])
                    kb.op("dve", lambda e, o_=o_: e.tensor_tensor(out=o_[:, :], in0=PO[:, :], in1=rr[:, :], op=ALU.mult), reads=[PO, rr], writes=[o_])
                    kb.dma("sp", self.brbT[h * 128:(h + 1) * 128, q0:q0 + 512], o_[:, :], reads=[o_])

    def phase_pool(self, l):
        nc, kb = self.nc, self.kb
        WIN = (2, 4, 8, 16)
        with ExitStack() as ps:
            inv = [self.sb(ps, "inv%d" % g, [128, T], F32) for g in range(4)]
            for g in range(4):
                kb.dma("sp", inv[g][:, :], self.poolinv[g].partition_broadcast(128), writes=[inv[g]])
            bp = self.sb(ps, "bp", [128, 8], F32)
            sp_ = self.sb(ps, "spl", [128, 8], F32)
            kb.dma("sp", bp[:, :], self.bpT[l], writes=[bp])
            kb.dma("sp", sp_[:, :], self.spT[l], writes=[sp_])
            kb.op("dve", lambda e: e.tensor_tensor(out=bp[:, :], in0=bp[:, :], in1=sp_[:, :], op=ALU.mult), reads=[bp, sp_], writes=[bp])
            pooled = self.sb(ps, "pooled", [128, 8, T], BF16)
            ub = [self.sb(ps, "ub%d" % i, [128, T], BF16) for i in range(2)]
            u32 = [self.sb(ps, "u32%d" % i, [128, T], F32) for i in range(2)]
            sa = [self.sb(ps, "sa%d" % i, [128, T], F32) for i in range(2)]
            for cc in range(8):
                g = cc // 2
                u_, f_ = ub[cc % 2], u32[cc % 2]
                kb.dma("sp", u_[:, :], self.upT[cc * 128:(cc + 1) * 128, :], writes=[u_])
                kb.op("act", lambda e, u_=u_, f_=f_: e.activation(out=f_[:, :], in_=u_[:, :], func=AF.Identity), reads=[u_], writes=[f_])
                cur = f_
                step = 1
                i = 0
                while step < WIN[g]:
                    nxt = sa[i % 2]
                    i += 1
                    kb.op("pool", lambda e, cur=cur, nxt=nxt, step=step: e.tensor_copy(out=nxt[:, 0:step], in_=cur[:, 0:step]),
                          reads=[cur], writes=[nxt])
                    kb.op("dve", lambda e, cur=cur, nxt=nxt, step=step: e.tensor_tensor(out=nxt[:, step:T], in0=cur[:, step:T], in1=cur[:, 0:T - step], op=ALU.add),
                          reads=[cur], wpart=[nxt])
                    cur = nxt
                    step *= 2
                kb.op("dve", lambda e, cur=cur, g=g: e.tensor_tensor(out=cur[:, :], in0=cur[:, :], in1=inv[g][:, :], op=ALU.mult),
                      reads=[cur, inv[g]], writes=[cur])
                kb.op("dve", lambda e, cur=cur, f_=f_, cc=cc: e.tensor_tensor(out=pooled[:, cc, :], in0=cur[:, :], in1=f_[:, :], op=ALU.subtract),
                      reads=[cur, f_], wpart=[pooled])
            wp = self.sb(ps, "wp", [128, 8, 256], BF16)
            kb.dma("pool", wp[:, :, :], self.w_pool[l].rearrange("g (kc p) e -> p (g kc) e", p=128), writes=[wp])
            pacc = [self.pm(ps, "ppool%d" % i, [128, 512]) for i in range(8)]
            ot = [self.sb(ps, "pot%d" % i, [128, T], BF16) for i in range(2)]
            n = 0
            for g in range(4):
                for eb in range(2):
                    j = g * 2 + eb
                    banks = pacc[(n % 2) * 4:(n % 2) * 4 + 4]
                    o = ot[n % 2]
                    n += 1
                    for kc in range(2):
                        for tt in range(4):
                            p = banks[tt]
                            kb.op("pe", lambda e, p=p, g=g, kc=kc, eb=eb, tt=tt: e.matmul(
                                p[:, :], lhsT=wp[:, g * 2 + kc, eb * 128:(eb + 1) * 128], rhs=pooled[:, g * 2 + kc, tt * 512:(tt + 1) * 512],
                                start=(kc == 0), stop=(kc == 1)), reads=[wp, pooled], writes=[p] if kc == 0 else [], wpart=[p] if kc else [])
                    for tt in range(4):
                        p = banks[tt]
                        kb.op("act", lambda e, p=p, o=o, tt=tt, j=j: e.activation(
                            out=o[:, tt * 512:(tt + 1) * 512], in_=p[:, :], func=AF.Identity, bias=bp[:, j:j + 1], scale=sp_[:, j:j + 1]),
                            reads=[p, bp, sp_], writes=[o] if tt == 0 else [], wpart=[o] if tt else [])
                    kb.dma("sp", self.brcT[j * 128:(j + 1) * 128, :], o[:, :], reads=[o])

    def phase_merge(self, l, xin):
        nc, kb = self.nc, self.kb
        with ExitStack() as ps:
            br = [self.sb(ps, "br%d" % j, [128, 8, T], BF16) for j in range(3)]
            for j, src in enumerate((self.braT, self.brbT, self.brcT)):
                kb.dma("sp", br[j][:, :, :], src.rearrange("(kc p) t -> p kc t", p=128), writes=[br[j]])
            ws = self.WStream(self, ps, "wb", 8, 512, 6)
            pacc = [self.pm(ps, "pmg%d" % i, [128, 512]) for i in range(8)]
            gt = [self.sb(ps, "gt%d" % i, [128, T], BF16) for i in range(3)]
            acc = [self.sb(ps, "macc%d" % i, [128, T], F32) for i in range(2)]
            tmp = [self.sb(ps, "mtmp%d" % i, [128, 512], F32) for i in range(3)]
            mo = [self.sb(ps, "mo%d" % i, [128, T], BF16) for i in range(2)]
            n = 0
            ng = 0
            nt = 0
            def wl(dt_):
                return [ws.load(self.w_branch[l, j][:, dt_ * 512:(dt_ + 1) * 512], 512) for j in range(3)]

            pend = {0: wl(0)}
            for dt in range(4):
                if dt + 1 < 4:
                    pend[dt + 1] = wl(dt + 1)
                wts = pend.pop(dt)
                for f in range(4):
                    dblk = dt * 4 + f
                    a_ = acc[dblk % 2]
                    m_ = mo[dblk % 2]
                    for j in range(3):
                        banks = pacc[(n % 2) * 4:(n % 2) * 4 + 4]
                        n += 1
                        g_ = gt[ng % 3]
                        ng += 1
                        r0 = j * D + dblk * 128
                        kb.dma("sp", g_[:, :], self.gatesT[r0:r0 + 128, :], writes=[g_])
                        for k in range(8):
                            for tt in range(4):
                                p = banks[tt]
                                kb.op("pe", lambda e, p=p, k=k, tt=tt, f=f, j=j, w=wts[j]: e.matmul(
                                    p[:, :], lhsT=w[:, k, f * 128:(f + 1) * 128], rhs=br[j][:, k, tt * 512:(tt + 1) * 512],
                                    start=(k == 0), stop=(k == 7)), reads=[wts[j], br[j]], writes=[p] if k == 0 else [], wpart=[p] if k else [])
                        for tt in range(4):
                            p = banks[tt]
                            sl = slice(tt * 512, (tt + 1) * 512)
                            if j == 0:
                                kb.op("dve", lambda e, p=p, g_=g_, a_=a_, sl=sl: e.tensor_tensor(out=a_[:, sl], in0=p[:, :], in1=g_[:, sl], op=ALU.mult),
                                      reads=[p, g_], writes=[a_] if tt == 0 else [], wpart=[a_] if tt else [])
                            else:
                                t_ = tmp[nt % 3]
                                nt += 1
                                kb.op("dve", lambda e, p=p, g_=g_, t_=t_, sl=sl: e.tensor_tensor(out=t_[:, :], in0=p[:, :], in1=g_[:, sl], op=ALU.mult),
                                      reads=[p, g_], writes=[t_])
                                if j == 1:
                                    kb.op("pool", lambda e, a_=a_, t_=t_, sl=sl: e.tensor_tensor(out=a_[:, sl], in0=a_[:, sl], in1=t_[:, :], op=ALU.add),
                                          reads=[a_, t_], wpart=[a_])
                                else:
                                    kb.op("pool", lambda e, a_=a_, t_=t_, m_=m_, sl=sl: e.tensor_tensor(out=m_[:, sl], in0=a_[:, sl], in1=t_[:, :], op=ALU.add),
                                          reads=[a_, t_], writes=[m_] if tt == 0 else [], wpart=[m_] if tt else [])
                    kb.dma("act", self.mergedT[dblk * 128:(dblk + 1) * 128, :], m_[:, :], reads=[m_])
        kb.barrier()
        with ExitStack() as ps:
            mT = self.sb(ps, "mT", [128, 16, T], BF16)
            kb.dma("sp", mT[:, :, :], self.mergedT.rearrange("(kc p) t -> p kc t", p=128), writes=[mT])
            xdst = self.xmid
            self.gemm_resid(ps, mT, 16, 0, NTB, self.w_out[l], self.mod_d[l, 2 * D:3 * D], xin, xdst, 0, "wo")

    def gemm_resid(self, ps, aT, KC, tb0, ntb, W, gate_row, xsrc, xdst, row0, tag):
        nc, kb = self.nc, self.kb
        gb = self.sb(ps, tag + "gb", [128, D], F32)
        kb.dma("sp", gb[:, :], gate_row.partition_broadcast(128), writes=[gb])
        nsp = 2 if KC > 16 else 1
        kh = KC // nsp
        ws = self.WStream(self, ps, tag + "w", kh, 512, 3 * nsp if nsp == 1 else 4)
        pacc = [self.pm(ps, tag + "p%d" % i, [128, 512]) for i in range(4)]
        xr = [self.sb(ps, tag + "xr%d" % i, [128, 512], F32) for i in range(3)]
        yy = [self.sb(ps, tag + "yy%d" % i, [128, 512], F32) for i in range(3)]
        n = 0
        tiles = [(ct, tb) for ct in range(4) for tb in range(ntb)]

        def xload(i):
            ct_, tb_ = tiles[i]
            rr_ = row0 + tb_ * 128
            kb.dma("sp", xr[i % 3][:, :], xsrc[rr_:rr_ + 128, ct_ * 512:(ct_ + 1) * 512], writes=[xr[i % 3]])

        xload(0)
        xload(1)
        def wload(ct_):
            return [ws.load(W[h * kh * 128:(h + 1) * kh * 128, ct_ * 512:(ct_ + 1) * 512], 512) for h in range(nsp)]

        pend = {0: wload(0)}
        for ct in range(4):
            c0 = ct * 512
            if ct + 1 < 4:
                pend[ct + 1] = wload(ct + 1)
            wts = pend.pop(ct)
            for tb in range(ntb):
                p = pacc[n % 4]
                x_ = xr[n % 3]
                y_ = yy[n % 3]
                if n + 2 < len(tiles):
                    xload(n + 2)
                n += 1
                r0 = row0 + tb * 128
                for k in range(KC):
                    w = wts[k // kh]
                    kb.op("pe", lambda e, p=p, k=k, tb=tb, w=w: e.matmul(
                        p[:, :], lhsT=aT[:, k, tb * 128:(tb + 1) * 128], rhs=w[:, k % kh, :], start=(k == 0), stop=(k == KC - 1)),
                        reads=[w, aT], writes=[p] if k == 0 else [], wpart=[p] if k else [])
                kb.op("dve", lambda e, p=p, y_=y_, c0=c0: e.tensor_tensor(out=y_[:, :], in0=p[:, :], in1=gb[:, c0:c0 + 512], op=ALU.mult),
                      reads=[p, gb], writes=[y_])
                kb.op("pool", lambda e, y_=y_, x_=x_: e.tensor_tensor(out=y_[:, :], in0=y_[:, :], in1=x_[:, :], op=ALU.add),
                      reads=[y_, x_], writes=[y_])
                kb.dma("sp", xdst[r0:r0 + 128, c0:c0 + 512], y_[:, :], reads=[y_])

    def phase_ffn(self, l, xo):
        nc, kb = self.nc, self.kb
        NH = 2
        TH = T // NH
        for th in range(NH):
            with ExitStack() as ps:
                actT = self.sb(ps, "actT", [128, 44, TH], BF16)
                with ExitStack() as ps2:
                    h2T = self.sb(ps2, "h2T", [128, 16, TH], BF16)
                    h2T.alt = h2T
                    with ExitStack() as ps3:
                        gs, sh = self.load_mod_cols(ps3, l, 2, self.g2T, "m2")
                        self.norm_to_hT(ps3, self.xmid, th * (TH // 128), TH // 128, h2T, gs, sh, "n2")
                    kb.barrier()
                    ws = self.WStream(self, ps2, "wf", 16, 512, 4)
                    pacc = [self.pm(ps2, "pf%d" % i, [128, 512]) for i in range(8)]
                    sg = [self.sb(ps2, "sg%d" % i, [128, 512], F32) for i in range(3)]
                    n = 0
                    ns = 0
                    W = self.w_ffn_in[l]
                    for ft in range(FFN // 512):
                        wg = ws.load(W[:, ft * 512:(ft + 1) * 512], 512)
                        wu = ws.load(W[:, FFN + ft * 512:FFN + (ft + 1) * 512], 512)
                        for f in range(4):
                            banks = pacc[(n % 2) * 4:(n % 2) * 4 + 4]
                            n += 1
                            for gi, w in enumerate((wg, wu)):
                                for k in range(16):
                                    for tt in range(TH // 512):
                                        p = banks[gi * 2 + tt]
                                        kb.op("pe", lambda e, p=p, k=k, tt=tt, f=f, w=w: e.matmul(
                                            p[:, :], lhsT=w[:, k, f * 128:(f + 1) * 128], rhs=h2T[:, k, tt * 512:(tt + 1) * 512],
                                            start=(k == 0), stop=(k == 15)), reads=[w, h2T, h2T.alt], writes=[p] if k == 0 else [], wpart=[p] if k else [])
                            for tt in range(TH // 512):
                                s_ = sg[ns % 3]
                                ns += 1
                                kb.op("act", lambda e, p=banks[tt], s_=s_: e.activation(out=s_[:, :], in_=p[:, :], func=AF.Silu), reads=[banks[tt]], writes=[s_])
                                kb.op("dve", lambda e, p=banks[2 + tt], s_=s_, fb=ft * 4 + f, tt=tt: e.tensor_tensor(
                                    out=actT[:, fb, tt * 512:(tt + 1) * 512], in0=p[:, :], in1=s_[:, :], op=ALU.mult),
                                    reads=[banks[2 + tt], s_], wpart=[actT])
                kb.barrier()
                with ExitStack() as ps2:
                    self.gemm_resid(ps2, actT, 44, 0, TH // 128, self.w_ffn_out[l], self.mod_d[l, 5 * D:6 * D], self.xmid, xo, th * TH, "fo")
            kb.barrier()


def _consts():
    i = np.arange(128)
    ident = np.eye(128, dtype=np.float32)
    U = (i[:, None] <= i[None, :]).astype(np.float32)
    negT = np.where(i[:, None] <= i[None, :], 0.0, NEG).astype(np.float32)
    neg = negT.T.copy()
    tri = U.copy()
    ones = np.ones((128, 128), np.float32)
    cst = np.concatenate([ident, U, negT, neg, tri, ones], axis=1)
    pos = np.arange(T, dtype=np.float32)
    freqs = (np.float32(10000.0) ** (-np.arange(0, 64, 2, dtype=np.float32) / np.float32(64))).astype(np.float32)
    ang = pos[:, None] * freqs[None, :]
    cos = np.cos(ang).astype(np.float32).T
    sin = np.sin(ang).astype(np.float32).T
    cosT = np.concatenate([cos, cos], axis=0)
    sinT = np.concatenate([sin, sin], axis=0)
    P = np.zeros((64, 64), np.float32)
    for m in range(32):
        P[m + 32, m] = -1.0
        P[m, m + 32] = 1.0
    ropec = np.concatenate([cosT, sinT, P], axis=1).astype(np.float32)
    tt = np.arange(T)
    poolinv = np.stack([1.0 / np.minimum(tt + 1, w) for w in (2, 4, 8, 16)]).astype(np.float32)
    return cst, ropec, poolinv


def _colT(v, n):
    return np.ascontiguousarray(v.reshape(n, 128).T)


def make_in_maps(inp, cores):
    f = lambda a: np.ascontiguousarray(np.asarray(a, dtype=np.float32))
    cst, ropec, poolinv = _consts()
    g = {k: f(v) for k, v in inp.items()}

    def colT_l(a, n):
        return np.stack([_colT(a[l], n) for l in range(DEPTH)])

    def qk192(a):
        o = np.zeros((DEPTH, 128, 2), np.float32)
        o[:, :, 0] = a[:, 0:128]
        o[:, 0:64, 1] = a[:, 128:192]
        return o

    shared = {
        "w_ada": g["w_ada"], "b_ada": g["b_ada"], "g1T": colT_l(g["g_norm1"], 16), "g2T": colT_l(g["g_norm2"], 16),
        "w_in": g["w_in"], "b_mgate": g["b_mgate"].reshape(DEPTH, 8), "g_mnorm": g["g_mnorm"].reshape(DEPTH, 1024),
        "gqlT": colT_l(g["g_qlat"], 4), "w_uq": g["w_uq"], "gkvT": colT_l(g["g_kvlat"], 2), "w_ukv": g["w_ukv"],
        "gqnT": qk192(g["g_qn"]), "gknT": qk192(g["g_kn"]), "w_pool": g["w_pool"],
        "bpT": colT_l(g["b_pool"].reshape(DEPTH, 1024), 8), "spT": colT_l(g["s_pool"], 8),
        "w_branch": g["w_branch"], "w_out": g["w_out"], "w_ffn_in": g["w_ffn_in"], "w_ffn_out": g["w_ffn_out"],
        "cst": cst, "ropec": ropec, "poolinv": poolinv,
    }
    maps = []
    for b in cores:
        m = dict(shared)
        m["x"] = g["x"][b]
        m["cT"] = _colT(g["c"][b], 16)
        maps.append(m)
    return maps


_CACHE = {}


def kernel(**inputs):
    cores = list(range(8))
    if "nc" not in _CACHE:
        _CACHE["nc"] = Prog().build()
    nc = _CACHE["nc"]
    maps = make_in_maps(inputs, cores)
    res = run_bass_kernel_spmd(nc, maps, core_ids=cores)
    return np.stack([np.asarray(r["out"], dtype=np.float32) for r in res.results], axis=0)
```

```python
import types
import numpy as np
from contextlib import ExitStack
import concourse.bass as bass
import concourse.mybir as mybir
from concourse.bass_utils import run_bass_kernel_spmd

F32 = mybir.dt.float32
BF16 = mybir.dt.bfloat16
AF = mybir.ActivationFunctionType
ALU = mybir.AluOpType
AX = mybir.AxisListType

D = 2048
T = 2048
DEPTH = 2
NTB = T // 128
FFN = 5632
IN_W = 11080
EPS = 1e-6
C_QM, C_KM, C_VM, C_OM, C_IF, C_QL, C_KV, C_KR, C_UP, C_GT = 0, 512, 1024, 2048, 3072, 3080, 3592, 3848, 3912, 4936
NEG = -30000.0


class Buf:
    __slots__ = ("t", "w", "r", "name", "alt")

    def __init__(self, t, name=""):
        self.t = t
        self.w = {}
        self.r = {}
        self.name = name
        self.alt = None

    def __getitem__(self, k):
        return self.t[k]


def _freeze(fn):
    if fn.__closure__ is None:
        return fn
    cells = []
    for c in fn.__closure__:
        try:
            cells.append(types.CellType(c.cell_contents))
        except ValueError:
            cells.append(c)
    return types.FunctionType(fn.__code__, fn.__globals__, fn.__name__, fn.__defaults__, tuple(cells))


class Op:
    __slots__ = ("fn", "waits", "dma")

    def __init__(self, fn, waits, dma=None):
        self.fn = fn
        self.waits = waits
        self.dma = dma


ENGS = ["pe", "act", "dve", "pool", "sp"]


class KB:
    def __init__(self, nc, es, n_dma=24):
        self.nc = nc
        self.ops = {e: [] for e in ENGS}
        self.known = {e: {} for e in ENGS}
        self.sigset = {e: set() for e in ENGS}
        self.lastc = {e: 0 for e in ENGS}
        self.n_dma = n_dma
        self.dma_val = [0] * n_dma
        self.dma_rr2 = {True: 0, False: 0}
        self.csem = {e: es.enter_context(nc.semaphore("c_" + e)) for e in ENGS if e != "sp"}
        self.dsem = [es.enter_context(nc.semaphore("d%d" % i)) for i in range(n_dma)]

    def _collect(self, eng, reads, writes, wpart=()):
        need = {}
        own = ("c", eng)
        for b in reads:
            for k, v in b.w.items():
                if v > need.get(k, 0):
                    need[k] = v
        for b in writes:
            for dct in (b.w, b.r):
                for k, v in dct.items():
                    if v > need.get(k, 0):
                        need[k] = v
        for b in wpart:
            for k, v in b.w.items():
                if k == own:
                    continue
                if v > need.get(k, 0):
                    need[k] = v
        if eng == "pe":
            need.pop(own, None)
        waits = []
        kn = self.known[eng]
        for k, v in need.items():
            if kn.get(k, 0) >= v:
                continue
            kn[k] = v
            waits.append((k, v))
            if k[0] == "c":
                self.sigset[k[1]].add(v)
        return waits

    def op(self, eng, fn, reads=(), writes=(), wpart=()):
        waits = self._collect(eng, reads, writes, wpart)
        self.ops[eng].append(Op(_freeze(fn), waits))
        idx = len(self.ops[eng])
        self.lastc[eng] = idx
        key = ("c", eng)
        for b in reads:
            b.r[key] = idx
        for b in writes:
            b.w = {key: idx}
            b.r = {}
        for b in wpart:
            b.w[key] = idx
        return idx

    def dma(self, q, out_ap, in_ap, reads=(), writes=(), wpart=(), slow=False):
        lo, hi = (0, 6) if q == "pool" else (6, self.n_dma)
        k = lo + self.dma_rr2[q == "pool"] % (hi - lo)
        self.dma_rr2[q == "pool"] += 1
        prev = self.dma_val[k]
        waits = self._collect(q, reads, writes, wpart)
        key = ("d", k)
        if prev > 0 and self.known[q].get(key, 0) < prev:
            waits.append((key, prev))
            self.known[q][key] = prev
        new = prev + 16
        self.dma_val[k] = new
        if slow:
            fn = lambda e: e.dma_start(out=out_ap, in_=in_ap, allow_slow_non_contiguous=True)
        else:
            fn = lambda e: e.dma_start(out=out_ap, in_=in_ap)
        self.ops[q].append(Op(fn, waits, dma=k))
        for b in reads:
            b.r[key] = new
        for b in writes:
            b.w = {key: new}
            b.r = {}
        for b in wpart:
            b.w[key] = new

    def barrier(self):
        outstanding = {}
        for e in ENGS:
            if e != "sp" and self.lastc[e] > 0:
                outstanding[("c", e)] = self.lastc[e]
        for k in range(self.n_dma):
            if self.dma_val[k] > 0:
                outstanding[("d", k)] = self.dma_val[k]
        for e in ENGS:
            waits = []
            kn = self.known[e]
            for k, v in outstanding.items():
                if kn.get(k, 0) >= v:
                    continue
                kn[k] = v
                waits.append((k, v))
                if k[0] == "c":
                    self.sigset[k[1]].add(v)
            if waits:
                self.ops[e].append(Op(lambda eng: eng.nop(), waits))

    def emit(self):
        nc = self.nc
        rank = {}
        for e in ENGS:
            rank[e] = {idx: i + 1 for i, idx in enumerate(sorted(self.sigset[e]))}

        def run(e, engobj):
            rk = rank[e]
            for i, op in enumerate(self.ops[e], start=1):
                for (k, v) in op.waits:
                    if k[0] == "c":
                        engobj.wait_ge(self.csem[k[1]], rank[k[1]][v])
                    else:
                        engobj.wait_ge(self.dsem[k[1]], v)
                ins = op.fn(engobj)
                if op.dma is not None:
                    ins.then_inc(self.dsem[op.dma], 16)
                elif i in rk:
                    ins.then_inc(self.csem[e], 1)

        with nc.Block() as block:
            @block.tensor
            def _(pe):
                run("pe", pe)

            @block.scalar
            def _(act):
                run("act", act)

            @block.vector
            def _(dve):
                run("dve", dve)

            @block.gpsimd
            def _(pool):
                run("pool", pool)

            @block.sync
            def _(sp):
                run("sp", sp)


class Prog:
    def __init__(self, debug=None, stop_after=None):
        self.debug = debug or []
        self.stop_after = stop_after
        self.nc = bass.Bass("TRN2", target_bir_lowering=False)
        self.es = ExitStack()

    def din(self, name, shape, dt=F32):
        return self.nc.dram_tensor(name, list(shape), dt, kind="ExternalInput").ap()

    def dscr(self, name, shape, dt=BF16):
        kind = "ExternalOutput" if name in self.debug else "Internal"
        return self.nc.dram_tensor(name, list(shape), dt, kind=kind).ap()

    def sb(self, ps, name, shape, dt):
        self.uid = getattr(self, "uid", 0) + 1
        name = "%s_%d" % (name, self.uid)
        return Buf(ps.enter_context(self.nc.sbuf_tensor(name, list(shape), dt)), name)

    def pm(self, ps, name, shape, dt=F32):
        self.uid = getattr(self, "uid", 0) + 1
        name = "%s_%d" % (name, self.uid)
        return Buf(ps.enter_context(self.nc.psum_tensor(name, list(shape), dt)), name)

    def build(self):
        nc = self.nc
        es = self.es
        kb = self.kb = KB(nc, es)
        self.x = self.din("x", [T, D])
        self.cT = self.din("cT", [128, 16])
        self.w_ada = self.din("w_ada", [DEPTH, D, 6 * D])
        self.b_ada = self.din("b_ada", [DEPTH, 6 * D])
        self.g1T = self.din("g1T", [DEPTH, 128, 16])
        self.g2T = self.din("g2T", [DEPTH, 128, 16])
        self.w_in = self.din("w_in", [DEPTH, D, IN_W])
        self.b_mgate = self.din("b_mgate", [DEPTH, 8])
        self.g_mnorm = self.din("g_mnorm", [DEPTH, 1024])
        self.gqlT = self.din("gqlT", [DEPTH, 128, 4])
        self.w_uq = self.din("w_uq", [DEPTH, 512, 1536])
        self.gkvT = self.din("gkvT", [DEPTH, 128, 2])
        self.w_ukv = self.din("w_ukv", [DEPTH, 256, 2048])
        self.gqnT = self.din("gqnT", [DEPTH, 128, 2])
        self.gknT = self.din("gknT", [DEPTH, 128, 2])
        self.w_pool = self.din("w_pool", [DEPTH, 4, 256, 256])
        self.bpT = self.din("bpT", [DEPTH, 128, 8])
        self.spT = self.din("spT", [DEPTH, 128, 8])
        self.w_branch = self.din("w_branch", [DEPTH, 3, 1024, D])
        self.w_out = self.din("w_out", [DEPTH, D, D])
        self.w_ffn_in = self.din("w_ffn_in", [DEPTH, D, 2 * FFN])
        self.w_ffn_out = self.din("w_ffn_out", [DEPTH, FFN, D])
        self.cst = self.din("cst", [128, 128 * 6])
        self.ropec = self.din("ropec", [64, 2 * T + 64])
        self.poolinv = self.din("poolinv", [4, T])
        self.out = self.nc.dram_tensor("out", [T, D], F32, kind="ExternalOutput").ap()
        self.mod_d = self.dscr("mod_d", [DEPTH, 6 * D], F32)
        self.qmT = self.dscr("qmT", [512, T])
        self.kmT = self.dscr("kmT", [512, T])
        self.k_tm = self.dscr("k_tm", [T, 512])
        self.v_tm = self.dscr("v_tm", [T, 1024])
        self.so_tm = self.dscr("so_tm", [T, 1024])
        self.if_d = self.dscr("if_d", [T, 8], F32)
        self.qlatT = self.dscr("qlatT", [512, T])
        self.kvlatT = self.dscr("kvlatT", [256, T])
        self.krT = self.dscr("krT", [64, T])
        self.upT = self.dscr("upT", [1024, T])
        self.gatesT = self.dscr("gatesT", [6144, T])
        self.braT = self.dscr("braT", [1024, T])
        self.brbT = self.dscr("brbT", [1024, T])
        self.brcT = self.dscr("brcT", [1024, T])
        self.mergedT = self.dscr("mergedT", [D, T])
        self.xmid = self.dscr("xmid", [T, D], F32)
        self.x1 = self.dscr("x1", [T, D], F32)

        self.cf = self.sb(es, "cf", [128, 128 * 6], F32)
        self.cb = self.sb(es, "cb", [128, 128 * 6], BF16)
        kb.dma("sp", self.cf[:, :], self.cst[:, :], writes=[self.cf])
        kb.dma("pool", self.cb[:, :], self.cst[:, :], writes=[self.cb])
        self.stages = [
            ("mod", lambda: self.phase_mod()),
        ]
        xin = self.x
        for l in range(DEPTH):
            xo = self.out if l == DEPTH - 1 else self.x1
            self.stages += [
                ("A%d" % l, lambda l=l, xin=xin: self.phase_win(l, xin)),
                ("C%d" % l, lambda l=l: self.phase_mlstm(l)),
                ("D%d" % l, lambda l=l: self.phase_mla(l)),
                ("P%d" % l, lambda l=l: self.phase_pool(l)),
                ("E%d" % l, lambda l=l, xin=xin: self.phase_merge(l, xin)),
                ("F%d" % l, lambda l=l, xo=xo: self.phase_ffn(l, xo)),
            ]
            xin = xo
        for name, fn in self.stages:
            fn()
            kb.barrier()
            if self.stop_after == name:
                break
        kb.barrier()
        kb.emit()
        return nc

    def cF(self, i):
        return self.cf[:, i * 128:(i + 1) * 128]

    def cB(self, i):
        return self.cb[:, i * 128:(i + 1) * 128]

    def phase_mod(self):
        nc, kb = self.nc, self.kb
        with ExitStack() as ps:
            ct = self.sb(ps, "ct", [128, 16], F32)
            ca = self.sb(ps, "ca", [128, 16], F32)
            wt = [self.sb(ps, "wada%d" % i, [128, 16, 512], F32) for i in range(2)]
            pr = [self.pm(ps, "pmod%d" % i, [1, 512]) for i in range(2)]
            brow = self.sb(ps, "brow", [1, DEPTH * 6 * D], F32)
            orow = [self.sb(ps, "orow%d" % i, [1, 512], F32) for i in range(2)]
            kb.dma("sp", ct[:, :], self.cT[:, :], writes=[ct])
            kb.dma("sp", brow[:, :], self.b_ada.rearrange("l n -> (l n)").rearrange("(o n) -> o n", o=1), writes=[brow])
            kb.op("act", lambda e: e.activation(out=ca[:, :], in_=ct[:, :], func=AF.Silu), reads=[ct], writes=[ca])
            tiles = [(l, cbk) for l in range(DEPTH) for cbk in range(24)]

            def load(i):
                l, cbk = tiles[i]
                src = self.w_ada[l, :, cbk * 512:(cbk + 1) * 512].rearrange("(kc p) n -> p kc n", p=128)
                kb.dma("sp", wt[i % 2][:, :, :], src, writes=[wt[i % 2]])

            load(0)
            for i, (l, cbk) in enumerate(tiles):
                if i + 1 < len(tiles):
                    load(i + 1)
                w = wt[i % 2]
                p = pr[i % 2]
                o = orow[i % 2]
                for kc in range(16):
                    kb.op("pe", lambda e, w=w, p=p, kc=kc: e.matmul(p[0:1, :], lhsT=ca[:, kc:kc + 1], rhs=w[:, kc, :],
                                                                    start=(kc == 0), stop=(kc == 15)),
                          reads=[ca, w], writes=[p] if kc == 0 else [], wpart=[p] if kc else [])
                off = l * 6 * D + cbk * 512
                kb.op("dve", lambda e, p=p, o=o, off=off: e.tensor_tensor(out=o[0:1, :], in0=p[0:1, :], in1=brow[0:1, off:off + 512], op=ALU.add),
                      reads=[p, brow], writes=[o])
                kb.dma("sp", self.mod_d[l:l + 1, cbk * 512:(cbk + 1) * 512], o[0:1, :], reads=[o])

    def load_mod_cols(self, ps, l, which, gT, name):
        nc, kb = self.nc, self.kb
        sh = self.sb(ps, name + "sh", [128, 16], F32)
        sc = self.sb(ps, name + "sc", [128, 16], F32)
        g = self.sb(ps, name + "g", [128, 16], F32)
        gs = self.sb(ps, name + "gs", [128, 16], F32)
        base = 0 if which == 1 else 3 * D
        kb.dma("sp", sh[:, :], self.mod_d[l, base:base + D].rearrange("(j p) -> p j", p=128), writes=[sh], slow=True)
        kb.dma("sp", sc[:, :], self.mod_d[l, base + D:base + 2 * D].rearrange("(j p) -> p j", p=128), writes=[sc], slow=True)
        kb.dma("sp", g[:, :], gT[l], writes=[g])
        kb.op("dve", lambda e: e.tensor_scalar(out=sc[:, :], in0=sc[:, :], scalar1=1.0, scalar2=1.0, op0=ALU.add, op1=ALU.mult),
              reads=[sc], writes=[sc])
        kb.op("dve", lambda e: e.tensor_tensor(out=gs[:, :], in0=sc[:, :], in1=g[:, :], op=ALU.mult), reads=[sc, g], writes=[gs])
        return gs, sh

    def norm_to_hT(self, ps, xsrc, tb0, ntb, hT, gs, sh, tag):
        nc, kb = self.nc, self.kb
        xt = [self.sb(ps, tag + "xt%d" % i, [128, D], F32) for i in range(2)]
        junk = self.sb(ps, tag + "junk", [128, D], BF16)
        xn = [self.sb(ps, tag + "xn%d" % i, [128, D], BF16) for i in range(2)]
        ss = [self.sb(ps, tag + "ss%d" % i, [128, 1], F32) for i in range(2)]
        rs = [self.sb(ps, tag + "rs%d" % i, [128, 1], F32) for i in range(2)]
        pt = [self.pm(ps, tag + "pt%d" % i, [128, 1024], BF16) for i in range(4)]
        ident = self.cB(0)
        for j in range(ntb):
            tb = tb0 + j
            x_, xn_, ss_, rs_ = xt[j % 2], xn[j % 2], ss[j % 2], rs[j % 2]
            kb.dma("sp", x_[:, :], xsrc[tb * 128:(tb + 1) * 128, :], writes=[x_])
            kb.op("act", lambda e, x_=x_, ss_=ss_: e.activation(out=junk[:, :], in_=x_[:, :], func=AF.Square, accum_out=ss_[:, 0:1]),
                  reads=[x_], writes=[junk, ss_])
            kb.op("act", lambda e, ss_=ss_: e.activation(out=ss_[:, :], in_=ss_[:, :], func=AF.Sqrt, bias=EPS, scale=1.0 / D),
                  reads=[ss_], writes=[ss_])
            kb.op("dve", lambda e, ss_=ss_, rs_=rs_: e.reciprocal(out=rs_[:, :], in_=ss_[:, :]), reads=[ss_], writes=[rs_])
            kb.op("act", lambda e, x_=x_, xn_=xn_, rs_=rs_: e.activation(out=xn_[:, :], in_=x_[:, :], func=AF.Identity, scale=rs_[:, 0:1]),
                  reads=[x_, rs_], writes=[xn_])
            for half in range(2):
                p = pt[(j * 2 + half) % 4]
                for q in range(8):
                    dc = half * 8 + q
                    kb.op("pe", lambda e, p=p, q=q, dc=dc, xn_=xn_: e.transpose(p[:, q * 128:(q + 1) * 128], xn_[:, dc * 128:(dc + 1) * 128], ident),
                          reads=[xn_, self.cb], writes=[p] if q == 0 else [], wpart=[p] if q else [])
                for q in range(8):
                    dc = half * 8 + q
                    if half == 0:
                        kb.op("dve", lambda e, p=p, q=q, dc=dc, j=j: e.tensor_scalar(
                            out=hT[:, dc, j * 128:(j + 1) * 128], in0=p[:, q * 128:(q + 1) * 128],
                            scalar1=gs[:, dc:dc + 1], scalar2=sh[:, dc:dc + 1], op0=ALU.mult, op1=ALU.add),
                            reads=[p, gs, sh], wpart=[hT])
                    else:
                        kb.op("act", lambda e, p=p, q=q, dc=dc, j=j: e.activation(
                            out=hT[:, dc, j * 128:(j + 1) * 128], in_=p[:, q * 128:(q + 1) * 128],
                            func=AF.Identity, bias=sh[:, dc:dc + 1], scale=gs[:, dc:dc + 1]),
                            reads=[p, gs, sh], wpart=[hT.alt])

    class WStream:
        def __init__(self, prog, ps, name, kc, ncols, nbuf=3):
            self.prog = prog
            self.bufs = [prog.sb(ps, "%s%d" % (name, i), [128, kc, ncols], BF16) for i in range(nbuf)]
            self.i = 0
            self.kc = kc

        def load(self, src2d, n):
            b = self.bufs[self.i % len(self.bufs)]
            self.i += 1
            self.prog.kb.dma("pool", b[:, :, 0:n], src2d.rearrange("(kc p) n -> p kc n", p=128), writes=[b])
            return b

    def phase_win(self, l, xin):
        nc, kb = self.nc, self.kb
        with ExitStack() as ps:
            hT = self.sb(ps, "hT", [128, 16, T], BF16)
            hT.alt = hT
            with ExitStack() as ps2:
                gs, sh = self.load_mod_cols(ps2, l, 1, self.g1T, "m1")
                self.norm_to_hT(ps2, xin, 0, NTB, hT, gs, sh, "n1")
            kb.barrier()
            ws = self.WStream(self, ps, "wi", 16, 512, 3)
            pacc = [self.pm(ps, "pacc%d" % i, [128, 512]) for i in range(8)]
            ot = [self.sb(ps, "ot%d" % i, [128, T], BF16) for i in range(3)]
            otm = [self.sb(ps, "otm%d" % i, [128, 512], BF16) for i in range(3)]
            ifs = self.sb(ps, "ifs", [128, NTB, 8], F32)
            W = self.w_in[l]
            state = {"pset": 0, "ot": 0, "pb": 0, "otm": 0, "ev": 0}

            def fm_tile(c0, n, dst, func, scale):
                wt = ws.load(W[:, c0:c0 + n], n)
                for f0 in range(0, n, 128):
                    m = min(128, n - f0)
                    pset = state["pset"]
                    state["pset"] ^= 1
                    banks = pacc[pset * 4:(pset + 1) * 4]
                    for k in range(16):
                        for tt in range(4):
                            p = banks[tt]
                            kb.op("pe", lambda e, p=p, k=k, tt=tt, f0=f0, m=m, wt=wt: e.matmul(
                                p[0:m, :], lhsT=wt[:, k, f0:f0 + m], rhs=hT[:, k, tt * 512:(tt + 1) * 512],
                                start=(k == 0), stop=(k == 15)),
                                reads=[wt, hT, hT.alt], writes=[p] if k == 0 else [], wpart=[p] if k else [])
                    o = ot[state["ot"] % 3]
                    state["ot"] += 1
                    for tt in range(4):
                        p = banks[tt]
                        eng = "act" if (func is not None or tt % 2 == 1) else "dve"
                        first = (tt == 0)
                        if eng == "act":
                            kb.op("act", lambda e, p=p, o=o, tt=tt, m=m: e.activation(
                                out=o[0:m, tt * 512:(tt + 1) * 512], in_=p[0:m, :], func=(func or AF.Identity), scale=scale),
                                reads=[p], writes=[o] if first else [], wpart=[] if first else [o])
                        else:
                            kb.op("dve", lambda e, p=p, o=o, tt=tt, m=m: e.tensor_scalar(
                                out=o[0:m, tt * 512:(tt + 1) * 512], in0=p[0:m, :], scalar1=scale, scalar2=None, op0=ALU.mult),
                                reads=[p], writes=[o] if first else [], wpart=[] if first else [o])
                    r0 = c0 - dst[1] + f0
                    kb.dma("sp", dst[0][r0:r0 + m, :], o[0:m, :], reads=[o])

            def tm_tile(c0, n, dst, func, scale):
                wt = ws.load(W[:, c0:c0 + n], n)
                for tb in range(NTB):
                    p = pacc[state["pb"] % 8]
                    state["pb"] += 1
                    for k in range(16):
                        kb.op("pe", lambda e, p=p, k=k, tb=tb, wt=wt: e.matmul(
                            p[:, 0:n], lhsT=hT[:, k, tb * 128:(tb + 1) * 128], rhs=wt[:, k, 0:n],
                            start=(k == 0), stop=(k == 15)),
                            reads=[wt, hT, hT.alt], writes=[p] if k == 0 else [], wpart=[p] if k else [])
                    if dst is None:
                        kb.op("dve", lambda e, p=p, tb=tb: e.tensor_copy(out=ifs[:, tb, :], in_=p[:, 0:8]), reads=[p], wpart=[ifs])
                        continue
                    o = otm[state["otm"] % 3]
                    state["otm"] += 1
                    state["ev"] += 1
                    if func is not None or state["ev"] % 2:
                        kb.op("act", lambda e, p=p, o=o: e.activation(out=o[:, 0:n], in_=p[:, 0:n], func=(func or AF.Identity), scale=scale),
                              reads=[p], writes=[o])
                    else:
                        kb.op("dve", lambda e, p=p, o=o: e.tensor_scalar(out=o[:, 0:n], in0=p[:, 0:n], scalar1=scale, scalar2=None, op0=ALU.mult),
                              reads=[p], writes=[o])
                    cc = c0 - dst[1]
                    kb.dma("sp", dst[0][tb * 128:(tb + 1) * 128, cc:cc + n], o[:, 0:n], reads=[o])

            ksc = float(128 ** -0.5)
            tm_tile(C_IF, 8, None, None, 1.0)
            tm_tile(C_KM, 512, (self.k_tm, C_KM), None, ksc)
            for c0 in (C_VM, C_VM + 512):
                tm_tile(c0, 512, (self.v_tm, C_VM), None, 1.0)
            for c0 in (C_OM, C_OM + 512):
                tm_tile(c0, 512, (self.so_tm, C_OM), AF.Sigmoid, 1.0)
            kb.dma("sp", self.if_d.rearrange("(c p) e -> p c e", p=128), ifs[:, :, :], reads=[ifs])
            fm_tile(C_QM, 512, (self.qmT, C_QM), None, 1.0)
            fm_tile(C_KM, 512, (self.kmT, C_KM), None, ksc)
            fm_tile(C_QL, 512, (self.qlatT, C_QL), None, 1.0)
            fm_tile(C_KV, 256, (self.kvlatT, C_KV), None, 1.0)
            fm_tile(C_KR, 64, (self.krT, C_KR), None, 1.0)
            for c0 in (C_UP, C_UP + 512):
                fm_tile(c0, 512, (self.upT, C_UP), None, 1.0)
            for c0 in range(C_GT, IN_W, 512):
                fm_tile(c0, 512, (self.gatesT, C_GT), AF.Sigmoid, 1.0)

    def phase_mlstm(self, l):
        nc, kb = self.nc, self.kb
        with ExitStack() as ps:
            cf, cbf = self.cf, self.cb
            identF, U, negT, neg, onesF = self.cF(0), self.cF(1), self.cF(2), self.cF(3), self.cF(5)
            identB = self.cB(0)
            qT = self.sb(ps, "mqT", [128, 4, T], BF16)
            kT = self.sb(ps, "mkT", [128, 4, T], BF16)
            ktm = self.sb(ps, "mktm", [128, NTB, 512], BF16)
            vaug = self.sb(ps, "mvaug", [128, NTB, 4, 257], BF16)
            ifs = self.sb(ps, "mifs", [128, NTB, 8], F32)
            bmg = self.sb(ps, "mbmg", [128, 8], F32)
            gmn = self.sb(ps, "mgmn", [128, 1024], F32)
            kb.dma("sp", qT[:, :, :], self.qmT.rearrange("(h d) t -> d h t", d=128), writes=[qT])
            kb.dma("sp", kT[:, :, :], self.kmT.rearrange("(h d) t -> d h t", d=128), writes=[kT])
            kb.dma("sp", ktm[:, :, :], self.k_tm.rearrange("(c p) e -> p c e", p=128), writes=[ktm])
            kb.op("pool", lambda e: e.memset(vaug[:, :, :, :], 1.0), writes=[vaug])
            for c in range(NTB):
                kb.dma("sp", vaug[:, c, :, 0:256], self.v_tm[c * 128:(c + 1) * 128, :].rearrange("p (h v) -> p h v", h=4), reads=[], wpart=[vaug])
            kb.dma("sp", ifs[:, :, :], self.if_d.rearrange("(c p) e -> p c e", p=128), writes=[ifs])
            kb.dma("sp", bmg[:, :], self.b_mgate[l].partition_broadcast(128), writes=[bmg])
            kb.dma("sp", gmn[:, :], self.g_mnorm[l].partition_broadcast(128), writes=[gmn])
            NI = NTB * 4
            mk = lambda n, w=NI: self.sb(ps, n, [128, w], F32)
            ipre, fpre, logf, bcs, bL, g, cm, gmax = [mk("m" + n) for n in ("ipre", "fpre", "logf", "bcs", "bL", "g", "cm", "gmax")]
            mprev = mk("mprev", NI + 4)
            mxL, decay, wsc, mx, nmx, em = [mk("m" + n) for n in ("mxL", "decay", "wsc", "mx", "nmx", "em")]
            pP = self.pm(ps, "mpP", [128, 512])
            pT = self.pm(ps, "mpT", [128, 1024], BF16)
            pX = [self.pm(ps, "mpX%d" % i, [128, 384]) for i in range(2)]
            pN = [self.pm(ps, "mpN%d" % i, [128, 257]) for i in range(2)]
            pC = [self.pm(ps, "mpC%d" % i, [128, 257]) for i in range(2)]
            for c in range(NTB):
                kb.op("dve", lambda e, c=c: e.tensor_tensor(out=ipre[:, c * 4:(c + 1) * 4], in0=ifs[:, c, 0:4], in1=bmg[:, 0:4], op=ALU.add),
                      reads=[ifs, bmg], wpart=[ipre])
                kb.op("dve", lambda e, c=c: e.tensor_tensor(out=fpre[:, c * 4:(c + 1) * 4], in0=ifs[:, c, 4:8], in1=bmg[:, 4:8], op=ALU.add),
                      reads=[ifs, bmg], wpart=[fpre])
            kb.op("act", lambda e: e.activation(out=logf[:, :], in_=fpre[:, :], func=AF.Exp, scale=-1.0), reads=[fpre], writes=[logf])
            kb.op("act", lambda e: e.activation(out=logf[:, :], in_=logf[:, :], func=AF.Ln, bias=1.0, scale=1.0), reads=[logf], writes=[logf])
            kb.op("dve", lambda e: e.tensor_scalar(out=logf[:, :], in0=logf[:, :], scalar1=-1.0, scalar2=None, op0=ALU.mult), reads=[logf], writes=[logf])
            kb.op("pe", lambda e: e.matmul(pP[:, 0:NI], lhsT=U, rhs=logf[:, :], start=True, stop=True), reads=[logf, cf], writes=[pP])
            kb.op("dve", lambda e: e.tensor_copy(out=bcs[:, :], in_=pP[:, 0:NI]), reads=[pP], writes=[bcs])
            kb.op("pe", lambda e: e.matmul(pP[:, 0:NI], lhsT=onesF, rhs=logf[:, :], start=True, stop=True), reads=[logf, cf], writes=[pP])
            kb.op("dve", lambda e: e.tensor_copy(out=bL[:, :], in_=pP[:, 0:NI]), reads=[pP], writes=[bL])
            kb.op("dve", lambda e: e.tensor_tensor(out=g[:, :], in0=ipre[:, :], in1=bcs[:, :], op=ALU.subtract), reads=[ipre, bcs], writes=[g])
            gb = [self.sb(ps, "mgb%d" % i, [128, 128], F32) for i in range(2)]
            tmpm = [self.sb(ps, "mtmpm%d" % i, [128, 128], F32) for i in range(2)]
            for idx in range(NI):
                gb_ = gb[idx % 2]
                tm_ = tmpm[idx % 2]
                pg = pX[idx % 2]
                kb.op("pool", lambda e, gb_=gb_, idx=idx: e.tensor_copy(out=gb_[:, :], in_=g[:, idx:idx + 1].to_broadcast([128, 128])), reads=[g], writes=[gb_])
                kb.op("pe", lambda e, gb_=gb_, pg=pg: e.matmul(pg[:, 0:128], lhsT=gb_[:, :], rhs=identF, start=True, stop=True), reads=[gb_, cf], writes=[pg])
                kb.op("dve", lambda e, pg=pg, tm_=tm_: e.tensor_tensor(out=tm_[:, :], in0=pg[:, 0:128], in1=neg, op=ALU.add), reads=[pg, cf], writes=[tm_])
                kb.op("dve", lambda e, tm_=tm_, idx=idx: e.reduce_max(out=cm[:, idx:idx + 1], in_=tm_[:, :], axis=AX.X), reads=[tm_], wpart=[cm])
                kb.op("dve", lambda e, pg=pg, idx=idx: e.reduce_max(out=gmax[:, idx:idx + 1], in_=pg[:, 0:128], axis=AX.X), reads=[pg], wpart=[gmax])
            kb.op("dve", lambda e: e.memset(mprev[:, 0:4], 0.0), writes=[mprev])
            for c in range(NTB):
                sl = slice(c * 4, (c + 1) * 4)
                sl2 = slice((c + 1) * 4, (c + 2) * 4)
                kb.op("dve", lambda e, sl=sl: e.tensor_tensor(out=mxL[:, sl], in0=mprev[:, sl], in1=gmax[:, sl], op=ALU.max), reads=[mprev, gmax], writes=[mxL])
                kb.op("dve", lambda e, sl=sl, sl2=sl2: e.tensor_tensor(out=mprev[:, sl2], in0=bL[:, sl], in1=mxL[:, sl], op=ALU.add), reads=[bL, mxL], writes=[mprev])
            mp = mprev
            kb.op("dve", lambda e: e.tensor_tensor(out=decay[:, :], in0=mp[:, 0:NI], in1=mxL[:, :], op=ALU.subtract), reads=[mp, mxL], writes=[decay])
            kb.op("act", lambda e: e.activation(out=decay[:, :], in_=decay[:, :], func=AF.Exp), reads=[decay], writes=[decay])
            kb.op("dve", lambda e: e.tensor_tensor(out=wsc[:, :], in0=g[:, :], in1=mxL[:, :], op=ALU.subtract), reads=[g, mxL], writes=[wsc])
            kb.op("act", lambda e: e.activation(out=wsc[:, :], in_=wsc[:, :], func=AF.Exp), reads=[wsc], writes=[wsc])
            kb.op("dve", lambda e: e.tensor_tensor(out=mx[:, :], in0=mp[:, 0:NI], in1=cm[:, :], op=ALU.max), reads=[mp, cm], writes=[mx])
            kb.op("dve", lambda e: e.tensor_scalar(out=nmx[:, :], in0=mx[:, :], scalar1=-1.0, scalar2=None, op0=ALU.mult), reads=[mx], writes=[nmx])
            kb.op("dve", lambda e: e.tensor_tensor(out=em[:, :], in0=bcs[:, :], in1=mx[:, :], op=ALU.add), reads=[bcs, mx], writes=[em])
            kb.op("act", lambda e: e.activation(out=em[:, :], in_=em[:, :], func=AF.Exp, scale=-1.0), reads=[em], writes=[em])
            CT = [self.sb(ps, "mCT%d" % h, [128, 257], F32) for h in range(4)]
            CTb = [self.sb(ps, "mCTb%d" % h, [128, 257], BF16) for h in range(4)]
            for h in range(4):
                kb.op("pool", lambda e, h=h: e.memset(CT[h][:, :], 0.0), writes=[CT[h]])
                kb.op("pool", lambda e, h=h: e.memset(CTb[h][:, :], 0.0), writes=[CTb[h]])
            nmxb = [self.sb(ps, "mnmxb%d" % i, [128, 128], F32) for i in range(2)]
            Dt = [self.sb(ps, "mDt%d" % i, [128, 128], F32) for i in range(2)]
            Wb = [self.sb(ps, "mWb%d" % i, [128, 128], F32) for i in range(2)]
            SD = [self.sb(ps, "mSD%d" % i, [128, 128], BF16) for i in range(2)]
            qw = [self.sb(ps, "mqw%d" % i, [128, 128], BF16) for i in range(2)]
            dn = [self.sb(ps, "mdn%d" % i, [128, 1], F32) for i in range(2)]
            hm = [self.sb(ps, "mhm%d" % i, [128, 256], F32) for i in range(2)]
            junk = self.sb(ps, "mjunk", [128, 256], F32)
            ss2 = [self.sb(ps, "mss2%d" % i, [128, 1], F32) for i in range(2)]
            t1 = [self.sb(ps, "mt1%d" % i, [128, 256], F32) for i in range(2)]
            vw = [self.sb(ps, "mvw%d" % i, [128, 257], BF16) for i in range(2)]
            soc = [self.sb(ps, "msoc%d" % i, [128, 1024], BF16) for i in range(2)]
            brat = [self.sb(ps, "mbrat%d" % i, [128, 1024], BF16) for i in range(2)]
            braTs = [self.sb(ps, "mbraTs%d" % i, [128, 8, 128], BF16) for i in range(2)]
            for c in range(NTB):
                cs = slice(c * 128, (c + 1) * 128)
                so_ = soc[c % 2]
                bt_ = brat[c % 2]
                kb.dma("sp", so_[:, :], self.so_tm[cs, :], writes=[so_])
                def mstage(h, st):
                    idx = c * 4 + h
                    i2 = idx % 2
                    X, N_, C_ = pX[i2], pN[i2], pC[i2]
                    nb_, D_, W_, S_, q_, dn_, hm_, ss_, t1_, vw_ = nmxb[i2], Dt[i2], Wb[i2], SD[i2], qw[i2], dn[i2], hm[i2], ss2[i2], t1[i2], vw[i2]
                    if st == 0:
                        kb.op("pool", lambda e, nb_=nb_, idx=idx: e.tensor_copy(out=nb_[:, :], in_=nmx[:, idx:idx + 1].to_broadcast([128, 128])), reads=[nmx], writes=[nb_])
                        kb.op("pe", lambda e, X=X, nb_=nb_: e.matmul(X[:, 0:128], lhsT=nb_[:, :], rhs=identF, start=True, stop=False), reads=[nb_, cf], writes=[X])
                        kb.op("pe", lambda e, X=X: e.matmul(X[:, 0:128], lhsT=identF, rhs=negT, start=False, stop=True), reads=[cf], wpart=[X])
                        kb.op("pe", lambda e, X=X, nb_=nb_: e.matmul(X[:, 128:256], lhsT=nb_[:, :], rhs=identF, start=True, stop=True), reads=[nb_, cf], wpart=[X])
                        kb.op("pe", lambda e, X=X, h=h, cs=cs: e.matmul(X[:, 256:384], lhsT=kT[:, h, cs], rhs=qT[:, h, cs], start=True, stop=True), reads=[kT, qT], wpart=[X])
                    if st == 1:
                        kb.op("act", lambda e, X=X, D_=D_, idx=idx: e.activation(out=D_[:, :], in_=X[:, 0:128], func=AF.Exp, bias=g[:, idx:idx + 1], scale=1.0),
                              reads=[X, g], writes=[D_])
                        kb.op("act", lambda e, X=X, W_=W_, idx=idx: e.activation(out=W_[:, :], in_=X[:, 128:256], func=AF.Exp, bias=mp[:, idx:idx + 1], scale=1.0),
                              reads=[X, mp], writes=[W_])
                    if st == 2:
                        kb.op("dve", lambda e, X=X, D_=D_, S_=S_: e.tensor_tensor(out=S_[:, :], in0=X[:, 256:384], in1=D_[:, :], op=ALU.mult), reads=[X, D_], writes=[S_])
                        kb.op("dve", lambda e, W_=W_, q_=q_, h=h, cs=cs: e.tensor_tensor(out=q_[:, :], in0=qT[:, h, cs], in1=W_[:, :], op=ALU.mult), reads=[qT, W_], writes=[q_])
                    if st == 3:
                        kb.op("pe", lambda e, N_=N_, q_=q_, h=h: e.matmul(N_[:, :], lhsT=q_[:, :], rhs=CTb[h][:, :], start=True, stop=False), reads=[q_, CTb[h]], writes=[N_])
                        kb.op("pe", lambda e, N_=N_, S_=S_, c=c, h=h: e.matmul(N_[:, :], lhsT=S_[:, :], rhs=vaug[:, c, h, :], start=False, stop=True), reads=[S_, vaug], wpart=[N_])
                        kb.op("act", lambda e, vw_=vw_, c=c, h=h, idx=idx: e.activation(out=vw_[:, :], in_=vaug[:, c, h, :], func=AF.Identity, scale=wsc[:, idx:idx + 1]),
                              reads=[vaug, wsc], writes=[vw_])
                        kb.op("pe", lambda e, C_=C_, vw_=vw_, c=c, h=h: e.matmul(C_[:, :], lhsT=ktm[:, c, h * 128:(h + 1) * 128], rhs=vw_[:, :], start=True, stop=True),
                              reads=[ktm, vw_], writes=[C_])
                    if st == 4:
                        kb.op("dve", lambda e, C_=C_, h=h, idx=idx: e.scalar_tensor_tensor(out=CT[h][:, :], in0=CT[h][:, :], scalar=decay[:, idx:idx + 1], in1=C_[:, :],
                                                                                          op0=ALU.mult, op1=ALU.add), reads=[CT[h], decay, C_], writes=[CT[h]])
                        kb.op("act", lambda e, h=h: e.activation(out=CTb[h][:, :], in_=CT[h][:, :], func=AF.Identity), reads=[CT[h]], writes=[CTb[h]])

                    if st == 5:
                        kb.op("act", lambda e, N_=N_, dn_=dn_: e.activation(out=dn_[:, :], in_=N_[:, 256:257], func=AF.Abs), reads=[N_], writes=[dn_])
                        kb.op("dve", lambda e, dn_=dn_, idx=idx: e.tensor_tensor(out=dn_[:, :], in0=dn_[:, :], in1=em[:, idx:idx + 1], op=ALU.max), reads=[dn_, em], writes=[dn_])
                        kb.op("dve", lambda e, dn_=dn_: e.reciprocal(out=dn_[:, :], in_=dn_[:, :]), reads=[dn_], writes=[dn_])
                    if st == 6:
                        kb.op("act", lambda e, N_=N_, hm_=hm_, dn_=dn_: e.activation(out=hm_[:, :], in_=N_[:, 0:256], func=AF.Identity, scale=dn_[:, 0:1]), reads=[N_, dn_], writes=[hm_])
                        kb.op("act", lambda e, hm_=hm_, ss_=ss_: e.activation(out=junk[:, :], in_=hm_[:, :], func=AF.Square, accum_out=ss_[:, 0:1]), reads=[hm_], writes=[junk, ss_])
                        kb.op("act", lambda e, ss_=ss_: e.activation(out=ss_[:, :], in_=ss_[:, :], func=AF.Sqrt, bias=EPS, scale=1.0 / 256), reads=[ss_], writes=[ss_])
                        kb.op("dve", lambda e, ss_=ss_: e.reciprocal(out=ss_[:, :], in_=ss_[:, :]), reads=[ss_], writes=[ss_])
                        kb.op("dve", lambda e, hm_=hm_, ss_=ss_, t1_=t1_, h=h: e.scalar_tensor_tensor(out=t1_[:, :], in0=hm_[:, :], scalar=ss_[:, 0:1], in1=gmn[:, h * 256:(h + 1) * 256],
                                                                                                     op0=ALU.mult, op1=ALU.mult), reads=[hm_, ss_, gmn], writes=[t1_])
                        kb.op("dve", lambda e, t1_=t1_, h=h, so_=so_, bt_=bt_: e.tensor_tensor(out=bt_[:, h * 256:(h + 1) * 256], in0=t1_[:, :], in1=so_[:, h * 256:(h + 1) * 256], op=ALU.mult),
                              reads=[t1_, so_], writes=[bt_] if h == 0 else [], wpart=[bt_] if h else [])

                for pair in ((0, 1), (2, 3)):
                    for st in range(7):
                        for h in pair:
                            mstage(h, st)
                bs_ = braTs[c % 2]
                for q in range(8):
                    kb.op("pe", lambda e, q=q, bt_=bt_: e.transpose(pT[:, q * 128:(q + 1) * 128], bt_[:, q * 128:(q + 1) * 128], identB),
                          reads=[bt_, cbf], writes=[pT] if q == 0 else [], wpart=[pT] if q else [])
                kb.op("act", lambda e, bs_=bs_: e.activation(out=bs_[:, :, :], in_=pT[:, :].rearrange("p (q t) -> p q t", q=8), func=AF.Identity), reads=[pT], writes=[bs_])
                kb.dma("sp", self.braT[:, cs].rearrange("(q p) t -> p q t", p=128), bs_[:, :, :], reads=[bs_])

    def phase_mla(self, l):
        nc, kb = self.nc, self.kb
        with ExitStack() as ps:
            ones_b, tri = self.cB(5), self.cB(4)
            cbuf, cfb = self.cb, self.cf
            ql = self.sb(ps, "ql", [128, 4, T], BF16)
            kvl = self.sb(ps, "kvl", [128, 2, T], BF16)
            kr = self.sb(ps, "kr", [64, T], BF16)
            rc = self.sb(ps, "rc", [64, 2 * T + 64], F32)
            gql = self.sb(ps, "gql", [128, 4], F32)
            gkv = self.sb(ps, "gkv", [128, 2], F32)
            gqn = self.sb(ps, "gqn", [128, 2], F32)
            gkn = self.sb(ps, "gkn", [128, 2], F32)
            wuq = self.sb(ps, "wuq", [128, 4, 1536], BF16)
            wukv = self.sb(ps, "wukv", [128, 2, 2048], BF16)
            kb.dma("sp", ql[:, :, :], self.qlatT.rearrange("(kc p) t -> p kc t", p=128), writes=[ql])
            kb.dma("sp", kvl[:, :, :], self.kvlatT.rearrange("(kc p) t -> p kc t", p=128), writes=[kvl])
            kb.dma("sp", kr[:, :], self.krT[:, :], writes=[kr])
            kb.dma("sp", rc[:, :], self.ropec[:, :], writes=[rc])
            kb.dma("sp", gql[:, :], self.gqlT[l], writes=[gql])
            kb.dma("sp", gkv[:, :], self.gkvT[l], writes=[gkv])
            kb.dma("sp", gqn[:, :], self.gqnT[l], writes=[gqn])
            kb.dma("sp", gkn[:, :], self.gknT[l], writes=[gkn])
            kb.dma("pool", wuq[:, :, :], self.w_uq[l].rearrange("(kc p) n -> p kc n", p=128), writes=[wuq])
            kb.dma("pool", wukv[:, :, :], self.w_ukv[l].rearrange("(kc p) n -> p kc n", p=128), writes=[wukv])
            kb.op("dve", lambda e: e.tensor_scalar(out=gqn[:, :], in0=gqn[:, :], scalar1=float(192 ** -0.5), scalar2=None, op0=ALU.mult),
                  reads=[gqn], writes=[gqn])
            cosT = lambda a, b: rc[:, a:b]
            sinT = lambda a, b: rc[:, T + a:T + b]
            Pm = rc[:, 2 * T:2 * T + 64]
            PA, PB, PC, PD, S0, S1, PO, PR = [self.pm(ps, "pa%d" % i, [128, 512]) for i in range(8)]
            sq = [self.sb(ps, "sq%d" % i, [128, 512], BF16) for i in range(4)]
            rsd = [self.sb(ps, "rsd%d" % i, [128, 512], F32) for i in range(2)]
            cnt = {"sq": 0, "rs": 0}

            def rstd_from(pss, nfeat):
                r = rsd[cnt["rs"] % 2]
                cnt["rs"] += 1
                kb.op("act", lambda e: e.activation(out=r[:, :], in_=pss[:, :], func=AF.Sqrt, bias=EPS, scale=1.0 / nfeat), reads=[pss], writes=[r])
                kb.op("dve", lambda e: e.reciprocal(out=r[:, :], in_=r[:, :]), reads=[r], writes=[r])
                return r

            def fm_rmsnorm(src, KC, nfeat, gcol, dst):
                for tt in range(4):
                    sl = slice(tt * 512, (tt + 1) * 512)
                    for kc in range(KC):
                        q_ = sq[cnt["sq"] % 4]
                        cnt["sq"] += 1
                        kb.op("dve", lambda e, q_=q_, kc=kc: e.tensor_tensor(out=q_[:, :], in0=src[:, kc, sl], in1=src[:, kc, sl], op=ALU.mult),
                              reads=[src], writes=[q_])
                        kb.op("pe", lambda e, q_=q_, kc=kc: e.matmul(PC[:, :], lhsT=ones_b, rhs=q_[:, :], start=(kc == 0), stop=(kc == KC - 1)),
                              reads=[q_, cbuf], writes=[PC] if kc == 0 else [], wpart=[PC] if kc else [])
                    r = rstd_from(PC, nfeat)
                    for kc in range(KC):
                        kb.op("dve", lambda e, kc=kc, r=r: e.scalar_tensor_tensor(out=dst[:, kc, sl], in0=src[:, kc, sl], scalar=gcol[:, kc:kc + 1],
                                                                                in1=r[:, :], op0=ALU.mult, op1=ALU.mult),
                              reads=[src, gcol, r], wpart=[dst])

            qln = self.sb(ps, "qln", [128, 4, T], BF16)
            kvn = self.sb(ps, "kvn", [128, 2, T], BF16)
            fm_rmsnorm(ql, 4, 512, gql, qln)
            fm_rmsnorm(kvl, 2, 256, gkv, kvn)
            krr = self.sb(ps, "krr", [64, T], F32)
            sqkr = self.sb(ps, "sqkr", [64, T], BF16)
            krg = self.sb(ps, "krg", [64, 512], F32)
            ta = self.sb(ps, "ta", [64, 512], F32)
            tb_ = self.sb(ps, "tb_", [64, 512], F32)
            kb.op("dve", lambda e: e.tensor_tensor(out=sqkr[:, :], in0=kr[:, :], in1=kr[:, :], op=ALU.mult), reads=[kr], writes=[sqkr])

            def rope_apply(xf, sl0, sl1, out_ap, out_buf):
                kb.op("pe", lambda e: e.matmul(PD[0:64, :], lhsT=Pm, rhs=xf[:, :], start=True, stop=True), reads=[xf, rc], writes=[PD])
                kb.op("dve", lambda e: e.tensor_tensor(out=ta[:, :], in0=xf[:, :], in1=cosT(sl0, sl1), op=ALU.mult), reads=[xf, rc], writes=[ta])
                kb.op("dve", lambda e: e.tensor_tensor(out=tb_[:, :], in0=PD[0:64, :], in1=sinT(sl0, sl1), op=ALU.mult), reads=[PD, rc], writes=[tb_])
                kb.op("pool", lambda e: e.tensor_tensor(out=out_ap, in0=ta[:, :], in1=tb_[:, :], op=ALU.add), reads=[ta, tb_], wpart=[out_buf])

            for tt in range(4):
                a, b = tt * 512, (tt + 1) * 512
                kb.op("dve", lambda e, a=a, b=b: e.tensor_scalar(out=krg[:, :], in0=kr[:, a:b], scalar1=gkn[0:64, 1:2], scalar2=None, op0=ALU.mult),
                      reads=[kr, gkn], writes=[krg])
                rope_apply(krg, a, b, krr[:, a:b], krr)

            QnT = [self.sb(ps, "QnT%d" % i, [128, T], BF16) for i in range(2)]
            QrT = [self.sb(ps, "QrT%d" % i, [64, T], BF16) for i in range(2)]
            KnT = [self.sb(ps, "KnT%d" % i, [128, T], BF16) for i in range(2)]
            KrT = [self.sb(ps, "KrT%d" % i, [64, T], BF16) for i in range(2)]
            Vh = [self.sb(ps, "Vh%d" % i, [128, NTB, 128], BF16) for i in range(2)]
            qrf = self.sb(ps, "qrf", [64, 512], F32)
            pt = [self.sb(ps, "pt%d" % i, [128, 512], BF16) for i in range(3)]
            rr = self.sb(ps, "rr", [128, 512], F32)
            ob = [self.sb(ps, "ob%d" % i, [128, 512], BF16) for i in range(2)]
            npt = 0
            nob = 0
            for h in range(8):
                Qn, Qr, Kn, Kr, V = QnT[h % 2], QrT[h % 2], KnT[h % 2], KrT[h % 2], Vh[h % 2]
                for tt in range(4):
                    a, b = tt * 512, (tt + 1) * 512
                    s1, s2, s3 = sq[cnt["sq"] % 4], sq[(cnt["sq"] + 1) % 4], sq[(cnt["sq"] + 2) % 4]
                    cnt["sq"] += 3
                    rq, rk = rsd[0], rsd[1]
                    ones64 = self.cb[0:64, 640:768]
                    for kc in range(4):
                        kb.op("pe", lambda e, kc=kc: e.matmul(PA[:, :], lhsT=wuq[:, kc, h * 192:h * 192 + 128], rhs=qln[:, kc, a:b],
                                                             start=(kc == 0), stop=(kc == 3)), reads=[wuq, qln], writes=[PA] if kc == 0 else [], wpart=[PA] if kc else [])
                    for kc in range(4):
                        kb.op("pe", lambda e, kc=kc: e.matmul(PB[0:64, :], lhsT=wuq[:, kc, h * 192 + 128:h * 192 + 192], rhs=qln[:, kc, a:b],
                                                             start=(kc == 0), stop=(kc == 3)), reads=[wuq, qln], writes=[PB] if kc == 0 else [], wpart=[PB] if kc else [])
                    for kc in range(2):
                        kb.op("pe", lambda e, kc=kc: e.matmul(S0[:, :], lhsT=wukv[:, kc, h * 256:h * 256 + 128], rhs=kvn[:, kc, a:b],
                                                             start=(kc == 0), stop=(kc == 1)), reads=[wukv, kvn], writes=[S0] if kc == 0 else [], wpart=[S0] if kc else [])
                    kb.op("act", lambda e: e.activation(out=s1[:, :], in_=PA[:, :], func=AF.Square), reads=[PA], writes=[s1])
                    kb.op("act", lambda e: e.activation(out=s2[0:64, :], in_=PB[0:64, :], func=AF.Square), reads=[PB], writes=[s2])
                    kb.op("act", lambda e: e.activation(out=s3[:, :], in_=S0[:, :], func=AF.Square), reads=[S0], writes=[s3])
                    kb.op("pe", lambda e: e.matmul(PC[:, :], lhsT=ones_b, rhs=s1[:, :], start=True, stop=False), reads=[s1, cbuf], writes=[PC])
                    kb.op("pe", lambda e: e.matmul(PC[:, :], lhsT=ones64, rhs=s2[0:64, :], start=False, stop=True), reads=[s2, cbuf], wpart=[PC])
                    kb.op("pe", lambda e: e.matmul(S1[:, :], lhsT=ones_b, rhs=s3[:, :], start=True, stop=False), reads=[s3, cbuf], writes=[S1])
                    kb.op("pe", lambda e: e.matmul(S1[:, :], lhsT=ones64, rhs=sqkr[:, a:b], start=False, stop=True), reads=[sqkr, cbuf], wpart=[S1])
                    kb.op("act", lambda e: e.activation(out=rq[:, :], in_=PC[:, :], func=AF.Sqrt, bias=EPS, scale=1.0 / 192), reads=[PC], writes=[rq])
                    kb.op("act", lambda e: e.activation(out=rk[:, :], in_=S1[:, :], func=AF.Sqrt, bias=EPS, scale=1.0 / 192), reads=[S1], writes=[rk])
                    kb.op("dve", lambda e: e.reciprocal(out=rq[:, :], in_=rq[:, :]), reads=[rq], writes=[rq])
                    kb.op("dve", lambda e: e.reciprocal(out=rk[:, :], in_=rk[:, :]), reads=[rk], writes=[rk])
                    kb.op("dve", lambda e: e.scalar_tensor_tensor(out=Qn[:, a:b], in0=PA[:, :], scalar=gqn[:, 0:1], in1=rq[:, :], op0=ALU.mult, op1=ALU.mult),
                          reads=[PA, gqn, rq], wpart=[Qn])
                    kb.op("dve", lambda e: e.scalar_tensor_tensor(out=qrf[:, :], in0=PB[0:64, :], scalar=gqn[0:64, 1:2], in1=rq[0:64, :], op0=ALU.mult, op1=ALU.mult),
                          reads=[PB, gqn, rq], writes=[qrf])
                    kb.op("dve", lambda e: e.scalar_tensor_tensor(out=Kn[:, a:b], in0=S0[:, :], scalar=gkn[:, 0:1], in1=rk[:, :], op0=ALU.mult, op1=ALU.mult),
                          reads=[S0, gkn, rk], wpart=[Kn])
                    kb.op("pool", lambda e: e.tensor_tensor(out=Kr[:, a:b], in0=krr[:, a:b], in1=rk[0:64, :], op=ALU.mult), reads=[krr, rk], wpart=[Kr])
                    rope_apply(qrf, a, b, Qr[:, a:b], Qr)
                for g4 in range(4):
                    for j in range(4):
                        tb = g4 * 4 + j
                        for kc in range(2):
                            kb.op("pe", lambda e, kc=kc, tb=tb, j=j: e.matmul(PB[:, j * 128:(j + 1) * 128], lhsT=kvn[:, kc, tb * 128:(tb + 1) * 128],
                                                                             rhs=wukv[:, kc, h * 256 + 128:h * 256 + 256], start=(kc == 0), stop=(kc == 1)),
                                  reads=[kvn, wukv], writes=[PB] if (j == 0 and kc == 0) else [], wpart=[] if (j == 0 and kc == 0) else [PB])
                    kb.op("act", lambda e, g4=g4: e.activation(out=V[:, g4 * 4:(g4 + 1) * 4, :], in_=PB[:, :].rearrange("p (j d) -> p j d", j=4), func=AF.Identity),
                          reads=[PB], wpart=[V])
                for qt in range(4):
                    q0 = qt * 512
                    nkb = 4 * (qt + 1)
                    def emit_S(kbi):
                        off = max(0, kbi * 128 - q0)
                        S = S0 if kbi % 2 == 0 else S1
                        ks = slice(kbi * 128, (kbi + 1) * 128)
                        kb.op("pe", lambda e, S=S, off=off, ks=ks: e.matmul(S[:, off:512], lhsT=Kn[:, ks], rhs=Qn[:, q0 + off:q0 + 512], start=True, stop=False),
                              reads=[Kn, Qn], writes=[S])
                        kb.op("pe", lambda e, S=S, off=off, ks=ks: e.matmul(S[:, off:512], lhsT=Kr[:, ks], rhs=Qr[:, q0 + off:q0 + 512], start=False, stop=True),
                              reads=[Kr, Qr], wpart=[S])

                    emit_S(0)
                    for kbi in range(nkb):
                        if kbi + 1 < nkb:
                            emit_S(kbi + 1)
                        off = max(0, kbi * 128 - q0)
                        S = S0 if kbi % 2 == 0 else S1
                        p_ = pt[npt % 3]
                        npt += 1
                        kb.op("act", lambda e, S=S, off=off, p_=p_: e.activation(out=p_[:, off:512], in_=S[:, off:512], func=AF.Exp), reads=[S], writes=[p_])
                        if kbi * 128 >= q0:
                            kb.op("pool", lambda e, off=off, p_=p_: e.tensor_tensor(out=p_[:, off:off + 128], in0=p_[:, off:off + 128], in1=tri, op=ALU.mult),
                                  reads=[p_, cbuf], writes=[p_])
                        first, last = (kbi == 0), (kbi == nkb - 1)
                        kb.op("pe", lambda e, off=off, p_=p_, kbi=kbi, first=first, last=last: e.matmul(PO[:, off:512], lhsT=V[:, kbi, :], rhs=p_[:, off:512], start=first, stop=last),
                              reads=[V, p_], writes=[PO] if first else [], wpart=[] if first else [PO])
                        kb.op("pe", lambda e, off=off, p_=p_, first=first, last=last: e.matmul(PR[:, off:512], lhsT=ones_b, rhs=p_[:, off:512], start=first, stop=last),
                              reads=[p_, cbuf], writes=[PR] if first else [], wpart=[] if first else [PR])
                    o_ = ob[nob % 2]
                    nob += 1
                    kb.op("dve", lambda e: e.reciprocal(out=rr[:, :], in_=PR[:, :]), reads=[PR], writes=[rr])
                    kb.op("dve", lambda e, o_=o_: e.tensor_tensor(out=o_[:, :], in0=PO[:, :], in1=rr[:, :], op=ALU.mult), reads=[PO, rr], writes=[o_])
                    kb.dma("sp", self.brbT[h * 128:(h + 1) * 128, q0:q0 + 512], o_[:, :], reads=[o_])

    def phase_pool(self, l):
        nc, kb = self.nc, self.kb
        WIN = (2, 4, 8, 16)
        with ExitStack() as ps:
            inv = [self.sb(ps, "inv%d" % g, [128, T], F32) for g in range(4)]
            for g in range(4):
                kb.dma("sp", inv[g][:, :], self.poolinv[g].partition_broadcast(128), writes=[inv[g]])
            bp = self.sb(ps, "bp", [128, 8], F32)
            sp_ = self.sb(ps, "spl", [128, 8], F32)
            kb.dma("sp", bp[:, :], self.bpT[l], writes=[bp])
            kb.dma("sp", sp_[:, :], self.spT[l], writes=[sp_])
            kb.op("dve", lambda e: e.tensor_tensor(out=bp[:, :], in0=bp[:, :], in1=sp_[:, :], op=ALU.mult), reads=[bp, sp_], writes=[bp])
            pooled = self.sb(ps, "pooled", [128, 8, T], BF16)
            ub = [self.sb(ps, "ub%d" % i, [128, T], BF16) for i in range(2)]
            u32 = [self.sb(ps, "u32%d" % i, [128, T], F32) for i in range(2)]
            sa = [self.sb(ps, "sa%d" % i, [128, T], F32) for i in range(2)]
            for cc in range(8):
                g = cc // 2
                u_, f_ = ub[cc % 2], u32[cc % 2]
                kb.dma("sp", u_[:, :], self.upT[cc * 128:(cc + 1) * 128, :], writes=[u_])
                kb.op("act", lambda e, u_=u_, f_=f_: e.activation(out=f_[:, :], in_=u_[:, :], func=AF.Identity), reads=[u_], writes=[f_])
                cur = f_
                step = 1
                i = 0
                while step < WIN[g]:
                    nxt = sa[i % 2]
                    i += 1
                    kb.op("pool", lambda e, cur=cur, nxt=nxt, step=step: e.tensor_copy(out=nxt[:, 0:step], in_=cur[:, 0:step]),
                          reads=[cur], writes=[nxt])
                    kb.op("dve", lambda e, cur=cur, nxt=nxt, step=step: e.tensor_tensor(out=nxt[:, step:T], in0=cur[:, step:T], in1=cur[:, 0:T - step], op=ALU.add),
                          reads=[cur], wpart=[nxt])
                    cur = nxt
                    step *= 2
                kb.op("dve", lambda e, cur=cur, g=g: e.tensor_tensor(out=cur[:, :], in0=cur[:, :], in1=inv[g][:, :], op=ALU.mult),
                      reads=[cur, inv[g]], writes=[cur])
                kb.op("dve", lambda e, cur=cur, f_=f_, cc=cc: e.tensor_tensor(out=pooled[:, cc, :], in0=cur[:, :], in1=f_[:, :], op=ALU.subtract),
                      reads=[cur, f_], wpart=[pooled])
            wp = self.sb(ps, "wp", [128, 8, 256], BF16)
            kb.dma("pool", wp[:, :, :], self.w_pool[l].rearrange("g (kc p) e -> p (g kc) e", p=128), writes=[wp])
            pacc = [self.pm(ps, "ppool%d" % i, [128, 512]) for i in range(8)]
            ot = [self.sb(ps, "pot%d" % i, [128, T], BF16) for i in range(2)]
            n = 0
            for g in range(4):
                for eb in range(2):
                    j = g * 2 + eb
                    banks = pacc[(n % 2) * 4:(n % 2) * 4 + 4]
                    o = ot[n % 2]
                    n += 1
                    for kc in range(2):
                        for tt in range(4):
                            p = banks[tt]
                            kb.op("pe", lambda e, p=p, g=g, kc=kc, eb=eb, tt=tt: e.matmul(
                                p[:, :], lhsT=wp[:, g * 2 + kc, eb * 128:(eb + 1) * 128], rhs=pooled[:, g * 2 + kc, tt * 512:(tt + 1) * 512],
                                start=(kc == 0), stop=(kc == 1)), reads=[wp, pooled], writes=[p] if kc == 0 else [], wpart=[p] if kc else [])
                    for tt in range(4):
                        p = banks[tt]
                        kb.op("act", lambda e, p=p, o=o, tt=tt, j=j: e.activation(
                            out=o[:, tt * 512:(tt + 1) * 512], in_=p[:, :], func=AF.Identity, bias=bp[:, j:j + 1], scale=sp_[:, j:j + 1]),
                            reads=[p, bp, sp_], writes=[o] if tt == 0 else [], wpart=[o] if tt else [])
                    kb.dma("sp", self.brcT[j * 128:(j + 1) * 128, :], o[:, :], reads=[o])

    def phase_merge(self, l, xin):
        nc, kb = self.nc, self.kb
        with ExitStack() as ps:
            br = [self.sb(ps, "br%d" % j, [128, 8, T], BF16) for j in range(3)]
            for j, src in enumerate((self.braT, self.brbT, self.brcT)):
                kb.dma("sp", br[j][:, :, :], src.rearrange("(kc p) t -> p kc t", p=128), writes=[br[j]])
            ws = self.WStream(self, ps, "wb", 8, 512, 6)
            pacc = [self.pm(ps, "pmg%d" % i, [128, 512]) for i in range(8)]
            gt = [self.sb(ps, "gt%d" % i, [128, T], BF16) for i in range(3)]
            acc = [self.sb(ps, "macc%d" % i, [128, T], F32) for i in range(2)]
            tmp = [self.sb(ps, "mtmp%d" % i, [128, 512], F32) for i in range(3)]
            mo = [self.sb(ps, "mo%d" % i, [128, T], BF16) for i in range(2)]
            n = 0
            ng = 0
            nt = 0
            def wl(dt_):
                return [ws.load(self.w_branch[l, j][:, dt_ * 512:(dt_ + 1) * 512], 512) for j in range(3)]

            pend = {0: wl(0)}
            for dt in range(4):
                if dt + 1 < 4:
                    pend[dt + 1] = wl(dt + 1)
                wts = pend.pop(dt)
                for f in range(4):
                    dblk = dt * 4 + f
                    a_ = acc[dblk % 2]
                    m_ = mo[dblk % 2]
                    for j in range(3):
                        banks = pacc[(n % 2) * 4:(n % 2) * 4 + 4]
                        n += 1
                        g_ = gt[ng % 3]
                        ng += 1
                        r0 = j * D + dblk * 128
                        kb.dma("sp", g_[:, :], self.gatesT[r0:r0 + 128, :], writes=[g_])
                        for k in range(8):
                            for tt in range(4):
                                p = banks[tt]
                                kb.op("pe", lambda e, p=p, k=k, tt=tt, f=f, j=j, w=wts[j]: e.matmul(
                                    p[:, :], lhsT=w[:, k, f * 128:(f + 1) * 128], rhs=br[j][:, k, tt * 512:(tt + 1) * 512],
                                    start=(k == 0), stop=(k == 7)), reads=[wts[j], br[j]], writes=[p] if k == 0 else [], wpart=[p] if k else [])
                        for tt in range(4):
                            p = banks[tt]
                            sl = slice(tt * 512, (tt + 1) * 512)
                            if j == 0:
                                kb.op("dve", lambda e, p=p, g_=g_, a_=a_, sl=sl: e.tensor_tensor(out=a_[:, sl], in0=p[:, :], in1=g_[:, sl], op=ALU.mult),
                                      reads=[p, g_], writes=[a_] if tt == 0 else [], wpart=[a_] if tt else [])
                            else:
                                t_ = tmp[nt % 3]
                                nt += 1
                                kb.op("dve", lambda e, p=p, g_=g_, t_=t_, sl=sl: e.tensor_tensor(out=t_[:, :], in0=p[:, :], in1=g_[:, sl], op=ALU.mult),
                                      reads=[p, g_], writes=[t_])
                                if j == 1:
                                    kb.op("pool", lambda e, a_=a_, t_=t_, sl=sl: e.tensor_tensor(out=a_[:, sl], in0=a_[:, sl], in1=t_[:, :], op=ALU.add),
                                          reads=[a_, t_], wpart=[a_])
                                else:
                                    kb.op("pool", lambda e, a_=a_, t_=t_, m_=m_, sl=sl: e.tensor_tensor(out=m_[:, sl], in0=a_[:, sl], in1=t_[:, :], op=ALU.add),
                                          reads=[a_, t_], writes=[m_] if tt == 0 else [], wpart=[m_] if tt else [])
                    kb.dma("act", self.mergedT[dblk * 128:(dblk + 1) * 128, :], m_[:, :], reads=[m_])
        kb.barrier()
        with ExitStack() as ps:
            mT = self.sb(ps, "mT", [128, 16, T], BF16)
            kb.dma("sp", mT[:, :, :], self.mergedT.rearrange("(kc p) t -> p kc t", p=128), writes=[mT])
            xdst = self.xmid
            self.gemm_resid(ps, mT, 16, 0, NTB, self.w_out[l], self.mod_d[l, 2 * D:3 * D], xin, xdst, 0, "wo")

    def gemm_resid(self, ps, aT, KC, tb0, ntb, W, gate_row, xsrc, xdst, row0, tag):
        nc, kb = self.nc, self.kb
        gb = self.sb(ps, tag + "gb", [128, D], F32)
        kb.dma("sp", gb[:, :], gate_row.partition_broadcast(128), writes=[gb])
        nsp = 2 if KC > 16 else 1
        kh = KC // nsp
        ws = self.WStream(self, ps, tag + "w", kh, 512, 3 * nsp if nsp == 1 else 4)
        pacc = [self.pm(ps, tag + "p%d" % i, [128, 512]) for i in range(4)]
        xr = [self.sb(ps, tag + "xr%d" % i, [128, 512], F32) for i in range(3)]
        yy = [self.sb(ps, tag + "yy%d" % i, [128, 512], F32) for i in range(3)]
        n = 0
        tiles = [(ct, tb) for ct in range(4) for tb in range(ntb)]

        def xload(i):
            ct_, tb_ = tiles[i]
            rr_ = row0 + tb_ * 128
            kb.dma("sp", xr[i % 3][:, :], xsrc[rr_:rr_ + 128, ct_ * 512:(ct_ + 1) * 512], writes=[xr[i % 3]])

        xload(0)
        xload(1)
        def wload(ct_):
            return [ws.load(W[h * kh * 128:(h + 1) * kh * 128, ct_ * 512:(ct_ + 1) * 512], 512) for h in range(nsp)]

        pend = {0: wload(0)}
        for ct in range(4):
            c0 = ct * 512
            if ct + 1 < 4:
                pend[ct + 1] = wload(ct + 1)
            wts = pend.pop(ct)
            for tb in range(ntb):
                p = pacc[n % 4]
                x_ = xr[n % 3]
                y_ = yy[n % 3]
                if n + 2 < len(tiles):
                    xload(n + 2)
                n += 1
                r0 = row0 + tb * 128
                for k in range(KC):
                    w = wts[k // kh]
                    kb.op("pe", lambda e, p=p, k=k, tb=tb, w=w: e.matmul(
                        p[:, :], lhsT=aT[:, k, tb * 128:(tb + 1) * 128], rhs=w[:, k % kh, :], start=(k == 0), stop=(k == KC - 1)),
                        reads=[w, aT], writes=[p] if k == 0 else [], wpart=[p] if k else [])
                kb.op("dve", lambda e, p=p, y_=y_, c0=c0: e.tensor_tensor(out=y_[:, :], in0=p[:, :], in1=gb[:, c0:c0 + 512], op=ALU.mult),
                      reads=[p, gb], writes=[y_])
                kb.op("pool", lambda e, y_=y_, x_=x_: e.tensor_tensor(out=y_[:, :], in0=y_[:, :], in1=x_[:, :], op=ALU.add),
                      reads=[y_, x_], writes=[y_])
                kb.dma("sp", xdst[r0:r0 + 128, c0:c0 + 512], y_[:, :], reads=[y_])

    def phase_ffn(self, l, xo):
        nc, kb = self.nc, self.kb
        NH = 2
        TH = T // NH
        for th in range(NH):
            with ExitStack() as ps:
                actT = self.sb(ps, "actT", [128, 44, TH], BF16)
                with ExitStack() as ps2:
                    h2T = self.sb(ps2, "h2T", [128, 16, TH], BF16)
                    h2T.alt = h2T
                    with ExitStack() as ps3:
                        gs, sh = self.load_mod_cols(ps3, l, 2, self.g2T, "m2")
                        self.norm_to_hT(ps3, self.xmid, th * (TH // 128), TH // 128, h2T, gs, sh, "n2")
                    kb.barrier()
                    ws = self.WStream(self, ps2, "wf", 16, 512, 4)
                    pacc = [self.pm(ps2, "pf%d" % i, [128, 512]) for i in range(8)]
                    sg = [self.sb(ps2, "sg%d" % i, [128, 512], F32) for i in range(3)]
                    n = 0
                    ns = 0
                    W = self.w_ffn_in[l]
                    for ft in range(FFN // 512):
                        wg = ws.load(W[:, ft * 512:(ft + 1) * 512], 512)
                        wu = ws.load(W[:, FFN + ft * 512:FFN + (ft + 1) * 512], 512)
                        for f in range(4):
                            banks = pacc[(n % 2) * 4:(n % 2) * 4 + 4]
                            n += 1
                            for gi, w in enumerate((wg, wu)):
                                for k in range(16):
                                    for tt in range(TH // 512):
                                        p = banks[gi * 2 + tt]
                                        kb.op("pe", lambda e, p=p, k=k, tt=tt, f=f, w=w: e.matmul(
                                            p[:, :], lhsT=w[:, k, f * 128:(f + 1) * 128], rhs=h2T[:, k, tt * 512:(tt + 1) * 512],
                                            start=(k == 0), stop=(k == 15)), reads=[w, h2T, h2T.alt], writes=[p] if k == 0 else [], wpart=[p] if k else [])
                            for tt in range(TH // 512):
                                s_ = sg[ns % 3]
                                ns += 1
                                kb.op("act", lambda e, p=banks[tt], s_=s_: e.activation(out=s_[:, :], in_=p[:, :], func=AF.Silu), reads=[banks[tt]], writes=[s_])
                                kb.op("dve", lambda e, p=banks[2 + tt], s_=s_, fb=ft * 4 + f, tt=tt: e.tensor_tensor(
                                    out=actT[:, fb, tt * 512:(tt + 1) * 512], in0=p[:, :], in1=s_[:, :], op=ALU.mult),
                                    reads=[banks[2 + tt], s_], wpart=[actT])
                kb.barrier()
                with ExitStack() as ps2:
                    self.gemm_resid(ps2, actT, 44, 0, TH // 128, self.w_ffn_out[l], self.mod_d[l, 5 * D:6 * D], self.xmid, xo, th * TH, "fo")
            kb.barrier()


def _consts():
    i = np.arange(128)
    ident = np.eye(128, dtype=np.float32)
    U = (i[:, None] <= i[None, :]).astype(np.float32)
    negT = np.where(i[:, None] <= i[None, :], 0.0, NEG).astype(np.float32)
    neg = negT.T.copy()
    tri = U.copy()
    ones = np.ones((128, 128), np.float32)
    cst = np.concatenate([ident, U, negT, neg, tri, ones], axis=1)
    pos = np.arange(T, dtype=np.float32)
    freqs = (np.float32(10000.0) ** (-np.arange(0, 64, 2, dtype=np.float32) / np.float32(64))).astype(np.float32)
    ang = pos[:, None] * freqs[None, :]
    cos = np.cos(ang).astype(np.float32).T
    sin = np.sin(ang).astype(np.float32).T
    cosT = np.concatenate([cos, cos], axis=0)
    sinT = np.concatenate([sin, sin], axis=0)
    P = np.zeros((64, 64), np.float32)
    for m in range(32):
        P[m + 32, m] = -1.0
        P[m, m + 32] = 1.0
    ropec = np.concatenate([cosT, sinT, P], axis=1).astype(np.float32)
    tt = np.arange(T)
    poolinv = np.stack([1.0 / np.minimum(tt + 1, w) for w in (2, 4, 8, 16)]).astype(np.float32)
    return cst, ropec, poolinv


def _colT(v, n):
    return np.ascontiguousarray(v.reshape(n, 128).T)


def make_in_maps(inp, cores):
    f = lambda a: np.ascontiguousarray(np.asarray(a, dtype=np.float32))
    cst, ropec, poolinv = _consts()
    g = {k: f(v) for k, v in inp.items()}

    def colT_l(a, n):
        return np.stack([_colT(a[l], n) for l in range(DEPTH)])

    def qk192(a):
        o = np.zeros((DEPTH, 128, 2), np.float32)
        o[:, :, 0] = a[:, 0:128]
        o[:, 0:64, 1] = a[:, 128:192]
        return o

    shared = {
        "w_ada": g["w_ada"], "b_ada": g["b_ada"], "g1T": colT_l(g["g_norm1"], 16), "g2T": colT_l(g["g_norm2"], 16),
        "w_in": g["w_in"], "b_mgate": g["b_mgate"].reshape(DEPTH, 8), "g_mnorm": g["g_mnorm"].reshape(DEPTH, 1024),
        "gqlT": colT_l(g["g_qlat"], 4), "w_uq": g["w_uq"], "gkvT": colT_l(g["g_kvlat"], 2), "w_ukv": g["w_ukv"],
        "gqnT": qk192(g["g_qn"]), "gknT": qk192(g["g_kn"]), "w_pool": g["w_pool"],
        "bpT": colT_l(g["b_pool"].reshape(DEPTH, 1024), 8), "spT": colT_l(g["s_pool"], 8),
        "w_branch": g["w_branch"], "w_out": g["w_out"], "w_ffn_in": g["w_ffn_in"], "w_ffn_out": g["w_ffn_out"],
        "cst": cst, "ropec": ropec, "poolinv": poolinv,
    }
    maps = []
    for b in cores:
        m = dict(shared)
        m["x"] = g["x"][b]
        m["cT"] = _colT(g["c"][b], 16)
        maps.append(m)
    return maps


_CACHE = {}


def kernel(**inputs):
    cores = list(range(8))
    if "nc" not in _CACHE:
        _CACHE["nc"] = Prog().build()
    nc = _CACHE["nc"]
    maps = make_in_maps(inputs, cores)
    res = run_bass_kernel_spmd(nc, maps, core_ids=cores)
    return np.stack([np.asarray(r["out"], dtype=np.float32) for r in res.results], axis=0)
```
